# Optimizing a Trainium2 kernel written in Bass

```python
import math
import jax, jax.numpy as jnp
from jax import lax
import numpy as np


D_MODEL = 2048
BATCH = 4
SEQ = 4096
DEPTH = 2

GRID_W = 64
CTX_LEN = 256
NORM_EPS = 1e-6
N_BRANCH = 4
BRANCH_WIDTH = D_MODEL // 2

SSD_HEADDIM = 64
SSD_D_INNER = BRANCH_WIDTH
SSD_HEADS = SSD_D_INNER // SSD_HEADDIM
SSD_GROUPS = 4
SSD_STATE = 128
SSD_CONV_DIM = SSD_D_INNER + 2 * SSD_GROUPS * SSD_STATE
SSD_CONV_K = 5
SSD_CHUNK = 128

NA_HEAD_DIM = 64
NA_HEADS = BRANCH_WIDTH // NA_HEAD_DIM
NA_WIN_R = 8
NA_WIN_C = 16

GLA_HEADS = 4
GLA_V_DIM = BRANCH_WIDTH
GLA_K_DIM = BRANCH_WIDTH // 2
GLA_HEAD_K = GLA_K_DIM // GLA_HEADS
GLA_HEAD_V = GLA_V_DIM // GLA_HEADS
GLA_GATE_RANK = 16
GLA_TAU = 16.0
GLA_CHUNK = 64

SWA_HEAD_DIM = 64
SWA_HEADS = BRANCH_WIDTH // SWA_HEAD_DIM
SWA_KV_HEADS = 4
SWA_WINDOW = 128
SWA_BLOCK = 128
ROPE_BASE = 10000.0
ROPE_AXIS_DIM = SWA_HEAD_DIM // 2

PEER_HEADS = 8
PEER_NKEYS = 128
PEER_EXPERTS = PEER_NKEYS * PEER_NKEYS
PEER_QDIM = 256
PEER_TOPK = 16
PEER_BLOCK = 128

IN_SPLITS = (SSD_D_INNER, SSD_CONV_DIM, 2 * SSD_HEADS,
             3 * BRANCH_WIDTH,
             GLA_K_DIM, GLA_K_DIM, GLA_V_DIM, GLA_V_DIM, 2 * GLA_GATE_RANK,
             SWA_HEADS * SWA_HEAD_DIM, SWA_KV_HEADS * SWA_HEAD_DIM, SWA_KV_HEADS * SWA_HEAD_DIM,
             N_BRANCH * D_MODEL)
IN_WIDTH = sum(IN_SPLITS)

F32 = jnp.float32

kernel_name = 'hybrid_ssd_na_gla_swa_peer_block'


def rms_norm(x, g):
    xf = x.astype(F32)
    y = xf * lax.rsqrt(jnp.mean(xf * xf, axis=-1, keepdims=True) + NORM_EPS)
    return (y * g.astype(F32)).astype(x.dtype)


def modulate(h, shift, scale):
    return h * (1 + scale) + shift


def split_cols(p):
    idx = np.cumsum(np.array(IN_SPLITS))[:-1].tolist()
    return jnp.split(p, idx, axis=-1)


def flip(t):
    return jnp.flip(t, axis=1)


def dwconv_centered(x, w, b):
    k = w.shape[0]
    y = lax.conv_general_dilated(x, w[:, None, :].astype(x.dtype), window_strides=(1,),
                                 padding=[(k // 2, k // 2)],
                                 dimension_numbers=('NWC', 'WIO', 'NWC'),
                                 feature_group_count=x.shape[-1])
    return y + b.astype(x.dtype)


def axial_rope_tables(s):
    t = jnp.arange(s)
    row = (t // GRID_W).astype(F32)
    col = (t % GRID_W).astype(F32)
    nf = ROPE_AXIS_DIM // 2
    inv = ROPE_BASE ** (-jnp.arange(nf, dtype=F32) / nf)
    ar = row[:, None] * inv
    ac = col[:, None] * inv
    return (jnp.cos(ar), jnp.sin(ar), jnp.cos(ac), jnp.sin(ac))


def apply_axial_rope(x, tabs):
    cr, sr, cc, sc = tabs
    xf = x.astype(F32)

    def rot(xa, cos, sin):
        x1, x2 = jnp.split(xa, 2, axis=-1)
        cos = cos[:, None, :]
        sin = sin[:, None, :]
        return jnp.concatenate([x1 * cos - x2 * sin, x1 * sin + x2 * cos], axis=-1)

    return jnp.concatenate([rot(xf[..., :ROPE_AXIS_DIM], cr, sr),
                            rot(xf[..., ROPE_AXIS_DIM:], cc, sc)], axis=-1).astype(x.dtype)


def ctx_attend(q, k, v, sink):
    bsz, n, hq, dh = q.shape
    hk = k.shape[2]
    grp = hq // hk
    qg = q.reshape(bsz, n, hk, grp, dh)
    logits = jnp.einsum('bqhgd,bkhd->bhgqk', qg, k).astype(F32) * dh ** -0.5
    if sink is not None:
        s_sink = jnp.broadcast_to(sink.astype(F32).reshape(hk, grp, 1, 1), logits.shape[:-1] + (1,))
        logits = jnp.concatenate([logits, s_sink], axis=-1)
    p = jax.nn.softmax(logits, axis=-1).astype(v.dtype)
    o = jnp.einsum('bhgqk,bkhd->bqhgd', p[..., :k.shape[1]], v)
    return o.reshape(bsz, n, hq * dh)


def segsum(a):
    n = a.shape[-1]
    cs = jnp.cumsum(a, axis=-1)
    diff = cs[..., :, None] - cs[..., None, :]
    mask = jnp.tril(jnp.ones((n, n), dtype=bool))
    return jnp.where(mask, diff, -jnp.inf)


def ssd_scan(xs, dt, a, bm, cm, h0):
    bsz, s, h, p = xs.shape
    n = bm.shape[-1]
    L = SSD_CHUNK
    nc = s // L
    xdt = (xs * dt[..., None]).reshape(bsz, nc, L, h, p)
    ad = (dt * a).reshape(bsz, nc, L, h).transpose(0, 3, 1, 2)
    bc = bm.reshape(bsz, nc, L, h, n)
    cc = cm.reshape(bsz, nc, L, h, n)
    a_cs = jnp.cumsum(ad, axis=-1)
    cb = jnp.einsum('bclhn,bcshn->bhcls', cc, bc) * jnp.exp(segsum(ad))
    y_diag = jnp.einsum('bhcls,bcshp->bclhp', cb, xdt)
    decay_states = jnp.exp(a_cs[..., -1:] - a_cs)
    states = jnp.einsum('bclhn,bhcl,bclhp->bchpn', bc, decay_states, xdt)
    if h0 is None:
        h0 = jnp.zeros_like(states[:, 0])
    states = jnp.concatenate([h0[:, None].astype(states.dtype), states], axis=1)
    decay_chunk = jnp.exp(segsum(jnp.pad(a_cs[..., -1], ((0, 0), (0, 0), (1, 0)))))
    new_states = jnp.einsum('bhzc,bchpn->bzhpn', decay_chunk, states)
    y_off = jnp.einsum('bclhn,bchpn,bhcl->bclhp', cc, new_states[:, :-1], jnp.exp(a_cs))
    return (y_diag + y_off).reshape(bsz, s, h, p), new_states[:, -1]


def ssd_bidir(xs, bm, cm, dt, a, h0f, h0b):
    yf, hf = ssd_scan(xs, dt[:, :, 0], a[0], bm, cm, h0f)
    yb, hb = ssd_scan(flip(xs), flip(dt[:, :, 1]), a[1], flip(bm), flip(cm), h0b)
    return yf + flip(yb), hf, hb


def ssd_branch(px, pc, conv_w, conv_b, a_log, dt_bias, d_skip, norm_g, need_ctx):
    a = -jnp.exp(a_log.astype(F32))

    def prep(xbc, dt_raw):
        bsz, s, _ = xbc.shape
        xbc = jax.nn.silu(dwconv_centered(xbc, conv_w, conv_b))
        xs, bm, cm = jnp.split(xbc, [SSD_D_INNER, SSD_D_INNER + SSD_GROUPS * SSD_STATE], axis=-1)
        rep = SSD_HEADS // SSD_GROUPS
        xs = xs.reshape(bsz, s, SSD_HEADS, SSD_HEADDIM)
        bm = jnp.repeat(bm.reshape(bsz, s, SSD_GROUPS, SSD_STATE), rep, axis=2)
        cm = jnp.repeat(cm.reshape(bsz, s, SSD_GROUPS, SSD_STATE), rep, axis=2)
        dt = jax.nn.softplus(dt_raw.reshape(bsz, s, 2, SSD_HEADS).astype(F32) + dt_bias.astype(F32))
        return xs, bm, cm, dt

    def finish(y, xs, z):
        bsz, s, _ = z.shape
        y = (y + d_skip.astype(F32)[:, None] * xs.astype(F32)).reshape(bsz, s, SSD_D_INNER).astype(z.dtype)
        return rms_norm(y * jax.nn.silu(z), norm_g)

    z, xbc, dt_raw = px
    zc, xbcc, dtc_raw = pc
    cx, cbm, ccm, cdt = prep(xbcc, dtc_raw)
    yc, hcf, hcb = ssd_bidir(cx, cbm, ccm, cdt, a, None, None)
    xs, bm, cm, dt = prep(xbc, dt_raw)
    y, _, _ = ssd_bidir(xs, bm, cm, dt, a, hcf, hcb)
    out_c = finish(yc, cx, zc) if need_ctx else None
    return finish(y, xs, z), out_c


def na_attend(q, k, v, kc, vc, rpb):
    bsz, s, h, dh = q.shape
    rows = s // GRID_W
    wr = min(NA_WIN_R, rows)
    n_nb = wr * NA_WIN_C
    scale = dh ** -0.5
    qg = q.reshape(bsz, rows, GRID_W, h, dh)
    kg = k.reshape(bsz, rows, GRID_W, h, dh)
    vg = v.reshape(bsz, rows, GRID_W, h, dh)
    cols = jnp.arange(GRID_W)
    c_start = jnp.clip(cols - NA_WIN_C // 2, 0, GRID_W - NA_WIN_C)
    col_idx = c_start[:, None] + jnp.arange(NA_WIN_C)[None, :]
    col_rel = col_idx - cols[:, None] + (NA_WIN_C - 1)

    def row_block(r):
        r0 = jnp.clip(r - wr // 2, 0, rows - wr)
        q_r = lax.dynamic_index_in_dim(qg, r, axis=1, keepdims=False)
        k_nb = lax.dynamic_slice_in_dim(kg, r0, wr, axis=1)[:, :, col_idx]
        v_nb = lax.dynamic_slice_in_dim(vg, r0, wr, axis=1)[:, :, col_idx]
        row_rel = r0 + jnp.arange(wr) - r + (NA_WIN_R - 1)
        bias = rpb[:, row_rel[:, None, None], col_rel[None, :, :]]
        bias = bias.transpose(0, 2, 1, 3).astype(F32)
        s_nb = jnp.einsum('bqhd,brqjhd->bhqrj', q_r, k_nb).astype(F32) * scale + bias
        s_cx = jnp.einsum('bqhd,bchd->bhqc', q_r, kc).astype(F32) * scale
        logits = jnp.concatenate([s_nb.reshape(bsz, h, GRID_W, n_nb), s_cx], axis=-1)
        p = jax.nn.softmax(logits, axis=-1).astype(v.dtype)
        p_nb = p[..., :n_nb].reshape(bsz, h, GRID_W, wr, NA_WIN_C)
        return (jnp.einsum('bhqrj,brqjhd->bqhd', p_nb, v_nb)
                + jnp.einsum('bhqc,bchd->bqhd', p[..., n_nb:], vc))

    o = lax.map(row_block, jnp.arange(rows))
    return o.transpose(1, 0, 2, 3, 4).reshape(bsz, s, h * dh)


def na_branch(pqkv, pqkvc, q_norm, k_norm, rpb, need_ctx):
    def heads(t):
        bsz, s, _ = t.shape
        t = t.reshape(bsz, s, 3, NA_HEADS, NA_HEAD_DIM)
        return rms_norm(t[:, :, 0], q_norm), rms_norm(t[:, :, 1], k_norm), t[:, :, 2]

    q, k, v = heads(pqkv)
    qc, kc, vc = heads(pqkvc)
    out = na_attend(q, k, v, kc, vc, rpb)
    out_c = ctx_attend(qc, kc, vc, None) if need_ctx else None
    return out, out_c


def gla_chunk_scan(q, k, v, g, s0):
    bsz, s, h, dk = q.shape
    dv = v.shape[-1]
    L = GLA_CHUNK
    nc = s // L
    q = q.reshape(bsz, nc, L, h, dk)
    k = k.reshape(bsz, nc, L, h, dk)
    v = v.reshape(bsz, nc, L, h, dv)
    gc = jnp.cumsum(g.reshape(bsz, nc, L, h, dk), axis=2)
    q_in = q * jnp.exp(gc)
    k_in = k * jnp.exp(-gc)
    k_out = k * jnp.exp(gc[:, :, -1:] - gc)
    att = jnp.einsum('bclhk,bcshk->bchls', q_in, k_in)
    att = jnp.where(jnp.tril(jnp.ones((L, L), dtype=bool)), att, 0.0)
    o_intra = jnp.einsum('bchls,bcshv->bclhv', att, v)
    s_chunk = jnp.einsum('bclhk,bclhv->bchkv', k_out, v)
    decay = jnp.exp(gc[:, :, -1])
    if s0 is None:
        s0 = jnp.zeros_like(s_chunk[:, 0])

    def step(state, inp):
        dec, sc = inp
        return state * dec[..., None] + sc, state

    s_final, s_before = lax.scan(step, s0.astype(s_chunk.dtype),
                                 (decay.transpose(1, 0, 2, 3), s_chunk.transpose(1, 0, 2, 3, 4)))
    o_inter = jnp.einsum('bclhk,cbhkv->bclhv', q_in, s_before)
    return (o_intra + o_inter).reshape(bsz, s, h, dv), s_final


def gla_bidir(q, k, v, g, s0f, s0b):
    of, sf = gla_chunk_scan(q, k, v, g[:, :, 0], s0f)
    ob, sb = gla_chunk_scan(flip(q), flip(k), flip(v), flip(g[:, :, 1]), s0b)
    return of + flip(ob), sf, sb


def gla_branch(px, pc, w_gate, b_gate, norm_g, need_ctx):
    def prep(q, k, v, glr):
        bsz, s, _ = q.shape
        q = q.reshape(bsz, s, GLA_HEADS, GLA_HEAD_K) * (GLA_HEAD_K ** -0.5)
        k = k.reshape(bsz, s, GLA_HEADS, GLA_HEAD_K)
        v = v.reshape(bsz, s, GLA_HEADS, GLA_HEAD_V)
        logit = jnp.einsum('bsdr,dre->bsde', glr.reshape(bsz, s, 2, GLA_GATE_RANK), w_gate) + b_gate
        g = (jax.nn.log_sigmoid(logit.astype(F32)) / GLA_TAU).reshape(bsz, s, 2, GLA_HEADS, GLA_HEAD_K)
        return q, k, v, g

    def finish(o, r):
        bsz, s, _ = r.shape
        o = rms_norm(o.astype(r.dtype), norm_g).reshape(bsz, s, GLA_V_DIM)
        return o * jax.nn.silu(r)

    q, k, v, r, glr = px
    qc, kc, vc, rc, glrc = pc
    cq, ck, cv, cg = prep(qc, kc, vc, glrc)
    oc, scf, scb = gla_bidir(cq, ck, cv, cg, None, None)
    lq, lk, lv, lg = prep(q, k, v, glr)
    o, _, _ = gla_bidir(lq, lk, lv, lg, scf, scb)
    out_c = finish(oc, rc) if need_ctx else None
    return finish(o, r), out_c


def swa_attend(q, k, v, kc, vc, sink):
    bsz, s, hq, dh = q.shape
    hk = k.shape[2]
    grp = hq // hk
    blk = SWA_BLOCK
    nb = s // blk
    scale = dh ** -0.5
    qb = q.reshape(bsz, nb, blk, hk, grp, dh)

    def band(t):
        tp = jnp.pad(t, ((0, 0), (blk, blk), (0, 0), (0, 0))).reshape(bsz, nb + 2, blk, hk, dh)
        return jnp.concatenate([tp[:, :-2], tp[:, 1:-1], tp[:, 2:]], axis=2)

    kb = band(k)
    vb = band(v)
    qpos = jnp.arange(s).reshape(nb, blk)
    kpos = jnp.arange(-blk, s + blk).reshape(nb + 2, blk)
    kpos = jnp.concatenate([kpos[:-2], kpos[1:-1], kpos[2:]], axis=1)
    valid = ((jnp.abs(qpos[:, :, None] - kpos[:, None, :]) <= SWA_WINDOW)
             & (kpos >= 0)[:, None, :] & (kpos < s)[:, None, :])
    s_loc = jnp.einsum('bnqhgd,bnkhd->bhgnqk', qb, kb).astype(F32) * scale
    s_loc = jnp.where(valid, s_loc, -jnp.inf)
    s_ctx = jnp.einsum('bnqhgd,bchd->bhgnqc', qb, kc).astype(F32) * scale
    s_sink = jnp.broadcast_to(sink.astype(F32).reshape(hk, grp, 1, 1, 1), s_loc.shape[:-1] + (1,))
    p = jax.nn.softmax(jnp.concatenate([s_loc, s_ctx, s_sink], axis=-1), axis=-1).astype(v.dtype)
    nk = 3 * blk
    nc = kc.shape[1]
    o = (jnp.einsum('bhgnqk,bnkhd->bnqhgd', p[..., :nk], vb)
         + jnp.einsum('bhgnqc,bchd->bnqhgd', p[..., nk:nk + nc], vc))
    return o.reshape(bsz, s, hq * dh)


def swa_branch(px, pc, q_norm, k_norm, sink, rope, need_ctx):
    def heads(q, k, v):
        bsz, s, _ = q.shape
        q = rms_norm(q.reshape(bsz, s, SWA_HEADS, SWA_HEAD_DIM), q_norm)
        k = rms_norm(k.reshape(bsz, s, SWA_KV_HEADS, SWA_HEAD_DIM), k_norm)
        return q, k, v.reshape(bsz, s, SWA_KV_HEADS, SWA_HEAD_DIM)

    q, k, v = heads(*px)
    qc, kc, vc = heads(*pc)
    q = apply_axial_rope(q, rope)
    k = apply_axial_rope(k, rope)
    out = swa_attend(q, k, v, kc, vc, sink)
    out_c = ctx_attend(qc, kc, vc, sink) if need_ctx else None
    return out, out_c


def merge_branches(gate_logits, outs, w_branch, w_out):
    g = jax.nn.sigmoid(gate_logits.reshape(gate_logits.shape[:-1] + (N_BRANCH, D_MODEL)))
    m = g[..., 0, :] * (outs[0] @ w_branch[0])
    for i in range(1, N_BRANCH):
        m = m + g[..., i, :] * (outs[i] @ w_branch[i])
    return m @ w_out


def mixer_sublayer(hx, hc, w_in, ssd_conv_w, ssd_conv_b, ssd_a_log, ssd_dt_bias, ssd_d, ssd_norm_g,
                   na_q_norm, na_k_norm, na_rpb, gla_w_gate, gla_b_gate, gla_norm_g,
                   swa_q_norm, swa_k_norm, swa_sink, w_branch, w_out, rope, need_ctx):
    px = split_cols(hx @ w_in)
    pc = split_cols(hc @ w_in)
    a_x, a_c = ssd_branch(px[0:3], pc[0:3], ssd_conv_w, ssd_conv_b, ssd_a_log, ssd_dt_bias, ssd_d,
                          ssd_norm_g, need_ctx)
    b_x, b_c = na_branch(px[3], pc[3], na_q_norm, na_k_norm, na_rpb, need_ctx)
    c_x, c_c = gla_branch(px[4:9], pc[4:9], gla_w_gate, gla_b_gate, gla_norm_g, need_ctx)
    d_x, d_c = swa_branch(px[9:12], pc[9:12], swa_q_norm, swa_k_norm, swa_sink, rope, need_ctx)
    out_x = merge_branches(px[12], (a_x, b_x, c_x, d_x), w_branch, w_out)
    out_c = merge_branches(pc[12], (a_c, b_c, c_c, d_c), w_branch, w_out) if need_ctx else None
    return out_x, out_c


def peer_ffn(h, wq, k1, k2, u_tab, v_tab):
    bsz, n, dm = h.shape
    t = h.reshape(bsz * n, dm)
    nt = t.shape[0]
    q = (t @ wq).reshape(nt, PEER_HEADS, 2, PEER_QDIM // 2)
    s1 = jnp.einsum('thd,kd->thk', q[:, :, 0], k1).astype(F32)
    s2 = jnp.einsum('thd,kd->thk', q[:, :, 1], k2).astype(F32)
    v1, i1 = lax.top_k(s1, PEER_TOPK)
    v2, i2 = lax.top_k(s2, PEER_TOPK)
    n_cand = PEER_TOPK * PEER_TOPK
    cand = (v1[..., :, None] + v2[..., None, :]).reshape(nt, PEER_HEADS, n_cand)
    cand_idx = (i1[..., :, None] * PEER_NKEYS + i2[..., None, :]).reshape(nt, PEER_HEADS, n_cand)
    top_s, pos = lax.top_k(cand, PEER_TOPK)
    idx = jnp.take_along_axis(cand_idx, pos, axis=-1)
    gate = jax.nn.softmax(top_s, axis=-1).astype(h.dtype)
    nsel = PEER_HEADS * PEER_TOPK
    nblk = nt // PEER_BLOCK

    def block(args):
        tb, ib, gb = args
        act = jax.nn.gelu(jnp.einsum('td,tkd->tk', tb, u_tab[ib]), approximate=False) * gb
        return jnp.einsum('tk,tkd->td', act, v_tab[ib])

    out = lax.map(block, (t.reshape(nblk, PEER_BLOCK, dm), idx.reshape(nblk, PEER_BLOCK, nsel),
                          gate.reshape(nblk, PEER_BLOCK, nsel)))
    return out.reshape(bsz, n, dm)


def setup_inputs(seed: int = 0) -> dict:
    key = jax.random.key(seed)
    ks = jax.random.split(key, 40)
    L = DEPTH
    D = D_MODEL

    def nrm(k, shape, std):
        return jax.random.normal(k, shape, F32) * std

    dt0 = jnp.exp(jax.random.uniform(ks[12], (L, 2, SSD_HEADS), F32, math.log(1e-3), math.log(1e-1)))
    return {
        'x': nrm(ks[0], (BATCH, SEQ, D), 1.0),
        'c': nrm(ks[1], (BATCH, D), 1.0),
        'ctx': nrm(ks[2], (BATCH, CTX_LEN, D), 1.0),
        'c_ctx': nrm(ks[3], (D,), 1.0),
        'w_ada': nrm(ks[4], (L, D, 6 * D), 0.5 * D ** -0.5),
        'b_ada': nrm(ks[5], (L, 6 * D), 0.02),
        'g_norm1': 1.0 + nrm(ks[6], (L, D), 0.02),
        'g_norm2': 1.0 + nrm(ks[7], (L, D), 0.02),
        'w_in': nrm(ks[8], (L, D, IN_WIDTH), D ** -0.5),
        'ssd_conv_w': nrm(ks[9], (L, SSD_CONV_K, SSD_CONV_DIM), SSD_CONV_K ** -0.5),
        'ssd_conv_b': nrm(ks[10], (L, SSD_CONV_DIM), 0.02),
        'ssd_a_log': jnp.log(jax.random.uniform(ks[11], (L, 2, SSD_HEADS), F32, 1.0, 16.0)),
        'ssd_dt_bias': dt0 + jnp.log(-jnp.expm1(-dt0)),
        'ssd_d': 1.0 + nrm(ks[13], (L, SSD_HEADS), 0.1),
        'ssd_norm_g': 1.0 + nrm(ks[14], (L, SSD_D_INNER), 0.02),
        'na_q_norm': 1.0 + nrm(ks[15], (L, NA_HEAD_DIM), 0.02),
        'na_k_norm': 1.0 + nrm(ks[16], (L, NA_HEAD_DIM), 0.02),
        'na_rpb': nrm(ks[17], (L, NA_HEADS, 2 * NA_WIN_R - 1, 2 * NA_WIN_C - 1), 0.1),
        'gla_w_gate': nrm(ks[18], (L, 2, GLA_GATE_RANK, GLA_K_DIM), GLA_GATE_RANK ** -0.5),
        'gla_b_gate': nrm(ks[19], (L, 2, GLA_K_DIM), 0.1),
        'gla_norm_g': 1.0 + nrm(ks[20], (L, GLA_HEAD_V), 0.02),
        'swa_q_norm': 1.0 + nrm(ks[21], (L, SWA_HEAD_DIM), 0.02),
        'swa_k_norm': 1.0 + nrm(ks[22], (L, SWA_HEAD_DIM), 0.02),
        'swa_sink': nrm(ks[23], (L, SWA_HEADS), 0.5),
        'w_branch': nrm(ks[24], (L, N_BRANCH, BRANCH_WIDTH, D), BRANCH_WIDTH ** -0.5),
        'w_out': nrm(ks[25], (L, D, D), D ** -0.5),
        'peer_wq': nrm(ks[26], (L, D, PEER_HEADS * PEER_QDIM), D ** -0.5),
        'peer_k1': nrm(ks[27], (L, PEER_NKEYS, PEER_QDIM // 2), (PEER_QDIM // 2) ** -0.5),
        'peer_k2': nrm(ks[28], (L, PEER_NKEYS, PEER_QDIM // 2), (PEER_QDIM // 2) ** -0.5),
        'peer_u': nrm(ks[29], (L, PEER_EXPERTS, D), D ** -0.5),
        'peer_v': nrm(ks[30], (L, PEER_EXPERTS, D), 0.5),
    }


def reference(x, c, ctx, c_ctx, w_ada, b_ada, g_norm1, g_norm2, w_in, ssd_conv_w, ssd_conv_b,
              ssd_a_log, ssd_dt_bias, ssd_d, ssd_norm_g, na_q_norm, na_k_norm, na_rpb,
              gla_w_gate, gla_b_gate, gla_norm_g, swa_q_norm, swa_k_norm, swa_sink,
              w_branch, w_out, peer_wq, peer_k1, peer_k2, peer_u, peer_v):
    rope = axial_rope_tables(x.shape[1])
    for l in range(DEPTH):
        need_ctx = l < DEPTH - 1
        mod_x = (jax.nn.silu(c) @ w_ada[l] + b_ada[l])[:, None, :]
        mod_c = (jax.nn.silu(c_ctx) @ w_ada[l] + b_ada[l])[None, None, :]
        sh1, sc1, gt1, sh2, sc2, gt2 = jnp.split(mod_x, 6, axis=-1)
        csh1, csc1, cgt1, csh2, csc2, cgt2 = jnp.split(mod_c, 6, axis=-1)
        hx = modulate(rms_norm(x, g_norm1[l]), sh1, sc1)
        hc = modulate(rms_norm(ctx, g_norm1[l]), csh1, csc1)
        mx, mc = mixer_sublayer(hx, hc, w_in[l], ssd_conv_w[l], ssd_conv_b[l], ssd_a_log[l],
                                ssd_dt_bias[l], ssd_d[l], ssd_norm_g[l], na_q_norm[l], na_k_norm[l],
                                na_rpb[l], gla_w_gate[l], gla_b_gate[l], gla_norm_g[l],
                                swa_q_norm[l], swa_k_norm[l], swa_sink[l], w_branch[l], w_out[l],
                                rope, need_ctx)
        x = x + gt1 * mx
        hx2 = modulate(rms_norm(x, g_norm2[l]), sh2, sc2)
        x = x + gt2 * peer_ffn(hx2, peer_wq[l], peer_k1[l], peer_k2[l], peer_u[l], peer_v[l])
        if need_ctx:
            ctx = ctx + cgt1 * mc
            hc2 = modulate(rms_norm(ctx, g_norm2[l]), csh2, csc2)
            ctx = ctx + cgt2 * peer_ffn(hc2, peer_wq[l], peer_k1[l], peer_k2[l], peer_u[l], peer_v[l])
    return x
```

```python
import numpy as np
from contextlib import ExitStack
import concourse.bass as bass
import concourse.mybir as mybir
from concourse.bass_utils import run_bass_kernel_spmd

F32 = mybir.dt.float32
BF16 = mybir.dt.bfloat16
U32 = mybir.dt.uint32
AF = mybir.ActivationFunctionType
ALU = mybir.AluOpType
AX = mybir.AxisListType

D = 2048
NKC = 16
IN_W = 19008
O_Z, O_XBC, O_DT, O_NA = 0, 1024, 3072, 3104
O_GQ, O_GK, O_GV, O_GR, O_GLR = 6176, 6688, 7200, 8224, 9248
O_SQ, O_SK, O_SV, O_GATE = 9280, 10304, 10560, 10816
EPS = 1e-6
NEG = -30000.0


class Buf:
    __slots__ = ("name", "w", "r")

    def __init__(self, name):
        self.name = name
        self.w = None
        self.r = {}


class KB:
    NDMA = 12
    LIMIT = 24000

    def __init__(self, nc, es, dma_queues=("sp",)):
        self.nc = nc
        self.es = es
        self.eng = {"pe": nc.tensor, "act": nc.scalar, "dve": nc.vector, "pool": nc.gpsimd, "sp": nc.sync}
        self.sem = {}
        self.cnt = {}
        self.cur = {}
        self.pending = []
        self.nsem = 0
        for k in self.eng:
            self._fresh(k)
        self.dq = {}
        for q in dma_queues:
            sl = []
            for i in range(self.NDMA):
                sl.append(self._fresh(("dma", q, i)))
            self.dq[q] = [sl, 0]
        self.seen = {k: {} for k in self.eng}
        self.bufs = {}
        self.nwait = 0
        self.ninst = 0
        self.tot = {k: 0 for k in self.eng}
        self.store_q = ("act", "pool")

    def _fresh(self, base):
        if base in self.cur:
            self.pending.append(self.cur[base])
        self.nsem += 1
        key = (base, self.nsem)
        self.sem[key] = self.es.enter_context(self.nc.semaphore("s%d" % self.nsem))
        self.cnt[key] = 0
        self.cur[base] = key
        return key

    def buf(self, name):
        b = self.bufs.get(name)
        if b is None:
            b = Buf(name)
            self.bufs[name] = b
        return b

    def _wait(self, e, key, val):
        if key[0] == e and e in ("pe", "sp"):
            return
        s = self.seen[e]
        if s.get(key, 0) >= val:
            return
        self.eng[e].wait_ge(self.sem[key], val)
        s[key] = val
        self.nwait += 1

    def _deps(self, e, reads, writes):
        for b in reads:
            if b.w is not None:
                self._wait(e, b.w[0], b.w[1])
        for b in writes:
            if b.w is not None:
                self._wait(e, b.w[0], b.w[1])
            for k, v in b.r.items():
                self._wait(e, k, v)

    def _record(self, tok, reads, writes):
        for b in reads:
            if b.r.get(tok[0], 0) < tok[1]:
                b.r[tok[0]] = tok[1]
        for b in writes:
            b.w = tok
            b.r = {}

    def _bl(self, xs):
        return [self.buf(x) if isinstance(x, str) else x for x in xs]

    def op(self, e, fn, reads=(), writes=()):
        reads = self._bl(reads)
        writes = self._bl(writes)
        self._deps(e, reads, writes)
        key = self.cur[e]
        if self.cnt[key] >= self.LIMIT:
            key = self._fresh(e)
        inst = fn(self.eng[e])
        self.cnt[key] += 1
        self.tot[e] += 1
        inst.then_inc(self.sem[key], 1)
        self._record((key, self.cnt[key]), reads, writes)
        self.ninst += 1
        return inst

    def dma(self, out, in_, reads=(), writes=(), q=None, **kw):
        is_store = any(isinstance(x, str) and (x.startswith("dram_") or x.startswith("out_") or x in ("modd", "y")) for x in writes)
        reads = self._bl([x for x in reads if not (isinstance(x, str) and (x.startswith("dram_") or x == "modd"))])
        writes = self._bl([x for x in writes if not (isinstance(x, str) and (x.startswith("dram_") or x.startswith("out_") or x in ("modd", "y")))])
        if q is None:
            q = "sp"
            if is_store and reads and reads[0].w is not None and reads[0].w[0][0] in self.store_q:
                q = reads[0].w[0][0]
        if q not in self.dq:
            self.dq[q] = [[self._fresh(("dma", q, i)) for i in range(self.NDMA)], 0]
        sl, i = self.dq[q]
        slot = i % self.NDMA
        key = sl[slot]
        self.dq[q][1] = i + 1
        if self.cnt[key] > 0:
            self._wait(q, key, self.cnt[key])
        if self.cnt[key] >= self.LIMIT:
            key = self._fresh(("dma", q, slot))
            sl[slot] = key
        self._deps(q, reads, writes)
        inst = self.eng[q].dma_start(out=out, in_=in_, **kw)
        self.cnt[key] += 16
        inst.then_inc(self.sem[key], 16)
        self._record((key, self.cnt[key]), reads, writes)
        self.ninst += 1
        return inst

    def barrier(self):
        keys = list(self.pending) + list(self.cur.values())
        self.pending = []
        for e in self.eng:
            for key in keys:
                if self.cnt[key] > 0:
                    self._wait(e, key, self.cnt[key])
        live = set(self.cur.values())
        for b in self.bufs.values():
            if b.w is not None and b.w[0] not in live:
                b.w = None
            b.r = {k: v for k, v in b.r.items() if k in live}


class Rot:
    def __init__(self, st, alloc, name, n, shape, dt):
        self.items = [(st.enter_context(alloc(name + str(i), shape, dt)), name + str(i)) for i in range(n)]
        self.i = 0

    def next(self):
        it = self.items[self.i % len(self.items)]
        self.i += 1
        return it


class Cfg:
    def __init__(self, TL=4096, TC=256, L=2, stages=None, dbg=(), dumps=False, dbg_layer=0, ntg=4, dve_heads=2):
        self.ntg = ntg
        self.dve_heads = dve_heads
        self.dbg_layer = dbg_layer
        self.dumps = dumps
        self.TL, self.TC, self.L = TL, TC, L
        self.T = TL + TC
        self.stages = stages
        self.dbg = tuple(dbg)

    def blocks(self, t0=0, t1=None):
        t1 = self.T if t1 is None else t1
        out = []
        if t0 < self.TC:
            out.append((0, self.TC))
            t0 = self.TC
        t = t0
        while t < t1:
            out.append((t, min(512, t1 - t)))
            t += 512
        return out


def proj_groups():
    g = []

    def fm(a, b):
        c = a
        while c < b:
            w = min(512, b - c)
            g.append((c, w, "f"))
            c += w

    def tm(a, b):
        c = a
        while c < b:
            w = min(512, b - c)
            g.append((c, w, "t"))
            c += w

    fm(O_Z, O_DT)
    tm(O_DT, O_NA)
    fm(O_NA, O_NA + 2048)
    tm(O_NA + 2048, O_GQ)
    fm(O_GQ, O_GV)
    tm(O_GK, O_GR)
    fm(O_GR, O_SV)
    tm(O_SV, O_GATE)
    fm(O_GATE, IN_W)
    return g


def na_classes(TL):
    R_ = TL // 64
    wr = min(8, R_)
    cls, idx = [], []
    for ti in range(TL // 128):
        ra, rb = 2 * ti, 2 * ti + 1
        r0a = min(max(ra - wr // 2, 0), R_ - wr)
        r0b = min(max(rb - wr // 2, 0), R_ - wr)
        kt_lo, kt_hi = r0a // 2, (r0b + wr - 1) // 2
        key = (r0a - 2 * ti, r0b - 2 * ti, kt_lo - ti, kt_hi - kt_lo + 1)
        if key not in cls:
            cls.append(key)
        idx.append(cls.index(key))
    return cls, idx


def na_bias_tables(TL, rpb):
    cls, _ = na_classes(TL)
    out = np.full((len(cls), 128, 16, 5, 128), NEG, np.float32)
    j = np.arange(128)[:, None]
    i = np.arange(128)[None, :]
    cq = i % 64
    ck = j % 64
    cstart = np.clip(cq - 8, 0, 48)
    colok = (ck >= cstart) & (ck < cstart + 16)
    crel = ck - cq + 15
    for ci, (off_a, off_b, klo_rel, nsl) in enumerate(cls):
        rq = i // 64
        r0 = np.where(rq == 0, off_a, off_b + 0)
        for sl in range(nsl):
            rk = 2 * (klo_rel + sl) + j // 64
            rowok = (rk >= r0) & (rk < r0 + 8)
            rrel = rk - rq + 7
            ok = rowok & colok & (rrel >= 0) & (rrel < 15)
            rr = np.clip(rrel, 0, 14)
            cc = np.clip(crel, 0, 30)
            for h in range(16):
                out[ci, :, h, sl, :] = np.where(ok, rpb[h][rr, cc], NEG)
    return out.reshape(len(cls), 128, 16 * 5 * 128)


def rope_tables(TL):
    t = np.arange(TL)
    row = (t // 64).astype(np.float32)
    col = (t % 64).astype(np.float32)
    nf = 16
    inv = (10000.0 ** (-np.arange(nf, dtype=np.float32) / nf)).astype(np.float32)
    ar = row[None, :] * inv[:, None]
    ac = col[None, :] * inv[:, None]
    cos = np.concatenate([np.cos(ar), np.cos(ar), np.cos(ac), np.cos(ac)], 0)
    sin = np.concatenate([np.sin(ar), np.sin(ar), np.sin(ac), np.sin(ac)], 0)
    tab = np.stack([np.concatenate([cos, cos], 0), np.concatenate([sin, sin], 0)], 0)
    return tab.astype(np.float32)


def rot_matrix():
    pm = np.zeros((128, 128), np.float32)
    for base in (0, 64):
        for a in (0, 32):
            for i in range(16):
                pm[base + a + 16 + i, base + a + i] = -1.0
                pm[base + a + i, base + a + 16 + i] = 1.0
    return pm


def build(cfg):
    TL, TC, T, L = cfg.TL, cfg.TC, cfg.T, cfg.L
    NT = T // 128
    nc = bass.Bass("TRN2", target_bir_lowering=False)

    def din(name, shape, dt=F32):
        return nc.dram_tensor(name, list(shape), dt, kind="ExternalInput").ap()

    def dscr(name, shape, dt=F32):
        return nc.dram_tensor(name, list(shape), dt, kind="Internal").ap()

    def dout(name, shape, dt=F32):
        return nc.dram_tensor(name, list(shape), dt, kind="ExternalOutput").ap()

    I = {}
    I["x"] = din("x", [TL, D])
    I["ctx"] = din("ctx", [TC, D])
    I["cc"] = din("cc", [128, 32])
    I["w_ada"] = din("w_ada", [L, D, 6 * D])
    I["bada"] = din("bada", [L, 128, 96])
    I["gn"] = din("gn", [L, 128, 32])
    I["w_in"] = din("w_in", [L, D, IN_W])
    I["ssd_cw"] = din("ssd_cw", [L, 128, 16, 5])
    I["ssd_cb"] = din("ssd_cb", [L, 128, 16])
    I["ssd_row"] = din("ssd_row", [L, 1, 64])
    I["ssd_dg"] = din("ssd_dg", [L, 128, 16])
    I["gla_wg"] = din("gla_wg", [L, 2, 16, 512])
    I["gla_bg"] = din("gla_bg", [L, 2, 1, 512])
    I["gla_ng"] = din("gla_ng", [L, 128, 2])
    I["att_g"] = din("att_g", [L, 128, 4])
    I["esink"] = din("esink", [L, 64, 16])
    I["rope"] = din("rope", [2, 128, TL])
    I["pm"] = din("pm", [128, 128])
    NCLS = len(na_classes(TL)[0])
    I["na_bias"] = din("na_bias", [L, NCLS, 128, 16 * 5 * 128])
    I["w_branch"] = din("w_branch", [L, 4096, D])
    I["w_out"] = din("w_out", [L, D, D])
    I["peer_wq"] = din("peer_wq", [L, D, D])
    I["peer_kk"] = din("peer_kk", [L, 128, 2, 128])
    I["peer_u"] = din("peer_u", [L, 16384, D])
    I["peer_v"] = din("peer_v", [L, 16384, D])
    y = dout("y", [TL, D])
    dbg_out = {}

    xs = [dscr("xs%d" % i, [T, D]) for i in range(3)]
    hT = dscr("hT", [D, T], BF16)
    PTa = dscr("PTa", [O_GATE, T])
    PTb = dscr("PTb", [IN_W - O_GATE, T])
    PK = dscr("PK", [T, O_GATE])

    class _PT:
        def __getitem__(self, key):
            rs, cs_ = key
            if rs.stop <= O_GATE:
                return PTa[rs, cs_]
            assert rs.start >= O_GATE
            return PTb[rs.start - O_GATE:rs.stop - O_GATE, cs_]
    PT = _PT()
    modd = dscr("modd", [2, 96 * 128])
    xcT = dscr("xcT", [2048, T], BF16)
    xtok = dscr("xtok", [T, 1536], BF16)
    yfT = dscr("yfT", [1024, T])
    aT = dscr("aT", [1024, T], BF16)
    nqk = dscr("nqk", [2048, T], BF16)
    sqk = dscr("sqk", [1280, T], BF16)
    vtok = dscr("vtok", [T, 1280], BF16)
    bT = dscr("bT", [1024, T], BF16)
    dT = dscr("dT", [1024, T], BF16)
    wb_br = dscr("wb_br", [4096, D], BF16)
    wb_out = dscr("wb_out", [D, D], BF16)
    wb_q = dscr("wb_q", [D, D], BF16)
    mTd = dscr("mTd", [D, T], BF16)
    qTd = dscr("qTd", [D, T])
    sc_d = dscr("sc_d", [T, 2048])
    thr_d = dscr("thr_d", [T, 16])
    UTd = dscr("UTd", [32, 128, 16 * 512], BF16)
    Vd = dscr("Vd", [16384, D], BF16)
    ofT = dscr("ofT", [1024, T])
    cT = dscr("cT", [1024, T], BF16)
    wb_in = dscr("wb_in", [D, IN_W], BF16)

    with ExitStack() as es:
        kb = KB(nc, es)
        uid = [0]

        def sb(name, shape, dt):
            uid[0] += 1
            return nc.sbuf_tensor("%s_u%d" % (name, uid[0]), shape, dt)

        def ps(name, shape, dt):
            uid[0] += 1
            return nc.psum_tensor("%s_u%d" % (name, uid[0]), shape, dt)

        ident_b = es.enter_context(sb("ident_b", [128, 128], BF16))
        ident_f = es.enter_context(sb("ident_f", [128, 128], F32))
        modt = es.enter_context(sb("modt", [128, 2, 96], F32))
        tabs = es.enter_context(sb("tabs", [128, 2, 6, 16], F32))
        gnt = es.enter_context(sb("gnt", [128, 32], F32))
        kb.op("pool", lambda e: e.memset(ident_f[:], 0.0), writes=["ident_f"])
        kb.op("pool", lambda e: e.affine_select(out=ident_f[:], in_=ident_f[:], pattern=[[-1, 128]],
                                                compare_op=ALU.not_equal, fill=1.0, base=0, channel_multiplier=1),
              reads=["ident_f"], writes=["ident_f"])
        kb.op("dve", lambda e: e.tensor_copy(out=ident_b[:], in_=ident_f[:]), reads=["ident_f"], writes=["ident_b"])

        ones_f = es.enter_context(sb("ones_f", [128, 128], F32))
        ones_b = es.enter_context(sb("ones_b", [128, 128], BF16))
        tri = [es.enter_context(sb("tri%d" % d, [128, 128], F32)) for d in range(2)]
        negm = [es.enter_context(sb("negm%d" % d, [128, 128], F32)) for d in range(2)]
        kcon = es.enter_context(sb("kcon", [128, 4], F32))
        kb.op("pool", lambda e: e.memset(kcon[:, 0:1], 1.0), writes=["kcon"])
        kb.op("pool", lambda e: e.memset(kcon[:, 1:2], EPS), writes=["kcon"])
        kb.op("pool", lambda e: e.memset(kcon[:, 2:4], 0.0), writes=["kcon"])
        kb.op("pool", lambda e: e.memset(kcon[:, 3:4], 64.0 * EPS), writes=["kcon"])
        blk64 = es.enter_context(sb("blk64", [128, 128], BF16))
        negm_b = [es.enter_context(sb("negm_b%d" % d, [128, 128], BF16)) for d in range(2)]
        kb.op("pool", lambda e: e.memset(blk64[:], 0.0), writes=["blk64"])
        kb.op("pool", lambda e: e.memset(blk64[0:64, 0:64], 1.0), writes=["blk64"])
        kb.op("pool", lambda e: e.memset(blk64[64:128, 64:128], 1.0), writes=["blk64"])
        C_ONE = kcon[:, 0:1]
        C_EPS = kcon[:, 1:2]
        kb.op("pool", lambda e: e.memset(ones_f[:], 1.0), writes=["ones_f"])
        kb.op("pool", lambda e: e.memset(ones_b[:], 1.0), writes=["ones_b"])
        for d in range(2):
            sgn = 1 if d == 0 else -1
            kb.op("pool", lambda e: e.affine_select(out=tri[d][:], in_=ones_f[:], pattern=[[sgn, 128]], compare_op=ALU.is_ge,
                                                    fill=0.0, base=0, channel_multiplier=-sgn), reads=["ones_f"], writes=["tri"])
            kb.op("pool", lambda e: e.memset(negm[d][:], 0.0), writes=["negm"])
            kb.op("pool", lambda e: e.affine_select(out=negm[d][:], in_=negm[d][:], pattern=[[sgn, 128]], compare_op=ALU.is_ge,
                                                    fill=NEG, base=0, channel_multiplier=-sgn), reads=["negm"], writes=["negm"])

        def dump(name, ap, reads):
            if name in dbg_out or not cfg.dumps:
                return
            shp = [ap.shape[0], int(np.prod(ap.shape[1:]))]
            o = dout("dbg_" + name, shp, ap.dtype)
            dbg_out[name] = o
            src = ap
            if len(ap.shape) == 3:
                src = ap.rearrange("p a b -> p (a b)")
            kb.dma(o[:, :], src, reads=reads, writes=["out_" + name])

        for d in range(2):
            kb.op("dve", lambda e: e.tensor_copy(out=negm_b[d][:], in_=negm[d][:]), reads=["negm"], writes=["negm_b"])

        def run(stage):
            return cfg.stages is None or stage in cfg.stages

        def conv_bf16(src, dst, R, C, tag):
            with ExitStack() as st:
                fin = Rot(st, sb, "cv_in_" + tag, 2, [128, 4096], F32)
                fo = Rot(st, sb, "cv_out_" + tag, 2, [128, 4096], BF16)
                k = 0
                for r in range(R // 128):
                    c = 0
                    while c < C:
                        w = min(4096, C - c)
                        ti, tn = fin.next()
                        to, on = fo.next()
                        kb.dma(ti[:, 0:w], src[r * 128:(r + 1) * 128, c:c + w], writes=[tn])
                        eng = ("dve", "pool", "act")[k % 3]
                        if eng == "act":
                            kb.op("act", lambda e: e.copy(out=to[:, 0:w], in_=ti[:, 0:w]), reads=[tn], writes=[on])
                        else:
                            kb.op(eng, lambda e: e.tensor_copy(out=to[:, 0:w], in_=ti[:, 0:w]), reads=[tn], writes=[on])
                        kb.dma(dst[r * 128:(r + 1) * 128, c:c + w], to[:, 0:w], reads=[on], writes=["dram_" + tag])
                        c += w
                        k += 1
                kb.barrier()

        def stage_mod(l):
            with ExitStack() as st:
                cct = st.enter_context(sb("cct", [128, 32], F32))
                scs = st.enter_context(sb("scs", [128, 32], F32))
                badat = st.enter_context(sb("badat", [128, 96], F32))
                wt = Rot(st, sb, "adaw", 2, [128, 16, 128], F32)
                mp = st.enter_context(ps("modp", [128, 96, 2], F32))
                tp = st.enter_context(ps("modtp", [96, 2, 128], F32))
                mrow = st.enter_context(sb("mrow", [96, 2, 128], F32))
                kb.dma(cct[:], I["cc"][:, :], writes=["cct"])
                kb.dma(badat[:], I["bada"][l], writes=["badat"])
                kb.dma(gnt[:], I["gn"][l], writes=["gnt"])
                kb.op("act", lambda e: e.activation(out=scs[:], in_=cct[:], func=AF.Silu), reads=["cct"], writes=["scs"])
                for ch in range(96):
                    w, wn = wt.next()
                    kb.dma(w[:], I["w_ada"][l, :, ch * 128:(ch + 1) * 128].rearrange("(kc p) f -> p kc f", p=128),
                           writes=[wn])
                    for kc in range(NKC):
                        kb.op("pe", lambda e: e.matmul(mp[:, ch, :], lhsT=w[:, kc, :], rhs=scs[:, kc * 2:kc * 2 + 2],
                                                       start=(kc == 0), stop=(kc == NKC - 1)),
                              reads=[wn, "scs"], writes=["modp"])
                for j in range(2):
                    kb.op("dve", lambda e: e.tensor_tensor(out=modt[:, j, :], in0=mp[:, :, j], in1=badat[:], op=ALU.add),
                          reads=["modp", "badat"], writes=["modt"])
                for j in range(2):
                    for (dst, sc_ch, gcol) in ((0, 16, 0), (3, 64, 16)):
                        kb.op("dve", lambda e: e.scalar_tensor_tensor(out=tabs[:, j, dst, :], in0=modt[:, j, sc_ch:sc_ch + 16],
                                                                       scalar=1.0, in1=gnt[:, gcol:gcol + 16],
                                                                       op0=ALU.add, op1=ALU.mult),
                              reads=["modt", "gnt"], writes=["tabs"])
                    for (dst, ch0) in ((1, 0), (2, 32), (4, 48), (5, 80)):
                        kb.op("dve", lambda e: e.tensor_copy(out=tabs[:, j, dst, :], in_=modt[:, j, ch0:ch0 + 16]),
                              reads=["modt"], writes=["tabs"])
                for j in range(2):
                    kb.op("pe", lambda e: e.transpose(out=tp[:, j, :], in_=modt[:, j, :], identity=ident_f[:]),
                          reads=["modt", "ident_f"], writes=["modtp"])
                kb.op("act", lambda e: e.copy(out=mrow[:], in_=tp[:]), reads=["modtp"], writes=["mrow"])
                for j in range(2):
                    kb.dma(modd[j].rearrange("(ch p) -> ch p", p=128), mrow[:, j, :], reads=["mrow"], writes=["modd"])
                kb.barrier()

        def stage_norm(src, which):
            si, bi = (0, 1) if which == 0 else (3, 4)
            with ExitStack() as st:
                xin = Rot(st, sb, "n_x", 2, [128, D], F32)
                junk = st.enter_context(sb("n_junk", [128, D], F32))
                xn = Rot(st, sb, "n_xn", 2, [128, D], BF16)
                stat = Rot(st, sb, "n_stat", 2, [128, 4], F32)
                hb = Rot(st, sb, "n_hb", 2, [128, NKC, 512], BF16)
                tps = Rot(st, ps, "n_tp", 2, [128, 8, 128], BF16)
                for (b0, bn) in cfg.blocks():
                    j = 1 if b0 < TC else 0
                    hblk, hn = hb.next()
                    for ti in range(bn // 128):
                        t0 = b0 + ti * 128
                        x, xname = xin.next()
                        s, sname = stat.next()
                        xb, xbn = xn.next()
                        kb.dma(x[:], src[t0:t0 + 128, :], reads=["dram_x"], writes=[xname])
                        kb.op("act", lambda e: e.activation(out=junk[:], in_=x[:], func=AF.Square),
                              reads=[xname], writes=["n_junk"])
                        kb.op("dve", lambda e: e.tensor_reduce(out=s[:, 0:1], in_=junk[:], axis=AX.X, op=ALU.add),
                              reads=["n_junk"], writes=[sname])
                        kb.op("act", lambda e: e.activation(out=s[:, 1:2], in_=s[:, 0:1], func=AF.Sqrt, scale=1.0 / D, bias=C_EPS),
                              reads=[sname, "kcon"], writes=[sname])
                        kb.op("dve", lambda e: e.reciprocal(out=s[:, 2:3], in_=s[:, 1:2]), reads=[sname], writes=[sname])
                        kb.op("dve", lambda e: e.tensor_scalar(out=xb[:], in0=x[:], scalar1=s[:, 2:3], scalar2=None, op0=ALU.mult),
                              reads=[xname, sname], writes=[xbn])
                        for half in range(2):
                            tp, tpn = tps.next()
                            for q in range(8):
                                kc = half * 8 + q
                                kb.op("pe", lambda e: e.transpose(out=tp[:, q, :], in_=xb[:, kc * 128:(kc + 1) * 128], identity=ident_b[:]),
                                      reads=[xbn, "ident_b"], writes=[tpn])
                            for q in range(8):
                                kc = half * 8 + q
                                if q % 2 == 0:
                                    kb.op("act", lambda e: e.activation(out=hblk[:, kc, ti * 128:(ti + 1) * 128], in_=tp[:, q, :],
                                                                        func=AF.Identity, scale=tabs[:, j, si, kc:kc + 1],
                                                                        bias=tabs[:, j, bi, kc:kc + 1]),
                                          reads=[tpn, "tabs"], writes=[hn])
                                else:
                                    kb.op("dve", lambda e: e.tensor_scalar(out=hblk[:, kc, ti * 128:(ti + 1) * 128], in0=tp[:, q, :],
                                                                           scalar1=tabs[:, j, si, kc:kc + 1], scalar2=tabs[:, j, bi, kc:kc + 1],
                                                                           op0=ALU.mult, op1=ALU.add),
                                          reads=[tpn, "tabs"], writes=[hn])
                    kb.dma(hT[:, b0:b0 + bn].rearrange("(kc p) t -> p kc t", p=128), hblk[:, :, 0:bn], reads=[hn], writes=["dram_hT"])
                kb.barrier()

        def stage_proj():
            groups = proj_groups()
            with ExitStack() as st:
                hb = Rot(st, sb, "p_hb", 2, [128, NKC, 512], BF16)
                wt = Rot(st, sb, "p_w", 3, [128, NKC, 512], BF16)
                pp = Rot(st, ps, "p_ps", 4, [128, 512], F32)
                og = Rot(st, sb, "p_o", 4, [128, 512], F32)
                k = 0
                for (b0, bn) in cfg.blocks():
                    hblk, hn = hb.next()
                    kb.dma(hblk[:, :, 0:bn], hT[:, b0:b0 + bn].rearrange("(kc p) t -> p kc t", p=128), reads=["dram_hT"], writes=[hn])
                    for (c0, cw, mode) in groups:
                        w, wn = wt.next()
                        kb.dma(w[:, :, 0:cw], wb_in[:, c0:c0 + cw].rearrange("(kc p) c -> p kc c", p=128), reads=["dram_wb_in"], writes=[wn])
                        if mode == "f":
                            c = 0
                            while c < cw:
                                m = min(128, cw - c)
                                p, pn = pp.next()
                                o, on = og.next()
                                for kc in range(NKC):
                                    kb.op("pe", lambda e: e.matmul(p[0:m, 0:bn], lhsT=w[:, kc, c:c + m], rhs=hblk[:, kc, 0:bn],
                                                                   start=(kc == 0), stop=(kc == NKC - 1)),
                                          reads=[wn, hn], writes=[pn])
                                if k % 2 == 0:
                                    kb.op("act", lambda e: e.copy(out=o[0:m, 0:bn], in_=p[0:m, 0:bn]), reads=[pn], writes=[on])
                                else:
                                    kb.op("dve", lambda e: e.tensor_copy(out=o[0:m, 0:bn], in_=p[0:m, 0:bn]), reads=[pn], writes=[on])
                                k += 1
                                kb.dma(PT[c0 + c:c0 + c + m, b0:b0 + bn], o[0:m, 0:bn], reads=[on], writes=["dram_PT"])
                                c += m
                        else:
                            for ti in range(bn // 128):
                                p, pn = pp.next()
                                o, on = og.next()
                                for kc in range(NKC):
                                    kb.op("pe", lambda e: e.matmul(p[:, 0:cw], lhsT=hblk[:, kc, ti * 128:(ti + 1) * 128], rhs=w[:, kc, 0:cw],
                                                                   start=(kc == 0), stop=(kc == NKC - 1)),
                                          reads=[wn, hn], writes=[pn])
                                if k % 2 == 0:
                                    kb.op("act", lambda e: e.copy(out=o[:, 0:cw], in_=p[:, 0:cw]), reads=[pn], writes=[on])
                                else:
                                    kb.op("dve", lambda e: e.tensor_copy(out=o[:, 0:cw], in_=p[:, 0:cw]), reads=[pn], writes=[on])
                                k += 1
                                kb.dma(PK[b0 + ti * 128:b0 + (ti + 1) * 128, c0:c0 + cw], o[:, 0:cw], reads=[on], writes=["dram_PK"])
                kb.barrier()

        def seg_chunks():
            return [(0, TC)], [(TC, T)]

        def stage_ssd_conv(l):
            with ExitStack() as st:
                cw = st.enter_context(sb("s_cw", [128, 16, 5], F32))
                cb = st.enter_context(sb("s_cb", [128, 16], F32))
                xin = Rot(st, sb, "s_xin", 3, [128, 516], F32)
                acc = Rot(st, sb, "s_acc", 2, [128, 512], F32)
                ob = Rot(st, sb, "s_ob", 3, [128, 512], BF16)
                kb.dma(cw[:], I["ssd_cw"][l], writes=["s_cw"])
                kb.dma(cb[:], I["ssd_cb"][l], writes=["s_cb"])
                for (b0, bn) in cfg.blocks():
                    s0, s1 = (0, TC) if b0 < TC else (TC, T)
                    for ch in range(16):
                        x, xn_ = xin.next()
                        a, an = acc.next()
                        o, on = ob.next()
                        lo = max(s0, b0 - 2)
                        hi = min(s1, b0 + bn + 2)
                        if lo > b0 - 2:
                            kb.op("pool", lambda e: e.memset(x[:, 0:2], 0.0), writes=[xn_])
                        if hi < b0 + bn + 2:
                            kb.op("pool", lambda e: e.memset(x[:, bn + 2:bn + 4], 0.0), writes=[xn_])
                        kb.dma(x[:, lo - (b0 - 2):hi - (b0 - 2)], PT[O_XBC + ch * 128:O_XBC + (ch + 1) * 128, lo:hi],
                               reads=["dram_PT"], writes=[xn_])
                        kb.op("dve", lambda e: e.tensor_scalar(out=a[:, 0:bn], in0=x[:, 0:bn], scalar1=cw[:, ch, 0:1], scalar2=None, op0=ALU.mult),
                              reads=[xn_, "s_cw"], writes=[an])
                        for k in range(1, 5):
                            kb.op("dve", lambda e: e.scalar_tensor_tensor(out=a[:, 0:bn], in0=x[:, k:k + bn], scalar=cw[:, ch, k:k + 1],
                                                                           in1=a[:, 0:bn], op0=ALU.mult, op1=ALU.add),
                                  reads=[xn_, "s_cw", an], writes=[an])
                        kb.op("act", lambda e: e.activation(out=o[:, 0:bn], in_=a[:, 0:bn], func=AF.Silu, bias=cb[:, ch:ch + 1], scale=1.0),
                              reads=[an, "s_cb"], writes=[on])
                        kb.dma(xcT[ch * 128:(ch + 1) * 128, b0:b0 + bn], o[:, 0:bn], reads=[on], writes=["dram_xcT"])
                kb.barrier()

        def stage_ssd_tok():
            with ExitStack() as st:
                xi = Rot(st, sb, "st_in", 2, [128, 12, 128], BF16)
                tp = Rot(st, ps, "st_tp", 2, [128, 12, 128], BF16)
                xo = Rot(st, sb, "st_o", 2, [128, 12, 128], BF16)
                for ti in range(NT):
                    t0 = ti * 128
                    a, an = xi.next()
                    p, pn = tp.next()
                    o, on = xo.next()
                    kb.dma(a[:], xcT[0:1536, t0:t0 + 128].rearrange("(c p) t -> p c t", p=128), reads=["dram_xcT"], writes=[an])
                    for c in range(12):
                        kb.op("pe", lambda e: e.transpose(out=p[:, c, :], in_=a[:, c, :], identity=ident_b[:]), reads=[an, "ident_b"], writes=[pn])
                    if ti % 2 == 0:
                        kb.op("act", lambda e: e.copy(out=o[:], in_=p[:]), reads=[pn], writes=[on])
                    else:
                        kb.op("dve", lambda e: e.tensor_copy(out=o[:], in_=p[:]), reads=[pn], writes=[on])
                    kb.dma(xtok[t0:t0 + 128, :], o[:].rearrange("p c t -> p (c t)"), reads=[on], writes=["dram_xtok"])
                kb.barrier()

        def stage_ssd_scan(l, d):
            nct, ncl = TC // 128, TL // 128
            if d == 0:
                order = list(range(nct)) + [nct + i for i in range(ncl)]
            else:
                order = list(range(nct - 1, -1, -1)) + [nct + i for i in range(ncl - 1, -1, -1)]
            with ExitStack() as st:
                rowb = st.enter_context(sb("sc_rowb", [128, 64], F32))
                abc = st.enter_context(sb("sc_abc", [128, 16], F32))
                dg = st.enter_context(sb("sc_dg", [128, 16], F32))
                S = st.enter_context(sb("sc_S", [128, 16, 64], F32))
                SbP = st.enter_context(sb("sc_SbP", [128, 16, 128], BF16))
                xdtP = Rot(st, sb, "sc_xdtP", 2, [128, 16, 128], BF16)
                xdtd = Rot(st, sb, "sc_xdtd", 2, [128, 16, 64], BF16)
                xt = Rot(st, sb, "sc_xt", 2, [128, 1536], BF16)
                bct = Rot(st, sb, "sc_bct", 2, [128, 8, 128], BF16)
                dtr = Rot(st, sb, "sc_dtr", 2, [128, 16], F32)
                sm = Rot(st, sb, "sc_sm", 2, [128, 8, 16], F32)
                adrep = Rot(st, sb, "sc_adrep", 2, [128, 16, 128], F32)
                cbs = Rot(st, sb, "sc_cbs", 2, [128, 128], F32)
                arg = Rot(st, sb, "sc_arg", 2, [128, 4, 128], F32)
                dm = Rot(st, sb, "sc_dm", 2, [128, 4, 128], F32)
                mt = Rot(st, sb, "sc_mt", 2, [128, 4, 128], BF16)
                ecs = Rot(st, sb, "sc_ecs", 2, [128, 4, 128], F32)
                cst = Rot(st, sb, "sc_cst", 2, [128, 4, 128], BF16)
                yo = Rot(st, sb, "sc_yo", 2, [128, 8, 128], F32)
                p_misc = st.enter_context(ps("sc_pmisc", [128, 512], F32))
                p_small = p_misc[:, 0:32]
                p_cb = p_misc[:, 128:256]
                p_n = p_misc[:, 256:384]
                p_csb = Rot(st, ps, "sc_pcsb", 2, [128, 4, 128], F32)
                p_y = st.enter_context(ps("sc_py", [128, 8, 128], F32))
                p_s = st.enter_context(ps("sc_pS", [128, 16, 64], F32))
                if d == 1:
                    yf = Rot(st, sb, "sc_yf", 2, [128, 8, 128], F32)
                    zt = Rot(st, sb, "sc_zt", 2, [128, 8, 128], F32)
                    xsT = Rot(st, sb, "sc_xsT", 2, [128, 8, 128], BF16)
                    sq = st.enter_context(sb("sc_sq", [128, 8, 128], BF16))
                    rs = st.enter_context(sb("sc_rs", [128, 128], F32))
                    ao = Rot(st, sb, "sc_ao", 2, [128, 8, 128], BF16)
                kb.dma(rowb[:], I["ssd_row"][l].to_broadcast([128, 64]), writes=["sc_rowb"])
                kb.dma(dg[:], I["ssd_dg"][l], writes=["sc_dg"])
                kb.op("act", lambda e: e.activation(out=abc[:], in_=rowb[:, d * 16:(d + 1) * 16], func=AF.Exp), reads=["sc_rowb"], writes=["sc_abc"])
                kb.op("dve", lambda e: e.tensor_scalar(out=abc[:], in0=abc[:], scalar1=-1.0, scalar2=None, op0=ALU.mult), reads=["sc_abc"], writes=["sc_abc"])
                kb.op("pool", lambda e: e.memset(S[:], 0.0), writes=["sc_S"])
                kb.op("pool", lambda e: e.memset(SbP[:], 0.0), writes=["sc_SbP"])
                for it in xdtP.items:
                    kb.op("pool", lambda e: e.memset(it[0][:], 0.0), writes=[it[1]])
                for ci in order:
                    t0 = ci * 128
                    x, xn_ = xt.next()
                    bc, bcn = bct.next()
                    dr, drn = dtr.next()
                    m, mn = sm.next()
                    xp, xpn = xdtP.next()
                    xd, xdn = xdtd.next()
                    ar, arn = adrep.next()
                    kb.dma(x[:], xtok[t0:t0 + 128, :], reads=["dram_xtok"], writes=[xn_])
                    kb.dma(bc[:], xcT[1024:2048, t0:t0 + 128].rearrange("(c p) t -> p c t", p=128), reads=["dram_xcT"], writes=[bcn])
                    kb.dma(dr[:], PK[t0:t0 + 128, O_DT + d * 16:O_DT + (d + 1) * 16], reads=["dram_PK"], writes=[drn])
                    kb.op("dve", lambda e: e.tensor_tensor(out=m[:, 0, :], in0=dr[:], in1=rowb[:, 32 + d * 16:32 + (d + 1) * 16], op=ALU.add),
                          reads=[drn, "sc_rowb"], writes=[mn])
                    kb.op("act", lambda e: e.activation(out=m[:, 0, :], in_=m[:, 0, :], func=AF.Exp), reads=[mn], writes=[mn])
                    kb.op("act", lambda e: e.activation(out=m[:, 0, :], in_=m[:, 0, :], func=AF.Ln, bias=C_ONE, scale=1.0), reads=[mn, "kcon"], writes=[mn])
                    kb.op("dve", lambda e: e.tensor_tensor(out=m[:, 1, :], in0=m[:, 0, :], in1=abc[:], op=ALU.mult), reads=[mn, "sc_abc"], writes=[mn])
                    kb.op("pe", lambda e: e.matmul(p_misc[:, 0:16], lhsT=tri[d][:], rhs=m[:, 1, :], start=True, stop=True), reads=["tri", mn], writes=["sc_psm"])
                    kb.op("pe", lambda e: e.matmul(p_misc[:, 16:32], lhsT=ones_f[:], rhs=m[:, 1, :], start=True, stop=True), reads=["ones_f", mn], writes=["sc_psm"])
                    kb.op("dve", lambda e: e.tensor_copy(out=m[:, 2:4, :], in_=p_misc[:, 0:32].rearrange("p (a h) -> p a h", a=2)), reads=["sc_psm"], writes=[mn])
                    kb.op("dve", lambda e: e.tensor_tensor(out=m[:, 4, :], in0=m[:, 3, :], in1=m[:, 2, :], op=ALU.subtract), reads=[mn], writes=[mn])
                    kb.op("act", lambda e: e.activation(out=m[:, 5:7, :], in_=m[:, 3:5, :], func=AF.Exp), reads=[mn], writes=[mn])
                    kb.op("dve", lambda e: e.tensor_tensor(out=m[:, 7, :], in0=m[:, 0, :], in1=m[:, 6, :], op=ALU.mult), reads=[mn], writes=[mn])
                    xv = x[:, 0:1024].rearrange("p (h q) -> p h q", q=64)
                    for par in range(2):
                        kb.op("dve", lambda e: e.tensor_tensor(out=xp[:, par::2, par * 64:(par + 1) * 64], in0=xv[:, par::2, :],
                                                               in1=m[:, 0, par::2].unsqueeze(2).to_broadcast([128, 8, 64]), op=ALU.mult),
                              reads=[xn_, mn], writes=[xpn])
                    kb.op("pool", lambda e: e.tensor_tensor(out=xd[:], in0=xv, in1=m[:, 7, :].unsqueeze(2).to_broadcast([128, 16, 64]), op=ALU.mult),
                          reads=[xn_, mn], writes=[xdn])
                    kb.op("pool", lambda e: e.tensor_copy(out=ar[:], in_=m[:, 1, :].unsqueeze(2).to_broadcast([128, 16, 128])), reads=[mn], writes=[arn])
                    for g in range(4):
                        pc, pcn = p_csb.next()
                        cb_, cbn = cbs.next()
                        a_, a_n = arg.next()
                        d_, d_n = dm.next()
                        m_, m_n = mt.next()
                        e_, e_n = ecs.next()
                        c_, c_n = cst.next()
                        kb.op("pe", lambda e: e.matmul(p_cb, lhsT=bc[:, g, :], rhs=bc[:, 4 + g, :], start=True, stop=True), reads=[bcn], writes=["sc_pcb"])
                        kb.op("act", lambda e: e.copy(out=cb_[:], in_=p_cb), reads=["sc_pcb"], writes=[cbn])
                        for hh in range(4):
                            h = g * 4 + hh
                            kb.op("pe", lambda e: e.matmul(pc[:, hh, :], lhsT=ar[:, h, :], rhs=tri[d][:], start=True, stop=True), reads=[arn, "tri"], writes=[pcn])
                        kb.op("dve", lambda e: e.tensor_tensor(out=a_[:], in0=pc[:], in1=m[:, 2, g * 4:(g + 1) * 4].unsqueeze(2).to_broadcast([128, 4, 128]),
                                                               op=ALU.subtract), reads=[pcn, mn], writes=[a_n])
                        kb.op("pool", lambda e: e.tensor_tensor(out=a_[:], in0=a_[:], in1=negm[d][:].unsqueeze(1).to_broadcast([128, 4, 128]), op=ALU.add),
                              reads=[a_n, "negm"], writes=[a_n])
                        kb.op("act", lambda e: e.activation(out=d_[:], in_=a_[:], func=AF.Exp), reads=[a_n], writes=[d_n])
                        kb.op("dve", lambda e: e.tensor_tensor(out=m_[:], in0=d_[:], in1=cb_[:].unsqueeze(1).to_broadcast([128, 4, 128]), op=ALU.mult),
                              reads=[d_n, cbn], writes=[m_n])
                        kb.op("act", lambda e: e.activation(out=e_[:], in_=pc[:], func=AF.Exp), reads=[pcn], writes=[e_n])
                        kb.op("pool", lambda e: e.tensor_tensor(out=c_[:], in0=e_[:], in1=bc[:, 4 + g, :].unsqueeze(1).to_broadcast([128, 4, 128]), op=ALU.mult),
                              reads=[e_n, bcn], writes=[c_n])
                        for hh in range(4):
                            h = g * 4 + hh
                            c2 = h // 2
                            kb.op("pe", lambda e: e.matmul(p_y[:, c2, :], lhsT=xp[:, h, :], rhs=m_[:, hh, :], start=(h % 2 == 0), stop=False),
                                  reads=[xpn, m_n], writes=["sc_py"])
                            kb.op("pe", lambda e: e.matmul(p_y[:, c2, :], lhsT=SbP[:, h, :], rhs=c_[:, hh, :], start=False, stop=(h % 2 == 1)),
                                  reads=["sc_SbP", c_n], writes=["sc_py"])
                    if d == 0:
                        dump("m", m[:], [mn]); dump("ar", ar[:, 0:2, :], [arn]); dump("arg", a_[:], [a_n]); dump("dm", d_[:], [d_n])
                        dump("mt", m_[:], [m_n]); dump("ecs", e_[:], [e_n]); dump("cst", c_[:], [c_n]); dump("cbs", cb_[:], [cbn])
                        dump("xp", xp[:, 0:4, :], [xpn]); dump("xd", xd[:, 0:4, :], [xdn]); dump("rowb", rowb[:], ["sc_rowb"])
                    for g in range(4):
                        kb.op("pe", lambda e: e.matmul(p_s[:, g * 4:(g + 1) * 4, :], lhsT=x[:, 1024 + g * 128:1024 + (g + 1) * 128],
                                                       rhs=xd[:, g * 4:(g + 1) * 4, :], start=True, stop=True), reads=[xn_, xdn], writes=["sc_pS"])
                    kb.op("dve", lambda e: e.tensor_tensor(out=S[:], in0=S[:], in1=m[:, 5, :].unsqueeze(2).to_broadcast([128, 16, 64]), op=ALU.mult),
                          reads=["sc_S", mn], writes=["sc_S"])
                    kb.op("dve", lambda e: e.tensor_tensor(out=S[:], in0=S[:], in1=p_s[:], op=ALU.add), reads=["sc_S", "sc_pS"], writes=["sc_S"])
                    for par in range(2):
                        kb.op("act", lambda e: e.copy(out=SbP[:, par::2, par * 64:(par + 1) * 64], in_=S[:, par::2, :]), reads=["sc_S"], writes=["sc_SbP"])
                    o, on = yo.next()
                    if d == 0:
                        kb.op("act", lambda e: e.copy(out=o[:], in_=p_y[:]), reads=["sc_py"], writes=[on])
                        kb.dma(yfT[:, t0:t0 + 128].rearrange("(c p) t -> p c t", p=128), o[:], reads=[on], writes=["dram_yfT"])
                    else:
                        f_, fn_ = yf.next()
                        z_, zn_ = zt.next()
                        q_, qn_ = xsT.next()
                        a2, a2n = ao.next()
                        kb.dma(f_[:], yfT[:, t0:t0 + 128].rearrange("(c p) t -> p c t", p=128), reads=["dram_yfT"], writes=[fn_])
                        kb.dma(z_[:], PT[O_Z:O_Z + 1024, t0:t0 + 128].rearrange("(c p) t -> p c t", p=128), reads=["dram_PT"], writes=[zn_])
                        kb.dma(q_[:], xcT[0:1024, t0:t0 + 128].rearrange("(c p) t -> p c t", p=128), reads=["dram_xcT"], writes=[qn_])
                        kb.op("dve", lambda e: e.tensor_tensor(out=o[:], in0=p_y[:], in1=f_[:], op=ALU.add), reads=["sc_py", fn_], writes=[on])
                        kb.op("pool", lambda e: e.tensor_tensor(out=f_[:], in0=q_[:], in1=dg[:, 0:8].unsqueeze(2).to_broadcast([128, 8, 128]), op=ALU.mult),
                              reads=[qn_, "sc_dg"], writes=[fn_])
                        kb.op("dve", lambda e: e.tensor_tensor(out=o[:], in0=o[:], in1=f_[:], op=ALU.add), reads=[on, fn_], writes=[on])
                        kb.op("act", lambda e: e.activation(out=z_[:], in_=z_[:], func=AF.Silu), reads=[zn_], writes=[zn_])
                        kb.op("dve", lambda e: e.tensor_tensor(out=o[:], in0=o[:], in1=z_[:], op=ALU.mult), reads=[on, zn_], writes=[on])
                        kb.op("act", lambda e: e.activation(out=sq[:], in_=o[:], func=AF.Square), reads=[on], writes=["sc_sq"])
                        for c2 in range(8):
                            kb.op("pe", lambda e: e.matmul(p_n, lhsT=ones_b[:], rhs=sq[:, c2, :], start=(c2 == 0), stop=(c2 == 7)),
                                  reads=["ones_b", "sc_sq"], writes=["sc_pn"])
                        kb.op("act", lambda e: e.activation(out=rs[:], in_=p_n, func=AF.Sqrt, scale=1.0 / 1024, bias=C_EPS), reads=["sc_pn", "kcon"], writes=["sc_rs"])
                        kb.op("dve", lambda e: e.reciprocal(out=rs[:], in_=rs[:]), reads=["sc_rs"], writes=["sc_rs"])
                        kb.op("dve", lambda e: e.tensor_tensor(out=o[:], in0=o[:], in1=rs[:].unsqueeze(1).to_broadcast([128, 8, 128]), op=ALU.mult),
                              reads=[on, "sc_rs"], writes=[on])
                        kb.op("pool", lambda e: e.tensor_tensor(out=a2[:], in0=o[:], in1=dg[:, 8:16].unsqueeze(2).to_broadcast([128, 8, 128]), op=ALU.mult),
                              reads=[on, "sc_dg"], writes=[a2n])
                        kb.dma(aT[:, t0:t0 + 128].rearrange("(c p) t -> p c t", p=128), a2[:], reads=[a2n], writes=["dram_aT"])
                kb.barrier()

        def stage_gla(l, d):
            ncc, ncl = TC // 64, TL // 64
            if d == 0:
                order = list(range(ncc + ncl))
            else:
                order = list(range(ncc - 1, -1, -1)) + [ncc + i for i in range(ncl - 1, -1, -1)]
            last = 63 if d == 0 else 0
            QS = 128.0 ** -0.5
            with ExitStack() as st:
                wg = st.enter_context(sb("g_wg", [16, 512], F32))
                bg = st.enter_context(sb("g_bg", [1, 512], F32))
                ngt = st.enter_context(sb("g_ng", [128, 2], F32))
                S = st.enter_context(sb("g_S", [128, 4, 256], F32))
                Sb = st.enter_context(sb("g_Sb", [128, 4, 256], BF16))
                glr = Rot(st, sb, "g_glr", 2, [16, 64], F32)
                qk = Rot(st, sb, "g_qk", 2, [128, 8, 64], F32)
                kv = Rot(st, sb, "g_kv", 2, [64, 1536], F32)
                vb = Rot(st, sb, "g_vb", 2, [64, 1024], BF16)
                gt = Rot(st, sb, "g_g", 2, [64, 512], F32)
                gcs = Rot(st, sb, "g_gcs", 2, [64, 512], F32)
                ko = Rot(st, sb, "g_ko", 2, [64, 512], BF16)
                eg = Rot(st, sb, "g_eg", 2, [128, 8, 64], F32)
                qkin = Rot(st, sb, "g_qkin", 2, [128, 8, 64], BF16)
                att = Rot(st, sb, "g_att", 2, [64, 4, 64], BF16)
                oo = Rot(st, sb, "g_oo", 2, [128, 8, 64], F32)
                p_a = st.enter_context(ps("g_pa", [64, 512], F32))
                p_b = st.enter_context(ps("g_pb", [64, 512], F32))
                p_c = st.enter_context(ps("g_pc", [128, 4, 64], F32))
                p_at = st.enter_context(ps("g_pat", [64, 4, 64], F32))
                p_o = st.enter_context(ps("g_po", [128, 8, 64], F32))
                p_s = st.enter_context(ps("g_ps", [128, 4, 256], F32))
                if d == 1:
                    of = Rot(st, sb, "g_of", 2, [128, 8, 64], F32)
                    rt = Rot(st, sb, "g_rt", 2, [128, 8, 64], F32)
                    sq = st.enter_context(sb("g_sq", [128, 8, 64], BF16))
                    rs = st.enter_context(sb("g_rs", [128, 4, 64], F32))
                    co = Rot(st, sb, "g_co", 2, [128, 8, 64], BF16)
                    p_n = st.enter_context(ps("g_pn", [128, 4, 64], F32))
                kb.dma(wg[:], I["gla_wg"][l, d], writes=["g_wg"])
                kb.dma(bg[:], I["gla_bg"][l, d], writes=["g_bg"])
                kb.dma(ngt[:], I["gla_ng"][l], writes=["g_ng"])
                kb.op("pool", lambda e: e.memset(S[:], 0.0), writes=["g_S"])
                kb.op("pool", lambda e: e.memset(Sb[:], 0.0), writes=["g_Sb"])
                tr = tri[d][0:64, 0:64]
                for ci in order:
                    t0 = ci * 64
                    gl, gln = glr.next()
                    q_, qn_ = qk.next()
                    k_, kn_ = kv.next()
                    v_, vn_ = vb.next()
                    g_, gn_ = gt.next()
                    gc_, gcn = gcs.next()
                    ko_, kon = ko.next()
                    e_, en_ = eg.next()
                    qi, qin = qkin.next()
                    at, atn = att.next()
                    o_, on_ = oo.next()
                    kb.dma(gl[:], PT[O_GLR + d * 16:O_GLR + (d + 1) * 16, t0:t0 + 64], reads=["dram_PT"], writes=[gln])
                    kb.dma(q_[:], PT[O_GQ:O_GQ + 1024, t0:t0 + 64].rearrange("(c p) t -> p c t", p=128), reads=["dram_PT"], writes=[qn_])
                    kb.dma(k_[:], PK[t0:t0 + 64, O_GK:O_GK + 1536], reads=["dram_PK"], writes=[kn_])
                    kb.op("pool", lambda e: e.tensor_copy(out=v_[:], in_=k_[:, 512:1536]), reads=[kn_], writes=[vn_])
                    kb.op("pe", lambda e: e.matmul(p_a[:], lhsT=gl[:], rhs=wg[:], start=True, stop=False), reads=[gln, "g_wg"], writes=["g_pa"])
                    kb.op("pe", lambda e: e.matmul(p_a[:], lhsT=ones_f[0:1, 0:64], rhs=bg[:], start=False, stop=True), reads=["ones_f", "g_bg"], writes=["g_pa"])
                    kb.op("act", lambda e: e.activation(out=g_[:], in_=p_a[:], func=AF.Exp, scale=-1.0), reads=["g_pa"], writes=[gn_])
                    kb.op("act", lambda e: e.activation(out=g_[:], in_=g_[:], func=AF.Ln, bias=kcon[0:64, 0:1], scale=1.0), reads=[gn_, "kcon"], writes=[gn_])
                    kb.op("dve", lambda e: e.tensor_scalar(out=g_[:], in0=g_[:], scalar1=-1.0 / 16.0, scalar2=None, op0=ALU.mult), reads=[gn_], writes=[gn_])
                    kb.op("pe", lambda e: e.matmul(p_b[:], lhsT=tr, rhs=g_[:], start=True, stop=True), reads=["tri", gn_], writes=["g_pb"])
                    kb.op("pe", lambda e: e.matmul(p_a[:], lhsT=ones_f[0:64, 0:64], rhs=g_[:], start=True, stop=True), reads=["ones_f", gn_], writes=["g_pa"])
                    for h in range(4):
                        kb.op("pe", lambda e: e.matmul(p_c[:, h, :], lhsT=g_[:, h * 128:(h + 1) * 128], rhs=tr, start=True, stop=True), reads=[gn_, "tri"], writes=["g_pc"])
                    kb.op("act", lambda e: e.copy(out=gc_[:], in_=p_b[:]), reads=["g_pb"], writes=[gcn])
                    kb.op("dve", lambda e: e.tensor_tensor(out=gc_[:], in0=p_a[:], in1=gc_[:], op=ALU.subtract), reads=["g_pa", gcn], writes=[gcn])
                    kb.op("act", lambda e: e.activation(out=gc_[:], in_=gc_[:], func=AF.Exp), reads=[gcn], writes=[gcn])
                    kb.op("dve", lambda e: e.tensor_tensor(out=ko_[:], in0=k_[:, 0:512], in1=gc_[:], op=ALU.mult), reads=[kn_, gcn], writes=[kon])
                    kb.op("act", lambda e: e.activation(out=e_[:, 0:4, :], in_=p_c[:], func=AF.Exp), reads=["g_pc"], writes=[en_])
                    kb.op("act", lambda e: e.activation(out=e_[:, 4:8, :], in_=p_c[:], func=AF.Exp, scale=-1.0), reads=["g_pc"], writes=[en_])
                    kb.op("dve", lambda e: e.scalar_tensor_tensor(out=qi[:, 0:4, :], in0=q_[:, 0:4, :], scalar=QS, in1=e_[:, 0:4, :], op0=ALU.mult, op1=ALU.mult),
                          reads=[qn_, en_], writes=[qin])
                    kb.op("pool", lambda e: e.tensor_tensor(out=qi[:, 4:8, :], in0=q_[:, 4:8, :], in1=e_[:, 4:8, :], op=ALU.mult), reads=[qn_, en_], writes=[qin])
                    for h in range(4):
                        kb.op("pe", lambda e: e.matmul(p_at[:, h, :], lhsT=qi[:, 4 + h, :], rhs=qi[:, h, :], start=True, stop=True), reads=[qin], writes=["g_pat"])
                    kb.op("dve", lambda e: e.tensor_tensor(out=at[:], in0=p_at[:], in1=tr.unsqueeze(1).to_broadcast([64, 4, 64]), op=ALU.mult),
                          reads=["g_pat", "tri"], writes=[atn])
                    for vc in range(8):
                        h = vc // 2
                        kb.op("pe", lambda e: e.matmul(p_o[:, vc, :], lhsT=v_[:, vc * 128:(vc + 1) * 128], rhs=at[:, h, :], start=True, stop=False),
                              reads=[vn_, atn], writes=["g_po"])
                        kb.op("pe", lambda e: e.matmul(p_o[:, vc, :], lhsT=Sb[:, h, (vc % 2) * 128:(vc % 2 + 1) * 128], rhs=qi[:, h, :], start=False, stop=True),
                              reads=["g_Sb", qin], writes=["g_po"])
                    for h in range(4):
                        kb.op("pe", lambda e: e.matmul(p_s[:, h, :], lhsT=ko_[:, h * 128:(h + 1) * 128], rhs=v_[:, h * 256:(h + 1) * 256], start=True, stop=True),
                              reads=[kon, vn_], writes=["g_ps"])
                    for h in range(4):
                        kb.op("dve", lambda e: e.scalar_tensor_tensor(out=S[:, h, :], in0=S[:, h, :], scalar=e_[:, h, last:last + 1], in1=p_s[:, h, :],
                                                                       op0=ALU.mult, op1=ALU.add), reads=["g_S", en_, "g_ps"], writes=["g_S"])
                    kb.op("act", lambda e: e.copy(out=Sb[:], in_=S[:]), reads=["g_S"], writes=["g_Sb"])
                    if d == 0:
                        kb.op("act", lambda e: e.copy(out=o_[:], in_=p_o[:]), reads=["g_po"], writes=[on_])
                        kb.dma(ofT[:, t0:t0 + 64].rearrange("(c p) t -> p c t", p=128), o_[:], reads=[on_], writes=["dram_ofT"])
                    else:
                        f_, fn_ = of.next()
                        r_, rn_ = rt.next()
                        c_, cn_ = co.next()
                        kb.dma(f_[:], ofT[:, t0:t0 + 64].rearrange("(c p) t -> p c t", p=128), reads=["dram_ofT"], writes=[fn_])
                        kb.dma(r_[:], PT[O_GR:O_GR + 1024, t0:t0 + 64].rearrange("(c p) t -> p c t", p=128), reads=["dram_PT"], writes=[rn_])
                        kb.op("dve", lambda e: e.tensor_tensor(out=o_[:], in0=p_o[:], in1=f_[:], op=ALU.add), reads=["g_po", fn_], writes=[on_])
                        kb.op("act", lambda e: e.activation(out=sq[:], in_=o_[:], func=AF.Square), reads=[on_], writes=["g_sq"])
                        for h in range(4):
                            for u in range(2):
                                kb.op("pe", lambda e: e.matmul(p_n[:, h, :], lhsT=ones_b[:], rhs=sq[:, 2 * h + u, :], start=(u == 0), stop=(u == 1)),
                                      reads=["ones_b", "g_sq"], writes=["g_pn"])
                        kb.op("act", lambda e: e.activation(out=rs[:], in_=p_n[:], func=AF.Sqrt, scale=1.0 / 256, bias=C_EPS), reads=["g_pn", "kcon"], writes=["g_rs"])
                        kb.op("dve", lambda e: e.reciprocal(out=rs[:], in_=rs[:]), reads=["g_rs"], writes=["g_rs"])
                        ov = o_[:].rearrange("p (h u) t -> p h u t", u=2)
                        kb.op("dve", lambda e: e.tensor_tensor(out=ov, in0=ov, in1=rs[:].unsqueeze(2).to_broadcast([128, 4, 2, 64]), op=ALU.mult),
                              reads=[on_, "g_rs"], writes=[on_])
                        for u in range(2):
                            kb.op("pool", lambda e: e.tensor_scalar(out=o_[:, u::2, :], in0=o_[:, u::2, :], scalar1=ngt[:, u:u + 1], scalar2=None, op0=ALU.mult),
                                  reads=[on_, "g_ng"], writes=[on_])
                        kb.op("act", lambda e: e.activation(out=r_[:], in_=r_[:], func=AF.Silu), reads=[rn_], writes=[rn_])
                        kb.op("dve", lambda e: e.tensor_tensor(out=c_[:], in0=o_[:], in1=r_[:], op=ALU.mult), reads=[on_, rn_], writes=[cn_])
                        kb.dma(cT[:, t0:t0 + 64].rearrange("(c p) t -> p c t", p=128), c_[:], reads=[cn_], writes=["dram_cT"])
                kb.barrier()

        def stage_qknorm(l):
            with ExitStack() as st:
                agt = st.enter_context(sb("q_ag", [128, 4], F32))
                pmf = st.enter_context(sb("q_pmf", [128, 128], F32))
                pmb = st.enter_context(sb("q_pmb", [128, 128], BF16))
                xin = Rot(st, sb, "q_x", 3, [128, 512], F32)
                sqb = Rot(st, sb, "q_sq", 2, [128, 512], BF16)
                rsd = Rot(st, sb, "q_rs", 2, [128, 512], F32)
                xnb = Rot(st, sb, "q_xn", 3, [128, 512], BF16)
                cs = Rot(st, sb, "q_cs", 2, [128, 2, 512], F32)
                t1 = Rot(st, sb, "q_t1", 2, [128, 512], F32)
                t2 = Rot(st, sb, "q_t2", 2, [128, 512], F32)
                xr = Rot(st, sb, "q_xr", 2, [128, 512], BF16)
                pp = Rot(st, ps, "q_ps", 2, [128, 512], F32)
                pr = Rot(st, ps, "q_pr", 2, [128, 512], F32)
                vin = Rot(st, sb, "q_vin", 2, [128, 1280], F32)
                vo = Rot(st, sb, "q_vo", 2, [128, 1280], BF16)
                kb.dma(agt[:], I["att_g"][l], writes=["q_ag"])
                kb.dma(pmf[:], I["pm"][:, :], writes=["q_pmf"])
                kb.op("dve", lambda e: e.tensor_copy(out=pmb[:], in_=pmf[:]), reads=["q_pmf"], writes=["q_pmb"])
                for ti in range(NT):
                    t0 = ti * 128
                    a, an = vin.next()
                    o, on = vo.next()
                    kb.dma(a[:, 0:1024], PK[t0:t0 + 128, O_NA + 2048:O_NA + 3072], reads=["dram_PK"], writes=[an])
                    kb.dma(a[:, 1024:1280], PK[t0:t0 + 128, O_SV:O_SV + 256], reads=["dram_PK"], writes=[an])
                    kb.op("pool", lambda e: e.tensor_copy(out=o[:], in_=a[:]), reads=[an], writes=[on])
                    kb.dma(vtok[t0:t0 + 128, :], o[:], reads=[on], writes=["dram_vtok"])
                chunks = []
                for c in range(8):
                    chunks.append((O_NA + c * 128, nqk, c * 128, 0, True, False))
                for c in range(8):
                    chunks.append((O_NA + 1024 + c * 128, nqk, 1024 + c * 128, 1, False, False))
                for c in range(8):
                    chunks.append((O_SQ + c * 128, sqk, c * 128, 2, True, True))
                for c in range(2):
                    chunks.append((O_SK + c * 128, sqk, 1024 + c * 128, 3, False, True))
                for (b0, bn) in cfg.blocks():
                    is_ctx = b0 < TC
                    if not is_ctx:
                        c_, cn_ = cs.next()
                        kb.dma(c_[:, :, 0:bn], I["rope"][:, :, b0 - TC:b0 - TC + bn].rearrange("a p t -> p a t"), writes=[cn_])
                    for (srow, dst, drow, gcol, is_q, rope) in chunks:
                        x, xn_ = xin.next()
                        q2, q2n = sqb.next()
                        r_, rn_ = rsd.next()
                        xb, xbn = xnb.next()
                        p, pn = pp.next()
                        kb.dma(x[:, 0:bn], PT[srow:srow + 128, b0:b0 + bn], reads=["dram_PT"], writes=[xn_])
                        kb.op("act", lambda e: e.activation(out=q2[:, 0:bn], in_=x[:, 0:bn], func=AF.Square), reads=[xn_], writes=[q2n])
                        kb.op("pe", lambda e: e.matmul(p[:, 0:bn], lhsT=blk64[:], rhs=q2[:, 0:bn], start=True, stop=True), reads=["blk64", q2n], writes=[pn])
                        if is_q:
                            kb.op("act", lambda e: e.activation(out=r_[:, 0:bn], in_=p[:, 0:bn], func=AF.Sqrt, scale=1.0, bias=kcon[:, 3:4]),
                                  reads=[pn, "kcon"], writes=[rn_])
                        else:
                            kb.op("act", lambda e: e.activation(out=r_[:, 0:bn], in_=p[:, 0:bn], func=AF.Sqrt, scale=1.0 / 64, bias=C_EPS),
                                  reads=[pn, "kcon"], writes=[rn_])
                        kb.op("dve", lambda e: e.reciprocal(out=r_[:, 0:bn], in_=r_[:, 0:bn]), reads=[rn_], writes=[rn_])
                        kb.op("dve", lambda e: e.scalar_tensor_tensor(out=xb[:, 0:bn], in0=x[:, 0:bn], scalar=agt[:, gcol:gcol + 1], in1=r_[:, 0:bn],
                                                                       op0=ALU.mult, op1=ALU.mult), reads=[xn_, "q_ag", rn_], writes=[xbn])
                        if rope and not is_ctx:
                            rp, rpn = pr.next()
                            a1, a1n = t1.next()
                            a2, a2n = t2.next()
                            o, on = xr.next()
                            kb.op("pe", lambda e: e.matmul(rp[:, 0:bn], lhsT=pmb[:], rhs=xb[:, 0:bn], start=True, stop=True), reads=["q_pmb", xbn], writes=[rpn])
                            kb.op("dve", lambda e: e.tensor_tensor(out=a1[:, 0:bn], in0=rp[:, 0:bn], in1=c_[:, 1, 0:bn], op=ALU.mult), reads=[rpn, cn_], writes=[a1n])
                            kb.op("pool", lambda e: e.tensor_tensor(out=a2[:, 0:bn], in0=xb[:, 0:bn], in1=c_[:, 0, 0:bn], op=ALU.mult), reads=[xbn, cn_], writes=[a2n])
                            kb.op("pool", lambda e: e.tensor_tensor(out=o[:, 0:bn], in0=a1[:, 0:bn], in1=a2[:, 0:bn], op=ALU.add), reads=[a1n, a2n], writes=[on])
                            kb.dma(dst[drow:drow + 128, b0:b0 + bn], o[:, 0:bn], reads=[on], writes=["dram_qk"])
                        else:
                            kb.dma(dst[drow:drow + 128, b0:b0 + bn], xb[:, 0:bn], reads=[xbn], writes=["dram_qk"])
                kb.barrier()

        def stage_attn(l, kind):
            ntl = TL // 128
            nct = TC // 128
            cls_list, cls_of = na_classes(TL)
            with ExitStack() as st:
                qt = Rot(st, sb, "a_q", 2, [128, 8, 128], BF16)
                kc_ = st.enter_context(sb("a_kc", [128, 8, nct * 128], BF16))
                vc_ = st.enter_context(sb("a_vc", [128, nct, 1024], BF16))
                kl = Rot(st, sb, "a_kl", 2, [128, 8, 5 * 128], BF16)
                vl = Rot(st, sb, "a_vl", 2, [128, 5, 1024], BF16)
                pT = Rot(st, sb, "a_pT", 3, [128, 7, 128], BF16)
                rd = Rot(st, sb, "a_rd", 2, [64, 128], F32)
                ob = Rot(st, sb, "a_ob", 2, [64, 16, 128], BF16)
                p_sc = Rot(st, ps, "a_psc", 2, [128, 8, 128], F32)
                p_od = Rot(st, ps, "a_pod", 2, [64, 2, 128], F32)
                if kind == "na":
                    bf_ = st.enter_context(sb("a_bf", [128, 5 * 128], F32))
                    bias = st.enter_context(sb("a_bias", [128, 16, 5, 128], BF16))
                    qsrc, ksrc, vcol, dst = nqk[0:1024], nqk[1024:2048], 0, bT
                    nkc = 8
                else:
                    esk = st.enter_context(sb("a_esk", [64, 16], F32))
                    kb.dma(esk[:], I["esink"][l], writes=["a_esk"])
                    kb.op("act", lambda e: e.activation(out=esk[:], in_=esk[:], func=AF.Exp), reads=["a_esk"], writes=["a_esk"])
                    qsrc, ksrc, vcol, dst = sqk[0:1024], sqk[1024:1280], 1024, dT
                    nkc = 4

                def load_k(tile_, t0, n):
                    if kind == "na":
                        return [(tile_[:, :, 0:n], ksrc[:, t0:t0 + n].rearrange("(c p) t -> p c t", p=128))]
                    src = ksrc[:, t0:t0 + n].rearrange("(g d) t -> d g t", d=64)
                    return [(tile_[0:64, 0:4, 0:n], src), (tile_[64:128, 0:4, 0:n], src)]

                for (a_, b_) in load_k(kc_, 0, TC):
                    kb.dma(a_, b_, reads=["dram_qk"], writes=["a_kc"])
                nv = 1024 if kind == "na" else 256
                kb.dma(vc_[:, :, 0:nv], vtok[0:TC, vcol:vcol + nv].rearrange("(n p) c -> p n c", p=128), reads=["dram_vtok"], writes=["a_vc"])
                cur_cls = None
                for qi in range(nct + ntl):
                    t0 = qi * 128
                    q_, qn_ = qt.next()
                    o_, on_ = ob.next()
                    kb.dma(q_[:], qsrc[:, t0:t0 + 128].rearrange("(c p) t -> p c t", p=128), reads=["dram_qk"], writes=[qn_])
                    keys = [("c", j, None) for j in range(nct)]
                    if qi >= nct:
                        ti = qi - nct
                        k_, kn_ = kl.next()
                        v_, vn_ = vl.next()
                        if kind == "na":
                            ci = cls_of[ti]
                            off_a, off_b, klo_rel, nsl = cls_list[ci]
                            kt_lo = ti + klo_rel
                            if ci != cur_cls:
                                cur_cls = ci
                                for h in range(16):
                                    kb.dma(bf_[:], I["na_bias"][l, ci, :, h * 640:(h + 1) * 640], writes=["a_bf"])
                                    kb.op("pool", lambda e: e.tensor_copy(out=bias[:, h, :, :], in_=bf_[:].rearrange("p (s i) -> p s i", s=5)),
                                          reads=["a_bf"], writes=["a_bias"])
                            for sl in range(nsl):
                                keys.append(("l", sl, ("na", sl)))
                        else:
                            kt_lo = max(ti - 1, 0)
                            kt_hi = min(ti + 1, ntl - 1)
                            nsl = kt_hi - kt_lo + 1
                            for sl in range(nsl):
                                kt = kt_lo + sl
                                keys.append(("l", sl, None if kt == ti else ("m", 1 if kt < ti else 0)))
                        tk = TC + kt_lo * 128
                        for (a_, b_) in load_k(k_, tk, nsl * 128):
                            kb.dma(a_, b_, reads=["dram_qk"], writes=[kn_])
                        kb.dma(v_[:, 0:nsl, 0:nv], vtok[tk:tk + nsl * 128, vcol:vcol + nv].rearrange("(n p) c -> p n c", p=128),
                               reads=["dram_vtok"], writes=[vn_])
                    nk = len(keys)
                    for h in range(16):
                        c, b = h // 2, (h % 2) * 64
                        kc_i = c if kind == "na" else h // 4
                        vh = h if kind == "na" else h // 4
                        sc, scn = p_sc.next()
                        od, odn = p_od.next()
                        p_, pn_ = pT.next()
                        r_, rn_ = rd.next()
                        for j, (src, idx, bsp) in enumerate(keys):
                            kt_ = kc_ if src == "c" else k_
                            ktn = "a_kc" if src == "c" else kn_
                            kb.op("pe", lambda e: e.matmul(sc[:, j, :], lhsT=kt_[b:b + 64, kc_i, idx * 128:(idx + 1) * 128], rhs=q_[b:b + 64, c, :],
                                                           start=True, stop=(bsp is None)), reads=[ktn, qn_], writes=[scn])
                            if bsp is not None:
                                if bsp[0] == "na":
                                    kb.op("pe", lambda e: e.matmul(sc[:, j, :], lhsT=ident_b[:], rhs=bias[:, h, bsp[1], :], start=False, stop=True),
                                          reads=["ident_b", "a_bias"], writes=[scn])
                                else:
                                    kb.op("pe", lambda e: e.matmul(sc[:, j, :], lhsT=ident_b[:], rhs=negm_b[bsp[1]][:], start=False, stop=True),
                                          reads=["ident_b", "negm_b"], writes=[scn])
                        kb.op("act", lambda e: e.activation(out=p_[:, 0:nk, :], in_=sc[:, 0:nk, :], func=AF.Exp), reads=[scn], writes=[pn_])
                        for j, (src, idx, bsp) in enumerate(keys):
                            vt_ = vc_ if src == "c" else v_
                            vtn = "a_vc" if src == "c" else vn_
                            kb.op("pe", lambda e: e.matmul(od[:, 0, :], lhsT=vt_[:, idx, vh * 64:(vh + 1) * 64], rhs=p_[:, j, :], start=(j == 0), stop=(j == nk - 1)),
                                  reads=[vtn, pn_], writes=[odn])
                        for j in range(nk):
                            kb.op("pe", lambda e: e.matmul(od[:, 1, :], lhsT=ones_b[:, 0:64], rhs=p_[:, j, :], start=(j == 0), stop=(j == nk - 1)),
                                  reads=["ones_b", pn_], writes=[odn])
                        if kind == "swa":
                            kb.op("dve", lambda e: e.tensor_scalar(out=r_[:], in0=od[:, 1, :], scalar1=esk[:, h:h + 1], scalar2=None, op0=ALU.add),
                                  reads=[odn, "a_esk"], writes=[rn_])
                            kb.op("dve", lambda e: e.reciprocal(out=r_[:], in_=r_[:]), reads=[rn_], writes=[rn_])
                        else:
                            kb.op("dve", lambda e: e.reciprocal(out=r_[:], in_=od[:, 1, :]), reads=[odn], writes=[rn_])
                        kb.op("dve", lambda e: e.tensor_tensor(out=o_[:, h, :], in0=od[:, 0, :], in1=r_[:], op=ALU.mult), reads=[odn, rn_], writes=[on_])
                    kb.dma(dst[:, t0:t0 + 128].rearrange("(h d) t -> d h t", d=64), o_[:], reads=[on_], writes=["dram_o" + kind])
                kb.barrier()

        def stage_merge():
            with ExitStack() as st:
                wbr = Rot(st, sb, "m_w", 2, [128, 32, 128], BF16)
                ob = [Rot(st, sb, "m_o%d" % i, 2, [128, 8, 512], BF16) for i in range(4)]
                gt = Rot(st, sb, "m_g", 4, [128, 512], F32)
                acc = Rot(st, sb, "m_acc", 2, [128, 512], F32)
                tmp = Rot(st, sb, "m_tmp", 2, [128, 512], F32)
                mo = Rot(st, sb, "m_mo", 2, [128, 512], BF16)
                pp = Rot(st, ps, "m_ps", 4, [128, 512], F32)
                srcs = [aT, bT, cT, dT]
                for fc in range(16):
                    w, wn = wbr.next()
                    kb.dma(w[:], wb_br[:, fc * 128:(fc + 1) * 128].rearrange("(a p) f -> p a f", p=128), reads=["dram_wb_br"], writes=[wn])
                    for (b0, bn) in cfg.blocks():
                        a, an = acc.next()
                        for i in range(4):
                            o, on = ob[i].next()
                            g, gn = gt.next()
                            p, pn = pp.next()
                            kb.dma(o[:, :, 0:bn], srcs[i][:, b0:b0 + bn].rearrange("(c p) t -> p c t", p=128), reads=["dram_br%d" % i], writes=[on])
                            kb.dma(g[:, 0:bn], PT[O_GATE + i * 2048 + fc * 128:O_GATE + i * 2048 + (fc + 1) * 128, b0:b0 + bn], reads=["dram_PT"], writes=[gn])
                            for kc in range(8):
                                kb.op("pe", lambda e: e.matmul(p[:, 0:bn], lhsT=w[:, i * 8 + kc, :], rhs=o[:, kc, 0:bn], start=(kc == 0), stop=(kc == 7)),
                                      reads=[wn, on], writes=[pn])
                            kb.op("act", lambda e: e.activation(out=g[:, 0:bn], in_=g[:, 0:bn], func=AF.Sigmoid), reads=[gn], writes=[gn])
                            if i == 0:
                                kb.op("dve", lambda e: e.tensor_tensor(out=a[:, 0:bn], in0=p[:, 0:bn], in1=g[:, 0:bn], op=ALU.mult), reads=[pn, gn], writes=[an])
                            else:
                                t_, tn = tmp.next()
                                kb.op("dve", lambda e: e.tensor_tensor(out=t_[:, 0:bn], in0=p[:, 0:bn], in1=g[:, 0:bn], op=ALU.mult), reads=[pn, gn], writes=[tn])
                                kb.op("pool", lambda e: e.tensor_tensor(out=a[:, 0:bn], in0=a[:, 0:bn], in1=t_[:, 0:bn], op=ALU.add), reads=[an, tn], writes=[an])
                        m_, mn = mo.next()
                        kb.op("act", lambda e: e.copy(out=m_[:, 0:bn], in_=a[:, 0:bn]), reads=[an], writes=[mn])
                        kb.dma(mTd[fc * 128:(fc + 1) * 128, b0:b0 + bn], m_[:, 0:bn], reads=[mn], writes=["dram_mTd"])
                kb.barrier()

        def resid_tile(st_tag, x, xname, pacc, pn, gtb, o, on):
            for oc in range(4):
                sl = slice(oc * 512, (oc + 1) * 512)
                kb.op("dve", lambda e: e.tensor_tensor(out=o[:, sl], in0=pacc[:, oc, :], in1=gtb[:, sl], op=ALU.mult), reads=[pn, st_tag + "_gtb"], writes=[on])
                kb.op("pool", lambda e: e.tensor_tensor(out=o[:, sl], in0=o[:, sl], in1=x[:, sl], op=ALU.add), reads=[on, xname], writes=[on])

        def stage_outproj(src, dst):
            with ExitStack() as st:
                wo = st.enter_context(sb("o_w", [128, NKC, D], BF16))
                gtb = st.enter_context(sb("o_gtb", [128, D], F32))
                mb = Rot(st, sb, "o_mb", 2, [128, NKC, 512], BF16)
                xin = Rot(st, sb, "o_x", 2, [128, D], F32)
                xo = Rot(st, sb, "o_xo", 2, [128, D], F32)
                pp = Rot(st, ps, "o_ps", 2, [128, 4, 512], F32)
                kb.dma(wo[:], wb_out[:, :].rearrange("(kc p) f -> p kc f", p=128), reads=["dram_wb_out"], writes=["o_w"])
                for (b0, bn) in cfg.blocks():
                    j = 1 if b0 < TC else 0
                    if b0 == 0 or b0 == TC:
                        kb.dma(gtb[:], modd[j:j + 1, 32 * 128:48 * 128].to_broadcast([128, D]), reads=["modd"], writes=["o_gtb"])
                    m_, mn = mb.next()
                    kb.dma(m_[:, :, 0:bn], mTd[:, b0:b0 + bn].rearrange("(kc p) t -> p kc t", p=128), reads=["dram_mTd"], writes=[mn])
                    for ti in range(bn // 128):
                        t0 = b0 + ti * 128
                        x, xname = xin.next()
                        o, on = xo.next()
                        p, pn = pp.next()
                        kb.dma(x[:], src[t0:t0 + 128, :], reads=["dram_x"], writes=[xname])
                        for oc in range(4):
                            for kc in range(NKC):
                                kb.op("pe", lambda e: e.matmul(p[:, oc, :], lhsT=m_[:, kc, ti * 128:(ti + 1) * 128], rhs=wo[:, kc, oc * 512:(oc + 1) * 512],
                                                               start=(kc == 0), stop=(kc == NKC - 1)), reads=[mn, "o_w"], writes=[pn])
                        resid_tile("o", x, xname, p, pn, gtb, o, on)
                        kb.dma(dst[t0:t0 + 128, :], o[:], reads=[on], writes=["dram_x1"])
                kb.barrier()

        def stage_peer_prep(l):
            conv_bf16(I["peer_wq"][l], wb_q, D, D, "wb_q")
            conv_bf16(I["peer_v"][l], Vd, 16384, D, "Vd")
            with ExitStack() as st:
                uin = Rot(st, sb, "u_in", 2, [128, D], F32)
                ub = Rot(st, sb, "u_b", 2, [128, D], BF16)
                ut = Rot(st, sb, "u_t", 2, [128, NKC, 512], BF16)
                tp = Rot(st, ps, "u_tp", 2, [128, NKC, 128], BF16)
                for ig in range(32):
                    u_, un = ut.next()
                    for et in range(4):
                        e0 = (ig * 4 + et) * 128
                        a, an = uin.next()
                        b, bn_ = ub.next()
                        p, pn = tp.next()
                        kb.dma(a[:], I["peer_u"][l, e0:e0 + 128, :], writes=[an])
                        kb.op("pool" if et % 2 else "dve", lambda e: e.tensor_copy(out=b[:], in_=a[:]), reads=[an], writes=[bn_])
                        for kc in range(NKC):
                            kb.op("pe", lambda e: e.transpose(out=p[:, kc, :], in_=b[:, kc * 128:(kc + 1) * 128], identity=ident_b[:]),
                                  reads=[bn_, "ident_b"], writes=[pn])
                        kb.op("act", lambda e: e.copy(out=u_[:, :, et * 128:(et + 1) * 128], in_=p[:]), reads=[pn], writes=[un])
                    kb.dma(UTd[ig], u_[:].rearrange("p k e -> p (k e)"), reads=[un], writes=["dram_UTd"])
                kb.barrier()

        def stage_peer_q():
            with ExitStack() as st:
                wq = st.enter_context(sb("pq_w", [128, NKC, D], BF16))
                hb = Rot(st, sb, "pq_h", 2, [128, NKC, 512], BF16)
                og = Rot(st, sb, "pq_o", 3, [128, 512], F32)
                pp = Rot(st, ps, "pq_ps", 4, [128, 512], F32)
                kb.dma(wq[:], wb_q[:, :].rearrange("(kc p) f -> p kc f", p=128), reads=["dram_wb_q"], writes=["pq_w"])
                k = 0
                for (b0, bn) in cfg.blocks():
                    h_, hn = hb.next()
                    kb.dma(h_[:, :, 0:bn], hT[:, b0:b0 + bn].rearrange("(kc p) t -> p kc t", p=128), reads=["dram_hT"], writes=[hn])
                    for fc in range(16):
                        p, pn = pp.next()
                        o, on = og.next()
                        for kc in range(NKC):
                            kb.op("pe", lambda e: e.matmul(p[:, 0:bn], lhsT=wq[:, kc, fc * 128:(fc + 1) * 128], rhs=h_[:, kc, 0:bn], start=(kc == 0), stop=(kc == NKC - 1)),
                                  reads=["pq_w", hn], writes=[pn])
                        if k % 2:
                            kb.op("act", lambda e: e.copy(out=o[:, 0:bn], in_=p[:, 0:bn]), reads=[pn], writes=[on])
                        else:
                            kb.op("dve", lambda e: e.tensor_copy(out=o[:, 0:bn], in_=p[:, 0:bn]), reads=[pn], writes=[on])
                        k += 1
                        kb.dma(qTd[fc * 128:(fc + 1) * 128, b0:b0 + bn], o[:, 0:bn], reads=[on], writes=["dram_qTd"])
                kb.barrier()

        def stage_peer_topk(l):
            BIGN = -1.0e30
            with ExitStack() as st:
                kk = st.enter_context(sb("pt_kk", [128, 2, 128], F32))
                qt = Rot(st, sb, "pt_q", 2, [128, 16, 128], F32)
                ss = Rot(st, sb, "pt_ss", 2, [128, 16, 128], F32)
                v12 = Rot(st, sb, "pt_v", 2, [128, 2, 16], F32)
                tmp = Rot(st, sb, "pt_tmp", 2, [128, 128], F32)
                cand = Rot(st, sb, "pt_cand", 2, [128, 16, 16], F32)
                tmp2 = Rot(st, sb, "pt_tmp2", 2, [128, 256], F32)
                tv = Rot(st, sb, "pt_tv", 2, [128, 16], F32)
                ez = Rot(st, sb, "pt_ez", 2, [128, 16], F32)
                thr = Rot(st, sb, "pt_thr", 2, [128, 16], F32)
                zz = Rot(st, sb, "pt_zz", 2, [128, 8], F32)
                pp = Rot(st, ps, "pt_ps", 2, [128, 8, 128], F32)
                kb.dma(kk[:], I["peer_kk"][l], writes=["pt_kk"])
                for ti in range(NT):
                    t0 = ti * 128
                    q_, qn_ = qt.next()
                    s_, sn_ = ss.next()
                    th, thn = thr.next()
                    z_, zn_ = zz.next()
                    kb.dma(q_[:], qTd[:, t0:t0 + 128].rearrange("(c p) t -> p c t", p=128), reads=["dram_qTd"], writes=[qn_])
                    for half in range(2):
                        p, pn = pp.next()
                        for cc in range(8):
                            c = half * 8 + cc
                            kb.op("pe", lambda e: e.matmul(p[:, cc, :], lhsT=q_[:, c, :], rhs=kk[:, c % 2, :], start=True, stop=True), reads=[qn_, "pt_kk"], writes=[pn])
                        if half == 0:
                            kb.op("act", lambda e: e.copy(out=s_[:, 0:8, :], in_=p[:]), reads=[pn], writes=[sn_])
                        else:
                            kb.op("dve", lambda e: e.tensor_copy(out=s_[:, 8:16, :], in_=p[:]), reads=[pn], writes=[sn_])
                    kb.dma(sc_d[t0:t0 + 128, :], s_[:].rearrange("p c k -> p (c k)"), reads=[sn_], writes=["dram_sc_d"])
                    for h in range(8):
                        v_, vn_ = v12.next()
                        c_, cn_ = cand.next()
                        t2, t2n = tmp2.next()
                        tv_, tvn = tv.next()
                        e_, en_ = ez.next()
                        for u in range(2):
                            t_, tn_ = tmp.next()
                            kb.op("dve", lambda e: e.max(out=v_[:, u, 0:8], in_=s_[:, 2 * h + u, :]), reads=[sn_], writes=[vn_])
                            kb.op("dve", lambda e: e.match_replace(out=t_[:], in_to_replace=v_[:, u, 0:8], in_values=s_[:, 2 * h + u, :], imm_value=BIGN),
                                  reads=[sn_, vn_], writes=[tn_])
                            kb.op("dve", lambda e: e.max(out=v_[:, u, 8:16], in_=t_[:]), reads=[tn_], writes=[vn_])
                        kb.op("dve", lambda e: e.tensor_tensor(out=c_[:], in0=v_[:, 0, :].unsqueeze(2).to_broadcast([128, 16, 16]),
                                                               in1=v_[:, 1, :].unsqueeze(1).to_broadcast([128, 16, 16]), op=ALU.add), reads=[vn_], writes=[cn_])
                        cf = c_[:].rearrange("p a b -> p (a b)")
                        kb.op("dve", lambda e: e.max(out=tv_[:, 0:8], in_=cf), reads=[cn_], writes=[tvn])
                        kb.op("dve", lambda e: e.match_replace(out=t2[:], in_to_replace=tv_[:, 0:8], in_values=cf, imm_value=BIGN), reads=[cn_, tvn], writes=[t2n])
                        kb.op("dve", lambda e: e.max(out=tv_[:, 8:16], in_=t2[:]), reads=[t2n], writes=[tvn])
                        kb.op("dve", lambda e: e.tensor_scalar(out=th[:, h:h + 1], in0=tv_[:, 15:16], scalar1=-1.0, scalar2=None, op0=ALU.mult), reads=[tvn], writes=[thn])
                        kb.op("act", lambda e: e.activation(out=e_[:], in_=tv_[:], func=AF.Exp, bias=th[:, h:h + 1], scale=1.0), reads=[tvn, thn], writes=[en_])
                        kb.op("dve", lambda e: e.tensor_reduce(out=z_[:, h:h + 1], in_=e_[:], axis=AX.X, op=ALU.add), reads=[en_], writes=[zn_])
                    kb.op("act", lambda e: e.activation(out=z_[:], in_=z_[:], func=AF.Ln), reads=[zn_], writes=[zn_])
                    kb.op("dve", lambda e: e.tensor_scalar(out=th[:, 8:16], in0=z_[:], scalar1=-1.0, scalar2=None, op0=ALU.mult), reads=[zn_], writes=[thn])
                    kb.dma(thr_d[t0:t0 + 128, :], th[:], reads=[thn], writes=["dram_thr_d"])
                kb.barrier()

        def stage_peer_main(src, dst, t_lo, off=0):
            DELTA = -1.0e-4
            NTG = cfg.ntg
            tiles = list(range(t_lo // 128, NT))
            n_ = len(tiles)
            sizes = [NTG] * (n_ // NTG)
            r_ = n_ % NTG
            if r_ == 3:
                sizes.append(3)
            elif r_ == 2:
                sizes[-1:] = [3, 3]
            elif r_ == 1:
                sizes[-2:] = [3, 3, 3]
            assert sum(sizes) == n_ and min(sizes) >= 3 and NTG == 4
            groups, p_ = [], 0
            for z in sizes:
                groups.append(tiles[p_:p_ + z])
                p_ += z
            with ExitStack() as st:
                acc = [st.enter_context(sb("pm_acc%d" % k, [128, D], F32)) for k in range(NTG)]
                h2 = [st.enter_context(sb("pm_h2%d" % k, [128, NKC, 128], BF16)) for k in range(NTG)]
                s2 = [st.enter_context(sb("pm_s2%d" % k, [128, 8, 128], F32)) for k in range(NTG)]
                s1t = [st.enter_context(sb("pm_s1t%d" % k, [128, 8, 128], F32)) for k in range(NTG)]
                th = [st.enter_context(sb("pm_th%d" % k, [128, 16], F32)) for k in range(NTG)]
                ut = Rot(st, sb, "pm_ut", 2, [128, NKC, 512], BF16)
                vt = Rot(st, sb, "pm_vt", 2, [128, 4, D], BF16)
                ge = Rot(st, sb, "pm_ge", 4, [128, 512], F32)
                E_ = Rot(st, sb, "pm_E", 4, [128, 4, 128], F32)
                S_ = Rot(st, sb, "pm_S", 2, [128, 4, 128], F32)
                G_ = Rot(st, sb, "pm_G", 3, [128, 4, 128], F32)
                ga = Rot(st, sb, "pm_ga", 3, [128, 4, 128], F32)
                ap = Rot(st, sb, "pm_ap", 2, [128, 512], BF16)
                at = Rot(st, sb, "pm_at", 2, [128, 4, 128], BF16)
                p_a = Rot(st, ps, "pm_pa", 2, [128, 512], F32)
                p_t = st.enter_context(ps("pm_pt", [128, 4, 128], BF16))
                p_o = st.enter_context(ps("pm_po", [128, 4, 512], F32))
                for grp in groups:
                    for k, ti in enumerate(grp):
                        t0 = ti * 128
                        scv = sc_d[t0:t0 + 128, :].rearrange("p (h u k) -> p h u k", u=2, k=128)
                        kb.dma(h2[k][:], hT[:, t0:t0 + 128].rearrange("(kc p) t -> p kc t", p=128), reads=["dram_hT"], writes=["pm_h2%d" % k])
                        kb.dma(s2[k][:], scv[:, :, 1, :], reads=["dram_sc_d"], writes=["pm_s2%d" % k])
                        kb.dma(s1t[k][:], scv[:, :, 0, :], reads=["dram_sc_d"], writes=["pm_s1t%d" % k])
                        kb.dma(th[k][:], thr_d[t0:t0 + 128, :], reads=["dram_thr_d"], writes=["pm_th%d" % k])
                        kb.op("dve", lambda e: e.tensor_tensor(out=th[k][:, 0:8], in0=th[k][:, 0:8], in1=th[k][:, 8:16], op=ALU.add),
                              reads=["pm_th%d" % k], writes=["pm_th%d" % k])
                        kb.op("dve", lambda e: e.tensor_tensor(out=s1t[k][:], in0=s1t[k][:], in1=th[k][:, 0:8].unsqueeze(2).to_broadcast([128, 8, 128]), op=ALU.add),
                              reads=["pm_s1t%d" % k, "pm_th%d" % k], writes=["pm_s1t%d" % k])
                        kb.op("dve", lambda e: e.tensor_scalar(out=th[k][:, 8:16], in0=th[k][:, 8:16], scalar1=DELTA, scalar2=None, op0=ALU.add),
                              reads=["pm_th%d" % k], writes=["pm_th%d" % k])
                        kb.op("act", lambda e: e.activation(out=th[k][:, 8:16], in_=th[k][:, 8:16], func=AF.Exp), reads=["pm_th%d" % k], writes=["pm_th%d" % k])
                    igc = {}
                    units = [(ig, k) for ig in range(32) for k in range(len(grp))]

                    def P1(ig, k):
                        c = {}
                        if k == 0:
                            u_, un = ut.next()
                            v_, vn = vt.next()
                            kb.dma(u_[:].rearrange("p k e -> p (k e)"), UTd[ig], reads=["dram_UTd"], writes=[un])
                            kb.dma(v_[:], Vd[ig * 512:(ig + 1) * 512, :].rearrange("(et p) d -> p et d", p=128), reads=["dram_Vd"], writes=[vn])
                            igc[ig] = (u_, un, v_, vn)
                        u_, un, v_, vn = igc[ig]
                        pa, pan = p_a.next()
                        g_, gn = ge.next()
                        for kc in range(NKC):
                            kb.op("pe", lambda e: e.matmul(pa[:], lhsT=h2[k][:, kc, :], rhs=u_[:, kc, :], start=(kc == 0), stop=(kc == NKC - 1)),
                                  reads=["pm_h2%d" % k, un], writes=[pan])
                        kb.op("act", lambda e: e.activation(out=g_[:], in_=pa[:], func=AF.Gelu), reads=[pan], writes=[gn])
                        c["g"] = (g_, gn)
                        return c

                    def P2(ig, k, c):
                        ga_, gan = ga.next()
                        c["ga"] = (ga_, gan)
                        for h in range(8):
                            e_, en = E_.next()
                            if h >= 8 - cfg.dve_heads:
                                s_, sn = S_.next()
                                kb.op("dve", lambda e: e.tensor_tensor(out=s_[:], in0=s1t[k][:, h, ig * 4:(ig + 1) * 4].unsqueeze(2).to_broadcast([128, 4, 128]),
                                                                       in1=s2[k][:, h, :].unsqueeze(1).to_broadcast([128, 4, 128]), op=ALU.add),
                                      reads=["pm_s1t%d" % k, "pm_s2%d" % k], writes=[sn])
                                kb.op("act", lambda e: e.activation(out=e_[:], in_=s_[:], func=AF.Exp), reads=[sn], writes=[en])
                            else:
                                for i4 in range(4):
                                    col = ig * 4 + i4
                                    kb.op("act", lambda e: e.activation(out=e_[:, i4, :], in_=s2[k][:, h, :], func=AF.Exp, bias=s1t[k][:, h, col:col + 1], scale=1.0),
                                          reads=["pm_s2%d" % k, "pm_s1t%d" % k], writes=[en])
                            if h == 0:
                                kb.op("dve", lambda e: e.scalar_tensor_tensor(out=ga_[:], in0=e_[:], scalar=th[k][:, 8 + h:9 + h], in1=e_[:], op0=ALU.is_ge, op1=ALU.mult),
                                      reads=[en, "pm_th%d" % k], writes=[gan])
                            else:
                                m_, mn = G_.next()
                                kb.op("dve", lambda e: e.scalar_tensor_tensor(out=m_[:], in0=e_[:], scalar=th[k][:, 8 + h:9 + h], in1=e_[:], op0=ALU.is_ge, op1=ALU.mult),
                                      reads=[en, "pm_th%d" % k], writes=[mn])
                                kb.op("pool", lambda e: e.tensor_tensor(out=ga_[:], in0=ga_[:], in1=m_[:], op=ALU.add), reads=[gan, mn], writes=[gan])

                    def P3a1(ig, k, c):
                        g_, gn = c["g"]
                        ga_, gan = c["ga"]
                        ap_, apn = ap.next()
                        kb.op("dve", lambda e: e.tensor_tensor(out=ap_[:], in0=g_[:], in1=ga_[:].rearrange("p a b -> p (a b)"), op=ALU.mult), reads=[gn, gan], writes=[apn])
                        for et in range(4):
                            kb.op("pe", lambda e: e.transpose(out=p_t[:, et, :], in_=ap_[:, et * 128:(et + 1) * 128], identity=ident_b[:]),
                                  reads=[apn, "ident_b"], writes=["pm_pt"])

                    def P3a2(ig, k, c):
                        u_, un, v_, vn = igc[ig]
                        at_, atn = at.next()
                        kb.op("act", lambda e: e.copy(out=at_[:], in_=p_t[:]), reads=["pm_pt"], writes=[atn])
                        for et in range(4):
                            for dc in range(4):
                                kb.op("pe", lambda e: e.matmul(p_o[:, dc, :], lhsT=at_[:, et, :], rhs=v_[:, et, dc * 512:(dc + 1) * 512],
                                                               start=(et == 0), stop=(et == 3)), reads=[atn, vn], writes=["pm_po"])

                    def P3b(ig, k, c):
                        an = "pm_acc%d" % k
                        if ig == 0:
                            for dc in range(4):
                                sl = slice(dc * 512, (dc + 1) * 512)
                                kb.op("act", lambda e: e.copy(out=acc[k][:, sl], in_=p_o[:, dc, :]), reads=["pm_po"], writes=[an])
                        else:
                            for hf in range(2):
                                sl = slice(hf * 1024, (hf + 1) * 1024)
                                kb.op("dve", lambda e: e.tensor_tensor(out=acc[k][:, sl], in0=p_o[:, 2 * hf:2 * hf + 2, :].rearrange("p a b -> p (a b)"), in1=acc[k][:, sl], op=ALU.add),
                                      reads=["pm_po", an], writes=[an])

                    N_ = len(units)
                    cs_ = {}
                    for i in range(min(2, N_)):
                        cs_[i] = P1(*units[i])
                    for i in range(N_ + 2):
                        if 0 <= i - 1 < N_:
                            P3a1(*units[i - 1], cs_[i - 1])
                        if i < N_:
                            P2(*units[i], cs_[i])
                        if i + 2 < N_:
                            cs_[i + 2] = P1(*units[i + 2])
                        if 0 <= i - 2 < N_:
                            P3b(*units[i - 2], cs_[i - 2])
                            del cs_[i - 2]
                        if 0 <= i - 1 < N_:
                            P3a2(*units[i - 1], cs_[i - 1])
                    ub0, ub0n = ut.items[0]
                    ub1, ub1n = ut.items[1]
                    gtb = ub0[:].rearrange("p k e -> p (k e)").bitcast(F32)[:, 0:D]
                    for k, ti in enumerate(grp):
                        t0 = ti * 128
                        j = 1 if t0 < TC else 0
                        xin = ub1[:].rearrange("p k e -> p (k e)").bitcast(F32)[:, 0:D]
                        kb.dma(gtb, modd[j:j + 1, 80 * 128:96 * 128].to_broadcast([128, D]), reads=["modd"], writes=[ub0n])
                        kb.dma(xin, src[t0:t0 + 128, :], reads=["dram_x1"], writes=[ub1n])
                        an = "pm_acc%d" % k
                        kb.op("dve", lambda e: e.tensor_tensor(out=acc[k][:], in0=acc[k][:], in1=gtb, op=ALU.mult), reads=[an, ub0n], writes=[an])
                        kb.op("pool", lambda e: e.tensor_tensor(out=acc[k][:], in0=acc[k][:], in1=xin, op=ALU.add), reads=[an, ub1n], writes=[an])
                        kb.dma(dst[t0 - off:t0 - off + 128, :], acc[k][:], reads=[an], writes=["dram_x2"])
                kb.barrier()

        kb.dma(xs[0][0:TC, :], I["ctx"][:, :], writes=["dram_x"])
        for t in range(0, TL, 1024):
            n = min(1024, TL - t)
            kb.dma(xs[0][TC + t:TC + t + n, :], I["x"][t:t + n, :], writes=["dram_x"])
        kb.barrier()
        xcur = xs[0]
        for l in range(L):
            last = (l == L - 1) and not cfg.dbg
            if run("prep"):
                conv_bf16(I["w_in"][l], wb_in, D, IN_W, "wb_in")
            if run("mod"):
                stage_mod(l)
            if run("norm1"):
                stage_norm(xcur, 0)
            if run("proj"):
                stage_proj()
            if run("ssd"):
                stage_ssd_conv(l)
                stage_ssd_tok()
                stage_ssd_scan(l, 0)
                stage_ssd_scan(l, 1)
            if run("gla"):
                stage_gla(l, 0)
                stage_gla(l, 1)
            if run("attn"):
                stage_qknorm(l)
                stage_attn(l, "na")
                stage_attn(l, "swa")
            if run("merge"):
                conv_bf16(I["w_branch"][l], wb_br, 4096, D, "wb_br")
                conv_bf16(I["w_out"][l], wb_out, D, D, "wb_out")
                stage_merge()
                stage_outproj(xcur, xs[1])
            if run("peer"):
                stage_peer_prep(l)
                stage_norm(xs[1], 1)
                stage_peer_q()
                stage_peer_topk(l)
                if last:
                    stage_peer_main(xs[1], y, TC, off=TC)
                else:
                    xnext = xs[2] if xcur is xs[0] else xs[0]
                    stage_peer_main(xs[1], xnext, 0)
                    xcur = xnext
            if cfg.dbg and l == cfg.dbg_layer:
                break

        for name in cfg.dbg:
            src = {"hT": hT, "PT": PTa, "PK": PK, "modd": modd, "aT": aT, "xcT": xcT, "yfT": yfT, "cT": cT, "ofT": ofT, "bT": bT, "dT": dT, "nqk": nqk, "sqk": sqk, "x1": xs[1], "x2": xs[2], "mTd": mTd, "qTd": qTd, "sc_d": sc_d, "thr_d": thr_d}[name]
            o = dout("dbg_" + name, list(src.shape), src.dtype)
            dbg_out[name] = o
            R = src.shape[0]
            step = max(1, (32 << 20) // (src.shape[1] * 4))
            for r in range(0, R, step):
                n = min(step, R - r)
                kb.dma(o[r:r + n, :], src[r:r + n, :], reads=["dram_" + name if name != "modd" else "modd"], writes=["out_" + name])
        if cfg.dbg or cfg.stages is not None:
            kb.dma(y[:, :], xs[0][TC:T, :], reads=["dram_x"], writes=["y"])
        kb.barrier()
        print("build: ninst", kb.ninst, "nwait", kb.nwait)
        print("cnt", kb.tot, "nsem", kb.nsem)
    return nc


def host_inputs(cfg, inp, b):
    L = cfg.L
    f = lambda a: np.ascontiguousarray(a, dtype=np.float32)
    m = {}
    m["x"] = f(inp["x"][b])
    m["ctx"] = f(inp["ctx"][b])
    cc = np.stack([inp["c"][b], inp["c_ctx"]], axis=-1)
    m["cc"] = f(cc.reshape(NKC, 128, 2).transpose(1, 0, 2).reshape(128, 32))
    m["w_ada"] = f(inp["w_ada"])
    m["bada"] = f(inp["b_ada"].reshape(L, 96, 128).transpose(0, 2, 1))
    gn = np.concatenate([inp["g_norm1"].reshape(L, 16, 128).transpose(0, 2, 1),
                         inp["g_norm2"].reshape(L, 16, 128).transpose(0, 2, 1)], axis=2)
    m["gn"] = f(gn)
    m["w_in"] = f(inp["w_in"])
    m["ssd_cw"] = f(inp["ssd_conv_w"].reshape(L, 5, 16, 128).transpose(0, 3, 2, 1))
    m["ssd_cb"] = f(inp["ssd_conv_b"].reshape(L, 16, 128).transpose(0, 2, 1))
    m["ssd_row"] = f(np.concatenate([inp["ssd_a_log"].reshape(L, 32), inp["ssd_dt_bias"].reshape(L, 32)], axis=1).reshape(L, 1, 64))
    dch = np.repeat(inp["ssd_d"], 64, axis=1).reshape(L, 8, 128).transpose(0, 2, 1)
    ng = inp["ssd_norm_g"].reshape(L, 8, 128).transpose(0, 2, 1)
    m["ssd_dg"] = f(np.concatenate([dch, ng], axis=2))
    t2_ = lambda a: np.tile(a, (1, 2))
    m["att_g"] = f(np.stack([t2_(inp["na_q_norm"]), t2_(inp["na_k_norm"]), t2_(inp["swa_q_norm"]), t2_(inp["swa_k_norm"])], axis=2))
    m["esink"] = f(np.broadcast_to(inp["swa_sink"][:, None, :], (L, 64, 16)))
    m["rope"] = rope_tables(cfg.TL)
    m["pm"] = rot_matrix()
    m["na_bias"] = f(np.stack([na_bias_tables(cfg.TL, inp["na_rpb"][l]) for l in range(L)], 0))
    m["w_branch"] = f(inp["w_branch"].reshape(L, 4096, D))
    m["w_out"] = f(inp["w_out"])
    m["peer_wq"] = f(inp["peer_wq"])
    m["peer_kk"] = f(np.stack([inp["peer_k1"].transpose(0, 2, 1), inp["peer_k2"].transpose(0, 2, 1)], axis=2))
    m["peer_u"] = f(inp["peer_u"])
    m["peer_v"] = f(inp["peer_v"])
    m["gla_wg"] = f(inp["gla_w_gate"])
    m["gla_bg"] = f(inp["gla_b_gate"].reshape(L, 2, 1, 512))
    m["gla_ng"] = f(inp["gla_norm_g"].reshape(L, 2, 128).transpose(0, 2, 1))
    return m


def kernel(**inputs):
    cfg = Cfg()
    nc = build(cfg)
    B = inputs["x"].shape[0]
    in_maps = [host_inputs(cfg, inputs, b) for b in range(B)]
    res = run_bass_kernel_spmd(nc, in_maps, core_ids=list(range(B)))
    return np.stack([res.results[b]["y"] for b in range(B)], axis=0).astype(np.float32)
```

```python
import numpy as np
from contextlib import ExitStack
import concourse.bass as bass
import concourse.mybir as mybir
from concourse.bass_utils import run_bass_kernel_spmd

F32 = mybir.dt.float32
BF16 = mybir.dt.bfloat16
U32 = mybir.dt.uint32
AF = mybir.ActivationFunctionType
ALU = mybir.AluOpType
AX = mybir.AxisListType

D = 2048
NKC = 16
IN_W = 19008
O_Z, O_XBC, O_DT, O_NA = 0, 1024, 3072, 3104
O_GQ, O_GK, O_GV, O_GR, O_GLR = 6176, 6688, 7200, 8224, 9248
O_SQ, O_SK, O_SV, O_GATE = 9280, 10304, 10560, 10816
EPS = 1e-6
NEG = -30000.0


class Buf:
    __slots__ = ("name", "w", "r")

    def __init__(self, name):
        self.name = name
        self.w = None
        self.r = {}


class KB:
    NDMA = 12
    LIMIT = 24000

    def __init__(self, nc, es, dma_queues=("sp",)):
        self.nc = nc
        self.es = es
        self.eng = {"pe": nc.tensor, "act": nc.scalar, "dve": nc.vector, "pool": nc.gpsimd, "sp": nc.sync}
        self.sem = {}
        self.cnt = {}
        self.cur = {}
        self.pending = []
        self.nsem = 0
        for k in self.eng:
            self._fresh(k)
        self.dq = {}
        for q in dma_queues:
            sl = []
            for i in range(self.NDMA):
                sl.append(self._fresh(("dma", q, i)))
            self.dq[q] = [sl, 0]
        self.seen = {k: {} for k in self.eng}
        self.bufs = {}
        self.nwait = 0
        self.ninst = 0
        self.tot = {k: 0 for k in self.eng}
        self.store_q = ("act", "pool")

    def _fresh(self, base):
        if base in self.cur:
            self.pending.append(self.cur[base])
        self.nsem += 1
        key = (base, self.nsem)
        self.sem[key] = self.es.enter_context(self.nc.semaphore("s%d" % self.nsem))
        self.cnt[key] = 0
        self.cur[base] = key
        return key

    def buf(self, name):
        b = self.bufs.get(name)
        if b is None:
            b = Buf(name)
            self.bufs[name] = b
        return b

    def _wait(self, e, key, val):
        if key[0] == e and e in ("pe", "sp"):
            return
        s = self.seen[e]
        if s.get(key, 0) >= val:
            return
        self.eng[e].wait_ge(self.sem[key], val)
        s[key] = val
        self.nwait += 1

    def _deps(self, e, reads, writes):
        for b in reads:
            if b.w is not None:
                self._wait(e, b.w[0], b.w[1])
        for b in writes:
            if b.w is not None:
                self._wait(e, b.w[0], b.w[1])
            for k, v in b.r.items():
                self._wait(e, k, v)

    def _record(self, tok, reads, writes):
        for b in reads:
            if b.r.get(tok[0], 0) < tok[1]:
                b.r[tok[0]] = tok[1]
        for b in writes:
            b.w = tok
            b.r = {}

    def _bl(self, xs):
        return [self.buf(x) if isinstance(x, str) else x for x in xs]

    def op(self, e, fn, reads=(), writes=()):
        reads = self._bl(reads)
        writes = self._bl(writes)
        self._deps(e, reads, writes)
        key = self.cur[e]
        if self.cnt[key] >= self.LIMIT:
            key = self._fresh(e)
        inst = fn(self.eng[e])
        self.cnt[key] += 1
        self.tot[e] += 1
        inst.then_inc(self.sem[key], 1)
        self._record((key, self.cnt[key]), reads, writes)
        self.ninst += 1
        return inst

    def dma(self, out, in_, reads=(), writes=(), q=None, **kw):
        is_store = any(isinstance(x, str) and (x.startswith("dram_") or x.startswith("out_") or x in ("modd", "y")) for x in writes)
        reads = self._bl([x for x in reads if not (isinstance(x, str) and (x.startswith("dram_") or x == "modd"))])
        writes = self._bl([x for x in writes if not (isinstance(x, str) and (x.startswith("dram_") or x.startswith("out_") or x in ("modd", "y")))])
        if q is None:
            q = "sp"
            if is_store and reads and reads[0].w is not None and reads[0].w[0][0] in self.store_q:
                q = reads[0].w[0][0]
        if q not in self.dq:
            self.dq[q] = [[self._fresh(("dma", q, i)) for i in range(self.NDMA)], 0]
        sl, i = self.dq[q]
        slot = i % self.NDMA
        key = sl[slot]
        self.dq[q][1] = i + 1
        if self.cnt[key] > 0:
            self._wait(q, key, self.cnt[key])
        if self.cnt[key] >= self.LIMIT:
            key = self._fresh(("dma", q, slot))
            sl[slot] = key
        self._deps(q, reads, writes)
        inst = self.eng[q].dma_start(out=out, in_=in_, **kw)
        self.cnt[key] += 16
        inst.then_inc(self.sem[key], 16)
        self._record((key, self.cnt[key]), reads, writes)
        self.ninst += 1
        return inst

    def barrier(self):
        keys = list(self.pending) + list(self.cur.values())
        self.pending = []
        for e in self.eng:
            for key in keys:
                if self.cnt[key] > 0:
                    self._wait(e, key, self.cnt[key])
        live = set(self.cur.values())
        for b in self.bufs.values():
            if b.w is not None and b.w[0] not in live:
                b.w = None
            b.r = {k: v for k, v in b.r.items() if k in live}


class Rot:
    def __init__(self, st, alloc, name, n, shape, dt):
        self.items = [(st.enter_context(alloc(name + str(i), shape, dt)), name + str(i)) for i in range(n)]
        self.i = 0

    def next(self):
        it = self.items[self.i % len(self.items)]
        self.i += 1
        return it


class Cfg:
    def __init__(self, TL=4096, TC=256, L=2, stages=None, dbg=(), dumps=False, dbg_layer=0, ntg=4):
        self.ntg = ntg
        self.dbg_layer = dbg_layer
        self.dumps = dumps
        self.TL, self.TC, self.L = TL, TC, L
        self.T = TL + TC
        self.stages = stages
        self.dbg = tuple(dbg)

    def blocks(self, t0=0, t1=None):
        t1 = self.T if t1 is None else t1
        out = []
        if t0 < self.TC:
            out.append((0, self.TC))
            t0 = self.TC
        t = t0
        while t < t1:
            out.append((t, min(512, t1 - t)))
            t += 512
        return out


def proj_groups():
    g = []

    def fm(a, b):
        c = a
        while c < b:
            w = min(512, b - c)
            g.append((c, w, "f"))
            c += w

    def tm(a, b):
        c = a
        while c < b:
            w = min(512, b - c)
            g.append((c, w, "t"))
            c += w

    fm(O_Z, O_DT)
    tm(O_DT, O_NA)
    fm(O_NA, O_NA + 2048)
    tm(O_NA + 2048, O_GQ)
    fm(O_GQ, O_GV)
    tm(O_GK, O_GR)
    fm(O_GR, O_SV)
    tm(O_SV, O_GATE)
    fm(O_GATE, IN_W)
    return g


def na_classes(TL):
    R_ = TL // 64
    wr = min(8, R_)
    cls, idx = [], []
    for ti in range(TL // 128):
        ra, rb = 2 * ti, 2 * ti + 1
        r0a = min(max(ra - wr // 2, 0), R_ - wr)
        r0b = min(max(rb - wr // 2, 0), R_ - wr)
        kt_lo, kt_hi = r0a // 2, (r0b + wr - 1) // 2
        key = (r0a - 2 * ti, r0b - 2 * ti, kt_lo - ti, kt_hi - kt_lo + 1)
        if key not in cls:
            cls.append(key)
        idx.append(cls.index(key))
    return cls, idx


def na_bias_tables(TL, rpb):
    cls, _ = na_classes(TL)
    out = np.full((len(cls), 128, 16, 5, 128), NEG, np.float32)
    j = np.arange(128)[:, None]
    i = np.arange(128)[None, :]
    cq = i % 64
    ck = j % 64
    cstart = np.clip(cq - 8, 0, 48)
    colok = (ck >= cstart) & (ck < cstart + 16)
    crel = ck - cq + 15
    for ci, (off_a, off_b, klo_rel, nsl) in enumerate(cls):
        rq = i // 64
        r0 = np.where(rq == 0, off_a, off_b + 0)
        for sl in range(nsl):
            rk = 2 * (klo_rel + sl) + j // 64
            rowok = (rk >= r0) & (rk < r0 + 8)
            rrel = rk - rq + 7
            ok = rowok & colok & (rrel >= 0) & (rrel < 15)
            rr = np.clip(rrel, 0, 14)
            cc = np.clip(crel, 0, 30)
            for h in range(16):
                out[ci, :, h, sl, :] = np.where(ok, rpb[h][rr, cc], NEG)
    return out.reshape(len(cls), 128, 16 * 5 * 128)


def rope_tables(TL):
    t = np.arange(TL)
    row = (t // 64).astype(np.float32)
    col = (t % 64).astype(np.float32)
    nf = 16
    inv = (10000.0 ** (-np.arange(nf, dtype=np.float32) / nf)).astype(np.float32)
    ar = row[None, :] * inv[:, None]
    ac = col[None, :] * inv[:, None]
    cos = np.concatenate([np.cos(ar), np.cos(ar), np.cos(ac), np.cos(ac)], 0)
    sin = np.concatenate([np.sin(ar), np.sin(ar), np.sin(ac), np.sin(ac)], 0)
    tab = np.stack([np.concatenate([cos, cos], 0), np.concatenate([sin, sin], 0)], 0)
    return tab.astype(np.float32)


def rot_matrix():
    pm = np.zeros((128, 128), np.float32)
    for base in (0, 64):
        for a in (0, 32):
            for i in range(16):
                pm[base + a + 16 + i, base + a + i] = -1.0
                pm[base + a + i, base + a + 16 + i] = 1.0
    return pm


def build(cfg):
    TL, TC, T, L = cfg.TL, cfg.TC, cfg.T, cfg.L
    NT = T // 128
    nc = bass.Bass("TRN2", target_bir_lowering=False)

    def din(name, shape, dt=F32):
        return nc.dram_tensor(name, list(shape), dt, kind="ExternalInput").ap()

    def dscr(name, shape, dt=F32):
        return nc.dram_tensor(name, list(shape), dt, kind="Internal").ap()

    def dout(name, shape, dt=F32):
        return nc.dram_tensor(name, list(shape), dt, kind="ExternalOutput").ap()

    I = {}
    I["x"] = din("x", [TL, D])
    I["ctx"] = din("ctx", [TC, D])
    I["cc"] = din("cc", [128, 32])
    I["w_ada"] = din("w_ada", [L, D, 6 * D])
    I["bada"] = din("bada", [L, 128, 96])
    I["gn"] = din("gn", [L, 128, 32])
    I["w_in"] = din("w_in", [L, D, IN_W])
    I["ssd_cw"] = din("ssd_cw", [L, 128, 16, 5])
    I["ssd_cb"] = din("ssd_cb", [L, 128, 16])
    I["ssd_row"] = din("ssd_row", [L, 1, 64])
    I["ssd_dg"] = din("ssd_dg", [L, 128, 16])
    I["gla_wg"] = din("gla_wg", [L, 2, 16, 512])
    I["gla_bg"] = din("gla_bg", [L, 2, 1, 512])
    I["gla_ng"] = din("gla_ng", [L, 128, 2])
    I["att_g"] = din("att_g", [L, 128, 4])
    I["esink"] = din("esink", [L, 64, 16])
    I["rope"] = din("rope", [2, 128, TL])
    I["pm"] = din("pm", [128, 128])
    NCLS = len(na_classes(TL)[0])
    I["na_bias"] = din("na_bias", [L, NCLS, 128, 16 * 5 * 128])
    I["w_branch"] = din("w_branch", [L, 4096, D])
    I["w_out"] = din("w_out", [L, D, D])
    I["peer_wq"] = din("peer_wq", [L, D, D])
    I["peer_kk"] = din("peer_kk", [L, 128, 2, 128])
    I["peer_u"] = din("peer_u", [L, 16384, D])
    I["peer_v"] = din("peer_v", [L, 16384, D])
    y = dout("y", [TL, D])
    dbg_out = {}

    xs = [dscr("xs%d" % i, [T, D]) for i in range(3)]
    hT = dscr("hT", [D, T], BF16)
    PTa = dscr("PTa", [O_GATE, T])
    PTb = dscr("PTb", [IN_W - O_GATE, T])
    PK = dscr("PK", [T, O_GATE])

    class _PT:
        def __getitem__(self, key):
            rs, cs_ = key
            if rs.stop <= O_GATE:
                return PTa[rs, cs_]
            assert rs.start >= O_GATE
            return PTb[rs.start - O_GATE:rs.stop - O_GATE, cs_]
    PT = _PT()
    modd = dscr("modd", [2, 96 * 128])
    xcT = dscr("xcT", [2048, T], BF16)
    xtok = dscr("xtok", [T, 1536], BF16)
    yfT = dscr("yfT", [1024, T])
    aT = dscr("aT", [1024, T], BF16)
    nqk = dscr("nqk", [2048, T], BF16)
    sqk = dscr("sqk", [1280, T], BF16)
    vtok = dscr("vtok", [T, 1280], BF16)
    bT = dscr("bT", [1024, T], BF16)
    dT = dscr("dT", [1024, T], BF16)
    wb_br = dscr("wb_br", [4096, D], BF16)
    wb_out = dscr("wb_out", [D, D], BF16)
    wb_q = dscr("wb_q", [D, D], BF16)
    mTd = dscr("mTd", [D, T], BF16)
    qTd = dscr("qTd", [D, T])
    sc_d = dscr("sc_d", [T, 2048])
    thr_d = dscr("thr_d", [T, 16])
    UTd = dscr("UTd", [32, 128, 16 * 512], BF16)
    Vd = dscr("Vd", [16384, D], BF16)
    ofT = dscr("ofT", [1024, T])
    cT = dscr("cT", [1024, T], BF16)
    wb_in = dscr("wb_in", [D, IN_W], BF16)

    with ExitStack() as es:
        kb = KB(nc, es)
        uid = [0]

        def sb(name, shape, dt):
            uid[0] += 1
            return nc.sbuf_tensor("%s_u%d" % (name, uid[0]), shape, dt)

        def ps(name, shape, dt):
            uid[0] += 1
            return nc.psum_tensor("%s_u%d" % (name, uid[0]), shape, dt)

        ident_b = es.enter_context(sb("ident_b", [128, 128], BF16))
        ident_f = es.enter_context(sb("ident_f", [128, 128], F32))
        modt = es.enter_context(sb("modt", [128, 2, 96], F32))
        tabs = es.enter_context(sb("tabs", [128, 2, 6, 16], F32))
        gnt = es.enter_context(sb("gnt", [128, 32], F32))
        kb.op("pool", lambda e: e.memset(ident_f[:], 0.0), writes=["ident_f"])
        kb.op("pool", lambda e: e.affine_select(out=ident_f[:], in_=ident_f[:], pattern=[[-1, 128]],
                                                compare_op=ALU.not_equal, fill=1.0, base=0, channel_multiplier=1),
              reads=["ident_f"], writes=["ident_f"])
        kb.op("dve", lambda e: e.tensor_copy(out=ident_b[:], in_=ident_f[:]), reads=["ident_f"], writes=["ident_b"])

        ones_f = es.enter_context(sb("ones_f", [128, 128], F32))
        ones_b = es.enter_context(sb("ones_b", [128, 128], BF16))
        tri = [es.enter_context(sb("tri%d" % d, [128, 128], F32)) for d in range(2)]
        negm = [es.enter_context(sb("negm%d" % d, [128, 128], F32)) for d in range(2)]
        kcon = es.enter_context(sb("kcon", [128, 4], F32))
        kb.op("pool", lambda e: e.memset(kcon[:, 0:1], 1.0), writes=["kcon"])
        kb.op("pool", lambda e: e.memset(kcon[:, 1:2], EPS), writes=["kcon"])
        kb.op("pool", lambda e: e.memset(kcon[:, 2:4], 0.0), writes=["kcon"])
        kb.op("pool", lambda e: e.memset(kcon[:, 3:4], 64.0 * EPS), writes=["kcon"])
        blk64 = es.enter_context(sb("blk64", [128, 128], BF16))
        negm_b = [es.enter_context(sb("negm_b%d" % d, [128, 128], BF16)) for d in range(2)]
        kb.op("pool", lambda e: e.memset(blk64[:], 0.0), writes=["blk64"])
        kb.op("pool", lambda e: e.memset(blk64[0:64, 0:64], 1.0), writes=["blk64"])
        kb.op("pool", lambda e: e.memset(blk64[64:128, 64:128], 1.0), writes=["blk64"])
        C_ONE = kcon[:, 0:1]
        C_EPS = kcon[:, 1:2]
        kb.op("pool", lambda e: e.memset(ones_f[:], 1.0), writes=["ones_f"])
        kb.op("pool", lambda e: e.memset(ones_b[:], 1.0), writes=["ones_b"])
        for d in range(2):
            sgn = 1 if d == 0 else -1
            kb.op("pool", lambda e: e.affine_select(out=tri[d][:], in_=ones_f[:], pattern=[[sgn, 128]], compare_op=ALU.is_ge,
                                                    fill=0.0, base=0, channel_multiplier=-sgn), reads=["ones_f"], writes=["tri"])
            kb.op("pool", lambda e: e.memset(negm[d][:], 0.0), writes=["negm"])
            kb.op("pool", lambda e: e.affine_select(out=negm[d][:], in_=negm[d][:], pattern=[[sgn, 128]], compare_op=ALU.is_ge,
                                                    fill=NEG, base=0, channel_multiplier=-sgn), reads=["negm"], writes=["negm"])

        def dump(name, ap, reads):
            if name in dbg_out or not cfg.dumps:
                return
            shp = [ap.shape[0], int(np.prod(ap.shape[1:]))]
            o = dout("dbg_" + name, shp, ap.dtype)
            dbg_out[name] = o
            src = ap
            if len(ap.shape) == 3:
                src = ap.rearrange("p a b -> p (a b)")
            kb.dma(o[:, :], src, reads=reads, writes=["out_" + name])

        for d in range(2):
            kb.op("dve", lambda e: e.tensor_copy(out=negm_b[d][:], in_=negm[d][:]), reads=["negm"], writes=["negm_b"])

        def run(stage):
            return cfg.stages is None or stage in cfg.stages

        def conv_bf16(src, dst, R, C, tag):
            with ExitStack() as st:
                fin = Rot(st, sb, "cv_in_" + tag, 2, [128, 4096], F32)
                fo = Rot(st, sb, "cv_out_" + tag, 2, [128, 4096], BF16)
                k = 0
                for r in range(R // 128):
                    c = 0
                    while c < C:
                        w = min(4096, C - c)
                        ti, tn = fin.next()
                        to, on = fo.next()
                        kb.dma(ti[:, 0:w], src[r * 128:(r + 1) * 128, c:c + w], writes=[tn])
                        eng = ("dve", "pool", "act")[k % 3]
                        if eng == "act":
                            kb.op("act", lambda e: e.copy(out=to[:, 0:w], in_=ti[:, 0:w]), reads=[tn], writes=[on])
                        else:
                            kb.op(eng, lambda e: e.tensor_copy(out=to[:, 0:w], in_=ti[:, 0:w]), reads=[tn], writes=[on])
                        kb.dma(dst[r * 128:(r + 1) * 128, c:c + w], to[:, 0:w], reads=[on], writes=["dram_" + tag])
                        c += w
                        k += 1
                kb.barrier()

        def stage_mod(l):
            with ExitStack() as st:
                cct = st.enter_context(sb("cct", [128, 32], F32))
                scs = st.enter_context(sb("scs", [128, 32], F32))
                badat = st.enter_context(sb("badat", [128, 96], F32))
                wt = Rot(st, sb, "adaw", 2, [128, 16, 128], F32)
                mp = st.enter_context(ps("modp", [128, 96, 2], F32))
                tp = st.enter_context(ps("modtp", [96, 2, 128], F32))
                mrow = st.enter_context(sb("mrow", [96, 2, 128], F32))
                kb.dma(cct[:], I["cc"][:, :], writes=["cct"])
                kb.dma(badat[:], I["bada"][l], writes=["badat"])
                kb.dma(gnt[:], I["gn"][l], writes=["gnt"])
                kb.op("act", lambda e: e.activation(out=scs[:], in_=cct[:], func=AF.Silu), reads=["cct"], writes=["scs"])
                for ch in range(96):
                    w, wn = wt.next()
                    kb.dma(w[:], I["w_ada"][l, :, ch * 128:(ch + 1) * 128].rearrange("(kc p) f -> p kc f", p=128),
                           writes=[wn])
                    for kc in range(NKC):
                        kb.op("pe", lambda e: e.matmul(mp[:, ch, :], lhsT=w[:, kc, :], rhs=scs[:, kc * 2:kc * 2 + 2],
                                                       start=(kc == 0), stop=(kc == NKC - 1)),
                              reads=[wn, "scs"], writes=["modp"])
                for j in range(2):
                    kb.op("dve", lambda e: e.tensor_tensor(out=modt[:, j, :], in0=mp[:, :, j], in1=badat[:], op=ALU.add),
                          reads=["modp", "badat"], writes=["modt"])
                for j in range(2):
                    for (dst, sc_ch, gcol) in ((0, 16, 0), (3, 64, 16)):
                        kb.op("dve", lambda e: e.scalar_tensor_tensor(out=tabs[:, j, dst, :], in0=modt[:, j, sc_ch:sc_ch + 16],
                                                                       scalar=1.0, in1=gnt[:, gcol:gcol + 16],
                                                                       op0=ALU.add, op1=ALU.mult),
                              reads=["modt", "gnt"], writes=["tabs"])
                    for (dst, ch0) in ((1, 0), (2, 32), (4, 48), (5, 80)):
                        kb.op("dve", lambda e: e.tensor_copy(out=tabs[:, j, dst, :], in_=modt[:, j, ch0:ch0 + 16]),
                              reads=["modt"], writes=["tabs"])
                for j in range(2):
                    kb.op("pe", lambda e: e.transpose(out=tp[:, j, :], in_=modt[:, j, :], identity=ident_f[:]),
                          reads=["modt", "ident_f"], writes=["modtp"])
                kb.op("act", lambda e: e.copy(out=mrow[:], in_=tp[:]), reads=["modtp"], writes=["mrow"])
                for j in range(2):
                    kb.dma(modd[j].rearrange("(ch p) -> ch p", p=128), mrow[:, j, :], reads=["mrow"], writes=["modd"])
                kb.barrier()

        def stage_norm(src, which):
            si, bi = (0, 1) if which == 0 else (3, 4)
            with ExitStack() as st:
                xin = Rot(st, sb, "n_x", 2, [128, D], F32)
                junk = st.enter_context(sb("n_junk", [128, D], F32))
                xn = Rot(st, sb, "n_xn", 2, [128, D], BF16)
                stat = Rot(st, sb, "n_stat", 2, [128, 4], F32)
                hb = Rot(st, sb, "n_hb", 2, [128, NKC, 512], BF16)
                tps = Rot(st, ps, "n_tp", 2, [128, 8, 128], BF16)
                for (b0, bn) in cfg.blocks():
                    j = 1 if b0 < TC else 0
                    hblk, hn = hb.next()
                    for ti in range(bn // 128):
                        t0 = b0 + ti * 128
                        x, xname = xin.next()
                        s, sname = stat.next()
                        xb, xbn = xn.next()
                        kb.dma(x[:], src[t0:t0 + 128, :], reads=["dram_x"], writes=[xname])
                        kb.op("act", lambda e: e.activation(out=junk[:], in_=x[:], func=AF.Square),
                              reads=[xname], writes=["n_junk"])
                        kb.op("dve", lambda e: e.tensor_reduce(out=s[:, 0:1], in_=junk[:], axis=AX.X, op=ALU.add),
                              reads=["n_junk"], writes=[sname])
                        kb.op("act", lambda e: e.activation(out=s[:, 1:2], in_=s[:, 0:1], func=AF.Sqrt, scale=1.0 / D, bias=C_EPS),
                              reads=[sname, "kcon"], writes=[sname])
                        kb.op("dve", lambda e: e.reciprocal(out=s[:, 2:3], in_=s[:, 1:2]), reads=[sname], writes=[sname])
                        kb.op("dve", lambda e: e.tensor_scalar(out=xb[:], in0=x[:], scalar1=s[:, 2:3], scalar2=None, op0=ALU.mult),
                              reads=[xname, sname], writes=[xbn])
                        for half in range(2):
                            tp, tpn = tps.next()
                            for q in range(8):
                                kc = half * 8 + q
                                kb.op("pe", lambda e: e.transpose(out=tp[:, q, :], in_=xb[:, kc * 128:(kc + 1) * 128], identity=ident_b[:]),
                                      reads=[xbn, "ident_b"], writes=[tpn])
                            for q in range(8):
                                kc = half * 8 + q
                                if q % 2 == 0:
                                    kb.op("act", lambda e: e.activation(out=hblk[:, kc, ti * 128:(ti + 1) * 128], in_=tp[:, q, :],
                                                                        func=AF.Identity, scale=tabs[:, j, si, kc:kc + 1],
                                                                        bias=tabs[:, j, bi, kc:kc + 1]),
                                          reads=[tpn, "tabs"], writes=[hn])
                                else:
                                    kb.op("dve", lambda e: e.tensor_scalar(out=hblk[:, kc, ti * 128:(ti + 1) * 128], in0=tp[:, q, :],
                                                                           scalar1=tabs[:, j, si, kc:kc + 1], scalar2=tabs[:, j, bi, kc:kc + 1],
                                                                           op0=ALU.mult, op1=ALU.add),
                                          reads=[tpn, "tabs"], writes=[hn])
                    kb.dma(hT[:, b0:b0 + bn].rearrange("(kc p) t -> p kc t", p=128), hblk[:, :, 0:bn], reads=[hn], writes=["dram_hT"])
                kb.barrier()

        def stage_proj():
            groups = proj_groups()
            with ExitStack() as st:
                hb = Rot(st, sb, "p_hb", 2, [128, NKC, 512], BF16)
                wt = Rot(st, sb, "p_w", 3, [128, NKC, 512], BF16)
                pp = Rot(st, ps, "p_ps", 4, [128, 512], F32)
                og = Rot(st, sb, "p_o", 4, [128, 512], F32)
                k = 0
                for (b0, bn) in cfg.blocks():
                    hblk, hn = hb.next()
                    kb.dma(hblk[:, :, 0:bn], hT[:, b0:b0 + bn].rearrange("(kc p) t -> p kc t", p=128), reads=["dram_hT"], writes=[hn])
                    for (c0, cw, mode) in groups:
                        w, wn = wt.next()
                        kb.dma(w[:, :, 0:cw], wb_in[:, c0:c0 + cw].rearrange("(kc p) c -> p kc c", p=128), reads=["dram_wb_in"], writes=[wn])
                        if mode == "f":
                            c = 0
                            while c < cw:
                                m = min(128, cw - c)
                                p, pn = pp.next()
                                o, on = og.next()
                                for kc in range(NKC):
                                    kb.op("pe", lambda e: e.matmul(p[0:m, 0:bn], lhsT=w[:, kc, c:c + m], rhs=hblk[:, kc, 0:bn],
                                                                   start=(kc == 0), stop=(kc == NKC - 1)),
                                          reads=[wn, hn], writes=[pn])
                                if k % 2 == 0:
                                    kb.op("act", lambda e: e.copy(out=o[0:m, 0:bn], in_=p[0:m, 0:bn]), reads=[pn], writes=[on])
                                else:
                                    kb.op("dve", lambda e: e.tensor_copy(out=o[0:m, 0:bn], in_=p[0:m, 0:bn]), reads=[pn], writes=[on])
                                k += 1
                                kb.dma(PT[c0 + c:c0 + c + m, b0:b0 + bn], o[0:m, 0:bn], reads=[on], writes=["dram_PT"])
                                c += m
                        else:
                            for ti in range(bn // 128):
                                p, pn = pp.next()
                                o, on = og.next()
                                for kc in range(NKC):
                                    kb.op("pe", lambda e: e.matmul(p[:, 0:cw], lhsT=hblk[:, kc, ti * 128:(ti + 1) * 128], rhs=w[:, kc, 0:cw],
                                                                   start=(kc == 0), stop=(kc == NKC - 1)),
                                          reads=[wn, hn], writes=[pn])
                                if k % 2 == 0:
                                    kb.op("act", lambda e: e.copy(out=o[:, 0:cw], in_=p[:, 0:cw]), reads=[pn], writes=[on])
                                else:
                                    kb.op("dve", lambda e: e.tensor_copy(out=o[:, 0:cw], in_=p[:, 0:cw]), reads=[pn], writes=[on])
                                k += 1
                                kb.dma(PK[b0 + ti * 128:b0 + (ti + 1) * 128, c0:c0 + cw], o[:, 0:cw], reads=[on], writes=["dram_PK"])
                kb.barrier()

        def seg_chunks():
            return [(0, TC)], [(TC, T)]

        def stage_ssd_conv(l):
            with ExitStack() as st:
                cw = st.enter_context(sb("s_cw", [128, 16, 5], F32))
                cb = st.enter_context(sb("s_cb", [128, 16], F32))
                xin = Rot(st, sb, "s_xin", 3, [128, 516], F32)
                acc = Rot(st, sb, "s_acc", 2, [128, 512], F32)
                ob = Rot(st, sb, "s_ob", 3, [128, 512], BF16)
                kb.dma(cw[:], I["ssd_cw"][l], writes=["s_cw"])
                kb.dma(cb[:], I["ssd_cb"][l], writes=["s_cb"])
                for (b0, bn) in cfg.blocks():
                    s0, s1 = (0, TC) if b0 < TC else (TC, T)
                    for ch in range(16):
                        x, xn_ = xin.next()
                        a, an = acc.next()
                        o, on = ob.next()
                        lo = max(s0, b0 - 2)
                        hi = min(s1, b0 + bn + 2)
                        if lo > b0 - 2:
                            kb.op("pool", lambda e: e.memset(x[:, 0:2], 0.0), writes=[xn_])
                        if hi < b0 + bn + 2:
                            kb.op("pool", lambda e: e.memset(x[:, bn + 2:bn + 4], 0.0), writes=[xn_])
                        kb.dma(x[:, lo - (b0 - 2):hi - (b0 - 2)], PT[O_XBC + ch * 128:O_XBC + (ch + 1) * 128, lo:hi],
                               reads=["dram_PT"], writes=[xn_])
                        kb.op("dve", lambda e: e.tensor_scalar(out=a[:, 0:bn], in0=x[:, 0:bn], scalar1=cw[:, ch, 0:1], scalar2=None, op0=ALU.mult),
                              reads=[xn_, "s_cw"], writes=[an])
                        for k in range(1, 5):
                            kb.op("dve", lambda e: e.scalar_tensor_tensor(out=a[:, 0:bn], in0=x[:, k:k + bn], scalar=cw[:, ch, k:k + 1],
                                                                           in1=a[:, 0:bn], op0=ALU.mult, op1=ALU.add),
                                  reads=[xn_, "s_cw", an], writes=[an])
                        kb.op("act", lambda e: e.activation(out=o[:, 0:bn], in_=a[:, 0:bn], func=AF.Silu, bias=cb[:, ch:ch + 1], scale=1.0),
                              reads=[an, "s_cb"], writes=[on])
                        kb.dma(xcT[ch * 128:(ch + 1) * 128, b0:b0 + bn], o[:, 0:bn], reads=[on], writes=["dram_xcT"])
                kb.barrier()

        def stage_ssd_tok():
            with ExitStack() as st:
                xi = Rot(st, sb, "st_in", 2, [128, 12, 128], BF16)
                tp = Rot(st, ps, "st_tp", 2, [128, 12, 128], BF16)
                xo = Rot(st, sb, "st_o", 2, [128, 12, 128], BF16)
                for ti in range(NT):
                    t0 = ti * 128
                    a, an = xi.next()
                    p, pn = tp.next()
                    o, on = xo.next()
                    kb.dma(a[:], xcT[0:1536, t0:t0 + 128].rearrange("(c p) t -> p c t", p=128), reads=["dram_xcT"], writes=[an])
                    for c in range(12):
                        kb.op("pe", lambda e: e.transpose(out=p[:, c, :], in_=a[:, c, :], identity=ident_b[:]), reads=[an, "ident_b"], writes=[pn])
                    if ti % 2 == 0:
                        kb.op("act", lambda e: e.copy(out=o[:], in_=p[:]), reads=[pn], writes=[on])
                    else:
                        kb.op("dve", lambda e: e.tensor_copy(out=o[:], in_=p[:]), reads=[pn], writes=[on])
                    kb.dma(xtok[t0:t0 + 128, :], o[:].rearrange("p c t -> p (c t)"), reads=[on], writes=["dram_xtok"])
                kb.barrier()

        def stage_ssd_scan(l, d):
            nct, ncl = TC // 128, TL // 128
            if d == 0:
                order = list(range(nct)) + [nct + i for i in range(ncl)]
            else:
                order = list(range(nct - 1, -1, -1)) + [nct + i for i in range(ncl - 1, -1, -1)]
            with ExitStack() as st:
                rowb = st.enter_context(sb("sc_rowb", [128, 64], F32))
                abc = st.enter_context(sb("sc_abc", [128, 16], F32))
                dg = st.enter_context(sb("sc_dg", [128, 16], F32))
                S = st.enter_context(sb("sc_S", [128, 16, 64], F32))
                SbP = st.enter_context(sb("sc_SbP", [128, 16, 128], BF16))
                xdtP = Rot(st, sb, "sc_xdtP", 2, [128, 16, 128], BF16)
                xdtd = Rot(st, sb, "sc_xdtd", 2, [128, 16, 64], BF16)
                xt = Rot(st, sb, "sc_xt", 2, [128, 1536], BF16)
                bct = Rot(st, sb, "sc_bct", 2, [128, 8, 128], BF16)
                dtr = Rot(st, sb, "sc_dtr", 2, [128, 16], F32)
                sm = Rot(st, sb, "sc_sm", 2, [128, 8, 16], F32)
                adrep = Rot(st, sb, "sc_adrep", 2, [128, 16, 128], F32)
                cbs = Rot(st, sb, "sc_cbs", 2, [128, 128], F32)
                arg = Rot(st, sb, "sc_arg", 2, [128, 4, 128], F32)
                dm = Rot(st, sb, "sc_dm", 2, [128, 4, 128], F32)
                mt = Rot(st, sb, "sc_mt", 2, [128, 4, 128], BF16)
                ecs = Rot(st, sb, "sc_ecs", 2, [128, 4, 128], F32)
                cst = Rot(st, sb, "sc_cst", 2, [128, 4, 128], BF16)
                yo = Rot(st, sb, "sc_yo", 2, [128, 8, 128], F32)
                p_misc = st.enter_context(ps("sc_pmisc", [128, 512], F32))
                p_small = p_misc[:, 0:32]
                p_cb = p_misc[:, 128:256]
                p_n = p_misc[:, 256:384]
                p_csb = Rot(st, ps, "sc_pcsb", 2, [128, 4, 128], F32)
                p_y = st.enter_context(ps("sc_py", [128, 8, 128], F32))
                p_s = st.enter_context(ps("sc_pS", [128, 16, 64], F32))
                if d == 1:
                    yf = Rot(st, sb, "sc_yf", 2, [128, 8, 128], F32)
                    zt = Rot(st, sb, "sc_zt", 2, [128, 8, 128], F32)
                    xsT = Rot(st, sb, "sc_xsT", 2, [128, 8, 128], BF16)
                    sq = st.enter_context(sb("sc_sq", [128, 8, 128], BF16))
                    rs = st.enter_context(sb("sc_rs", [128, 128], F32))
                    ao = Rot(st, sb, "sc_ao", 2, [128, 8, 128], BF16)
                kb.dma(rowb[:], I["ssd_row"][l].to_broadcast([128, 64]), writes=["sc_rowb"])
                kb.dma(dg[:], I["ssd_dg"][l], writes=["sc_dg"])
                kb.op("act", lambda e: e.activation(out=abc[:], in_=rowb[:, d * 16:(d + 1) * 16], func=AF.Exp), reads=["sc_rowb"], writes=["sc_abc"])
                kb.op("dve", lambda e: e.tensor_scalar(out=abc[:], in0=abc[:], scalar1=-1.0, scalar2=None, op0=ALU.mult), reads=["sc_abc"], writes=["sc_abc"])
                kb.op("pool", lambda e: e.memset(S[:], 0.0), writes=["sc_S"])
                kb.op("pool", lambda e: e.memset(SbP[:], 0.0), writes=["sc_SbP"])
                for it in xdtP.items:
                    kb.op("pool", lambda e: e.memset(it[0][:], 0.0), writes=[it[1]])
                for ci in order:
                    t0 = ci * 128
                    x, xn_ = xt.next()
                    bc, bcn = bct.next()
                    dr, drn = dtr.next()
                    m, mn = sm.next()
                    xp, xpn = xdtP.next()
                    xd, xdn = xdtd.next()
                    ar, arn = adrep.next()
                    kb.dma(x[:], xtok[t0:t0 + 128, :], reads=["dram_xtok"], writes=[xn_])
                    kb.dma(bc[:], xcT[1024:2048, t0:t0 + 128].rearrange("(c p) t -> p c t", p=128), reads=["dram_xcT"], writes=[bcn])
                    kb.dma(dr[:], PK[t0:t0 + 128, O_DT + d * 16:O_DT + (d + 1) * 16], reads=["dram_PK"], writes=[drn])
                    kb.op("dve", lambda e: e.tensor_tensor(out=m[:, 0, :], in0=dr[:], in1=rowb[:, 32 + d * 16:32 + (d + 1) * 16], op=ALU.add),
                          reads=[drn, "sc_rowb"], writes=[mn])
                    kb.op("act", lambda e: e.activation(out=m[:, 0, :], in_=m[:, 0, :], func=AF.Exp), reads=[mn], writes=[mn])
                    kb.op("act", lambda e: e.activation(out=m[:, 0, :], in_=m[:, 0, :], func=AF.Ln, bias=C_ONE, scale=1.0), reads=[mn, "kcon"], writes=[mn])
                    kb.op("dve", lambda e: e.tensor_tensor(out=m[:, 1, :], in0=m[:, 0, :], in1=abc[:], op=ALU.mult), reads=[mn, "sc_abc"], writes=[mn])
                    kb.op("pe", lambda e: e.matmul(p_misc[:, 0:16], lhsT=tri[d][:], rhs=m[:, 1, :], start=True, stop=True), reads=["tri", mn], writes=["sc_psm"])
                    kb.op("pe", lambda e: e.matmul(p_misc[:, 16:32], lhsT=ones_f[:], rhs=m[:, 1, :], start=True, stop=True), reads=["ones_f", mn], writes=["sc_psm"])
                    kb.op("dve", lambda e: e.tensor_copy(out=m[:, 2:4, :], in_=p_misc[:, 0:32].rearrange("p (a h) -> p a h", a=2)), reads=["sc_psm"], writes=[mn])
                    kb.op("dve", lambda e: e.tensor_tensor(out=m[:, 4, :], in0=m[:, 3, :], in1=m[:, 2, :], op=ALU.subtract), reads=[mn], writes=[mn])
                    kb.op("act", lambda e: e.activation(out=m[:, 5:7, :], in_=m[:, 3:5, :], func=AF.Exp), reads=[mn], writes=[mn])
                    kb.op("dve", lambda e: e.tensor_tensor(out=m[:, 7, :], in0=m[:, 0, :], in1=m[:, 6, :], op=ALU.mult), reads=[mn], writes=[mn])
                    xv = x[:, 0:1024].rearrange("p (h q) -> p h q", q=64)
                    for par in range(2):
                        kb.op("dve", lambda e: e.tensor_tensor(out=xp[:, par::2, par * 64:(par + 1) * 64], in0=xv[:, par::2, :],
                                                               in1=m[:, 0, par::2].unsqueeze(2).to_broadcast([128, 8, 64]), op=ALU.mult),
                              reads=[xn_, mn], writes=[xpn])
                    kb.op("pool", lambda e: e.tensor_tensor(out=xd[:], in0=xv, in1=m[:, 7, :].unsqueeze(2).to_broadcast([128, 16, 64]), op=ALU.mult),
                          reads=[xn_, mn], writes=[xdn])
                    kb.op("pool", lambda e: e.tensor_copy(out=ar[:], in_=m[:, 1, :].unsqueeze(2).to_broadcast([128, 16, 128])), reads=[mn], writes=[arn])
                    for g in range(4):
                        pc, pcn = p_csb.next()
                        cb_, cbn = cbs.next()
                        a_, a_n = arg.next()
                        d_, d_n = dm.next()
                        m_, m_n = mt.next()
                        e_, e_n = ecs.next()
                        c_, c_n = cst.next()
                        kb.op("pe", lambda e: e.matmul(p_cb, lhsT=bc[:, g, :], rhs=bc[:, 4 + g, :], start=True, stop=True), reads=[bcn], writes=["sc_pcb"])
                        kb.op("act", lambda e: e.copy(out=cb_[:], in_=p_cb), reads=["sc_pcb"], writes=[cbn])
                        for hh in range(4):
                            h = g * 4 + hh
                            kb.op("pe", lambda e: e.matmul(pc[:, hh, :], lhsT=ar[:, h, :], rhs=tri[d][:], start=True, stop=True), reads=[arn, "tri"], writes=[pcn])
                        kb.op("dve", lambda e: e.tensor_tensor(out=a_[:], in0=pc[:], in1=m[:, 2, g * 4:(g + 1) * 4].unsqueeze(2).to_broadcast([128, 4, 128]),
                                                               op=ALU.subtract), reads=[pcn, mn], writes=[a_n])
                        kb.op("pool", lambda e: e.tensor_tensor(out=a_[:], in0=a_[:], in1=negm[d][:].unsqueeze(1).to_broadcast([128, 4, 128]), op=ALU.add),
                              reads=[a_n, "negm"], writes=[a_n])
                        kb.op("act", lambda e: e.activation(out=d_[:], in_=a_[:], func=AF.Exp), reads=[a_n], writes=[d_n])
                        kb.op("dve", lambda e: e.tensor_tensor(out=m_[:], in0=d_[:], in1=cb_[:].unsqueeze(1).to_broadcast([128, 4, 128]), op=ALU.mult),
                              reads=[d_n, cbn], writes=[m_n])
                        kb.op("act", lambda e: e.activation(out=e_[:], in_=pc[:], func=AF.Exp), reads=[pcn], writes=[e_n])
                        kb.op("pool", lambda e: e.tensor_tensor(out=c_[:], in0=e_[:], in1=bc[:, 4 + g, :].unsqueeze(1).to_broadcast([128, 4, 128]), op=ALU.mult),
                              reads=[e_n, bcn], writes=[c_n])
                        for hh in range(4):
                            h = g * 4 + hh
                            c2 = h // 2
                            kb.op("pe", lambda e: e.matmul(p_y[:, c2, :], lhsT=xp[:, h, :], rhs=m_[:, hh, :], start=(h % 2 == 0), stop=False),
                                  reads=[xpn, m_n], writes=["sc_py"])
                            kb.op("pe", lambda e: e.matmul(p_y[:, c2, :], lhsT=SbP[:, h, :], rhs=c_[:, hh, :], start=False, stop=(h % 2 == 1)),
                                  reads=["sc_SbP", c_n], writes=["sc_py"])
                    if d == 0:
                        dump("m", m[:], [mn]); dump("ar", ar[:, 0:2, :], [arn]); dump("arg", a_[:], [a_n]); dump("dm", d_[:], [d_n])
                        dump("mt", m_[:], [m_n]); dump("ecs", e_[:], [e_n]); dump("cst", c_[:], [c_n]); dump("cbs", cb_[:], [cbn])
                        dump("xp", xp[:, 0:4, :], [xpn]); dump("xd", xd[:, 0:4, :], [xdn]); dump("rowb", rowb[:], ["sc_rowb"])
                    for g in range(4):
                        kb.op("pe", lambda e: e.matmul(p_s[:, g * 4:(g + 1) * 4, :], lhsT=x[:, 1024 + g * 128:1024 + (g + 1) * 128],
                                                       rhs=xd[:, g * 4:(g + 1) * 4, :], start=True, stop=True), reads=[xn_, xdn], writes=["sc_pS"])
                    kb.op("dve", lambda e: e.tensor_tensor(out=S[:], in0=S[:], in1=m[:, 5, :].unsqueeze(2).to_broadcast([128, 16, 64]), op=ALU.mult),
                          reads=["sc_S", mn], writes=["sc_S"])
                    kb.op("dve", lambda e: e.tensor_tensor(out=S[:], in0=S[:], in1=p_s[:], op=ALU.add), reads=["sc_S", "sc_pS"], writes=["sc_S"])
                    for par in range(2):
                        kb.op("act", lambda e: e.copy(out=SbP[:, par::2, par * 64:(par + 1) * 64], in_=S[:, par::2, :]), reads=["sc_S"], writes=["sc_SbP"])
                    o, on = yo.next()
                    if d == 0:
                        kb.op("act", lambda e: e.copy(out=o[:], in_=p_y[:]), reads=["sc_py"], writes=[on])
                        kb.dma(yfT[:, t0:t0 + 128].rearrange("(c p) t -> p c t", p=128), o[:], reads=[on], writes=["dram_yfT"])
                    else:
                        f_, fn_ = yf.next()
                        z_, zn_ = zt.next()
                        q_, qn_ = xsT.next()
                        a2, a2n = ao.next()
                        kb.dma(f_[:], yfT[:, t0:t0 + 128].rearrange("(c p) t -> p c t", p=128), reads=["dram_yfT"], writes=[fn_])
                        kb.dma(z_[:], PT[O_Z:O_Z + 1024, t0:t0 + 128].rearrange("(c p) t -> p c t", p=128), reads=["dram_PT"], writes=[zn_])
                        kb.dma(q_[:], xcT[0:1024, t0:t0 + 128].rearrange("(c p) t -> p c t", p=128), reads=["dram_xcT"], writes=[qn_])
                        kb.op("dve", lambda e: e.tensor_tensor(out=o[:], in0=p_y[:], in1=f_[:], op=ALU.add), reads=["sc_py", fn_], writes=[on])
                        kb.op("pool", lambda e: e.tensor_tensor(out=f_[:], in0=q_[:], in1=dg[:, 0:8].unsqueeze(2).to_broadcast([128, 8, 128]), op=ALU.mult),
                              reads=[qn_, "sc_dg"], writes=[fn_])
                        kb.op("dve", lambda e: e.tensor_tensor(out=o[:], in0=o[:], in1=f_[:], op=ALU.add), reads=[on, fn_], writes=[on])
                        kb.op("act", lambda e: e.activation(out=z_[:], in_=z_[:], func=AF.Silu), reads=[zn_], writes=[zn_])
                        kb.op("dve", lambda e: e.tensor_tensor(out=o[:], in0=o[:], in1=z_[:], op=ALU.mult), reads=[on, zn_], writes=[on])
                        kb.op("act", lambda e: e.activation(out=sq[:], in_=o[:], func=AF.Square), reads=[on], writes=["sc_sq"])
                        for c2 in range(8):
                            kb.op("pe", lambda e: e.matmul(p_n, lhsT=ones_b[:], rhs=sq[:, c2, :], start=(c2 == 0), stop=(c2 == 7)),
                                  reads=["ones_b", "sc_sq"], writes=["sc_pn"])
                        kb.op("act", lambda e: e.activation(out=rs[:], in_=p_n, func=AF.Sqrt, scale=1.0 / 1024, bias=C_EPS), reads=["sc_pn", "kcon"], writes=["sc_rs"])
                        kb.op("dve", lambda e: e.reciprocal(out=rs[:], in_=rs[:]), reads=["sc_rs"], writes=["sc_rs"])
                        kb.op("dve", lambda e: e.tensor_tensor(out=o[:], in0=o[:], in1=rs[:].unsqueeze(1).to_broadcast([128, 8, 128]), op=ALU.mult),
                              reads=[on, "sc_rs"], writes=[on])
                        kb.op("pool", lambda e: e.tensor_tensor(out=a2[:], in0=o[:], in1=dg[:, 8:16].unsqueeze(2).to_broadcast([128, 8, 128]), op=ALU.mult),
                              reads=[on, "sc_dg"], writes=[a2n])
                        kb.dma(aT[:, t0:t0 + 128].rearrange("(c p) t -> p c t", p=128), a2[:], reads=[a2n], writes=["dram_aT"])
                kb.barrier()

        def stage_gla(l, d):
            ncc, ncl = TC // 64, TL // 64
            if d == 0:
                order = list(range(ncc + ncl))
            else:
                order = list(range(ncc - 1, -1, -1)) + [ncc + i for i in range(ncl - 1, -1, -1)]
            last = 63 if d == 0 else 0
            QS = 128.0 ** -0.5
            with ExitStack() as st:
                wg = st.enter_context(sb("g_wg", [16, 512], F32))
                bg = st.enter_context(sb("g_bg", [1, 512], F32))
                ngt = st.enter_context(sb("g_ng", [128, 2], F32))
                S = st.enter_context(sb("g_S", [128, 4, 256], F32))
                Sb = st.enter_context(sb("g_Sb", [128, 4, 256], BF16))
                glr = Rot(st, sb, "g_glr", 2, [16, 64], F32)
                qk = Rot(st, sb, "g_qk", 2, [128, 8, 64], F32)
                kv = Rot(st, sb, "g_kv", 2, [64, 1536], F32)
                vb = Rot(st, sb, "g_vb", 2, [64, 1024], BF16)
                gt = Rot(st, sb, "g_g", 2, [64, 512], F32)
                gcs = Rot(st, sb, "g_gcs", 2, [64, 512], F32)
                ko = Rot(st, sb, "g_ko", 2, [64, 512], BF16)
                eg = Rot(st, sb, "g_eg", 2, [128, 8, 64], F32)
                qkin = Rot(st, sb, "g_qkin", 2, [128, 8, 64], BF16)
                att = Rot(st, sb, "g_att", 2, [64, 4, 64], BF16)
                oo = Rot(st, sb, "g_oo", 2, [128, 8, 64], F32)
                p_a = st.enter_context(ps("g_pa", [64, 512], F32))
                p_b = st.enter_context(ps("g_pb", [64, 512], F32))
                p_c = st.enter_context(ps("g_pc", [128, 4, 64], F32))
                p_at = st.enter_context(ps("g_pat", [64, 4, 64], F32))
                p_o = st.enter_context(ps("g_po", [128, 8, 64], F32))
                p_s = st.enter_context(ps("g_ps", [128, 4, 256], F32))
                if d == 1:
                    of = Rot(st, sb, "g_of", 2, [128, 8, 64], F32)
                    rt = Rot(st, sb, "g_rt", 2, [128, 8, 64], F32)
                    sq = st.enter_context(sb("g_sq", [128, 8, 64], BF16))
                    rs = st.enter_context(sb("g_rs", [128, 4, 64], F32))
                    co = Rot(st, sb, "g_co", 2, [128, 8, 64], BF16)
                    p_n = st.enter_context(ps("g_pn", [128, 4, 64], F32))
                kb.dma(wg[:], I["gla_wg"][l, d], writes=["g_wg"])
                kb.dma(bg[:], I["gla_bg"][l, d], writes=["g_bg"])
                kb.dma(ngt[:], I["gla_ng"][l], writes=["g_ng"])
                kb.op("pool", lambda e: e.memset(S[:], 0.0), writes=["g_S"])
                kb.op("pool", lambda e: e.memset(Sb[:], 0.0), writes=["g_Sb"])
                tr = tri[d][0:64, 0:64]
                for ci in order:
                    t0 = ci * 64
                    gl, gln = glr.next()
                    q_, qn_ = qk.next()
                    k_, kn_ = kv.next()
                    v_, vn_ = vb.next()
                    g_, gn_ = gt.next()
                    gc_, gcn = gcs.next()
                    ko_, kon = ko.next()
                    e_, en_ = eg.next()
                    qi, qin = qkin.next()
                    at, atn = att.next()
                    o_, on_ = oo.next()
                    kb.dma(gl[:], PT[O_GLR + d * 16:O_GLR + (d + 1) * 16, t0:t0 + 64], reads=["dram_PT"], writes=[gln])
                    kb.dma(q_[:], PT[O_GQ:O_GQ + 1024, t0:t0 + 64].rearrange("(c p) t -> p c t", p=128), reads=["dram_PT"], writes=[qn_])
                    kb.dma(k_[:], PK[t0:t0 + 64, O_GK:O_GK + 1536], reads=["dram_PK"], writes=[kn_])
                    kb.op("pool", lambda e: e.tensor_copy(out=v_[:], in_=k_[:, 512:1536]), reads=[kn_], writes=[vn_])
                    kb.op("pe", lambda e: e.matmul(p_a[:], lhsT=gl[:], rhs=wg[:], start=True, stop=False), reads=[gln, "g_wg"], writes=["g_pa"])
                    kb.op("pe", lambda e: e.matmul(p_a[:], lhsT=ones_f[0:1, 0:64], rhs=bg[:], start=False, stop=True), reads=["ones_f", "g_bg"], writes=["g_pa"])
                    kb.op("act", lambda e: e.activation(out=g_[:], in_=p_a[:], func=AF.Exp, scale=-1.0), reads=["g_pa"], writes=[gn_])
                    kb.op("act", lambda e: e.activation(out=g_[:], in_=g_[:], func=AF.Ln, bias=kcon[0:64, 0:1], scale=1.0), reads=[gn_, "kcon"], writes=[gn_])
                    kb.op("dve", lambda e: e.tensor_scalar(out=g_[:], in0=g_[:], scalar1=-1.0 / 16.0, scalar2=None, op0=ALU.mult), reads=[gn_], writes=[gn_])
                    kb.op("pe", lambda e: e.matmul(p_b[:], lhsT=tr, rhs=g_[:], start=True, stop=True), reads=["tri", gn_], writes=["g_pb"])
                    kb.op("pe", lambda e: e.matmul(p_a[:], lhsT=ones_f[0:64, 0:64], rhs=g_[:], start=True, stop=True), reads=["ones_f", gn_], writes=["g_pa"])
                    for h in range(4):
                        kb.op("pe", lambda e: e.matmul(p_c[:, h, :], lhsT=g_[:, h * 128:(h + 1) * 128], rhs=tr, start=True, stop=True), reads=[gn_, "tri"], writes=["g_pc"])
                    kb.op("act", lambda e: e.copy(out=gc_[:], in_=p_b[:]), reads=["g_pb"], writes=[gcn])
                    kb.op("dve", lambda e: e.tensor_tensor(out=gc_[:], in0=p_a[:], in1=gc_[:], op=ALU.subtract), reads=["g_pa", gcn], writes=[gcn])
                    kb.op("act", lambda e: e.activation(out=gc_[:], in_=gc_[:], func=AF.Exp), reads=[gcn], writes=[gcn])
                    kb.op("dve", lambda e: e.tensor_tensor(out=ko_[:], in0=k_[:, 0:512], in1=gc_[:], op=ALU.mult), reads=[kn_, gcn], writes=[kon])
                    kb.op("act", lambda e: e.activation(out=e_[:, 0:4, :], in_=p_c[:], func=AF.Exp), reads=["g_pc"], writes=[en_])
                    kb.op("act", lambda e: e.activation(out=e_[:, 4:8, :], in_=p_c[:], func=AF.Exp, scale=-1.0), reads=["g_pc"], writes=[en_])
                    kb.op("dve", lambda e: e.scalar_tensor_tensor(out=qi[:, 0:4, :], in0=q_[:, 0:4, :], scalar=QS, in1=e_[:, 0:4, :], op0=ALU.mult, op1=ALU.mult),
                          reads=[qn_, en_], writes=[qin])
                    kb.op("pool", lambda e: e.tensor_tensor(out=qi[:, 4:8, :], in0=q_[:, 4:8, :], in1=e_[:, 4:8, :], op=ALU.mult), reads=[qn_, en_], writes=[qin])
                    for h in range(4):
                        kb.op("pe", lambda e: e.matmul(p_at[:, h, :], lhsT=qi[:, 4 + h, :], rhs=qi[:, h, :], start=True, stop=True), reads=[qin], writes=["g_pat"])
                    kb.op("dve", lambda e: e.tensor_tensor(out=at[:], in0=p_at[:], in1=tr.unsqueeze(1).to_broadcast([64, 4, 64]), op=ALU.mult),
                          reads=["g_pat", "tri"], writes=[atn])
                    for vc in range(8):
                        h = vc // 2
                        kb.op("pe", lambda e: e.matmul(p_o[:, vc, :], lhsT=v_[:, vc * 128:(vc + 1) * 128], rhs=at[:, h, :], start=True, stop=False),
                              reads=[vn_, atn], writes=["g_po"])
                        kb.op("pe", lambda e: e.matmul(p_o[:, vc, :], lhsT=Sb[:, h, (vc % 2) * 128:(vc % 2 + 1) * 128], rhs=qi[:, h, :], start=False, stop=True),
                              reads=["g_Sb", qin], writes=["g_po"])
                    for h in range(4):
                        kb.op("pe", lambda e: e.matmul(p_s[:, h, :], lhsT=ko_[:, h * 128:(h + 1) * 128], rhs=v_[:, h * 256:(h + 1) * 256], start=True, stop=True),
                              reads=[kon, vn_], writes=["g_ps"])
                    for h in range(4):
                        kb.op("dve", lambda e: e.scalar_tensor_tensor(out=S[:, h, :], in0=S[:, h, :], scalar=e_[:, h, last:last + 1], in1=p_s[:, h, :],
                                                                       op0=ALU.mult, op1=ALU.add), reads=["g_S", en_, "g_ps"], writes=["g_S"])
                    kb.op("act", lambda e: e.copy(out=Sb[:], in_=S[:]), reads=["g_S"], writes=["g_Sb"])
                    if d == 0:
                        kb.op("act", lambda e: e.copy(out=o_[:], in_=p_o[:]), reads=["g_po"], writes=[on_])
                        kb.dma(ofT[:, t0:t0 + 64].rearrange("(c p) t -> p c t", p=128), o_[:], reads=[on_], writes=["dram_ofT"])
                    else:
                        f_, fn_ = of.next()
                        r_, rn_ = rt.next()
                        c_, cn_ = co.next()
                        kb.dma(f_[:], ofT[:, t0:t0 + 64].rearrange("(c p) t -> p c t", p=128), reads=["dram_ofT"], writes=[fn_])
                        kb.dma(r_[:], PT[O_GR:O_GR + 1024, t0:t0 + 64].rearrange("(c p) t -> p c t", p=128), reads=["dram_PT"], writes=[rn_])
                        kb.op("dve", lambda e: e.tensor_tensor(out=o_[:], in0=p_o[:], in1=f_[:], op=ALU.add), reads=["g_po", fn_], writes=[on_])
                        kb.op("act", lambda e: e.activation(out=sq[:], in_=o_[:], func=AF.Square), reads=[on_], writes=["g_sq"])
                        for h in range(4):
                            for u in range(2):
                                kb.op("pe", lambda e: e.matmul(p_n[:, h, :], lhsT=ones_b[:], rhs=sq[:, 2 * h + u, :], start=(u == 0), stop=(u == 1)),
                                      reads=["ones_b", "g_sq"], writes=["g_pn"])
                        kb.op("act", lambda e: e.activation(out=rs[:], in_=p_n[:], func=AF.Sqrt, scale=1.0 / 256, bias=C_EPS), reads=["g_pn", "kcon"], writes=["g_rs"])
                        kb.op("dve", lambda e: e.reciprocal(out=rs[:], in_=rs[:]), reads=["g_rs"], writes=["g_rs"])
                        ov = o_[:].rearrange("p (h u) t -> p h u t", u=2)
                        kb.op("dve", lambda e: e.tensor_tensor(out=ov, in0=ov, in1=rs[:].unsqueeze(2).to_broadcast([128, 4, 2, 64]), op=ALU.mult),
                              reads=[on_, "g_rs"], writes=[on_])
                        for u in range(2):
                            kb.op("pool", lambda e: e.tensor_scalar(out=o_[:, u::2, :], in0=o_[:, u::2, :], scalar1=ngt[:, u:u + 1], scalar2=None, op0=ALU.mult),
                                  reads=[on_, "g_ng"], writes=[on_])
                        kb.op("act", lambda e: e.activation(out=r_[:], in_=r_[:], func=AF.Silu), reads=[rn_], writes=[rn_])
                        kb.op("dve", lambda e: e.tensor_tensor(out=c_[:], in0=o_[:], in1=r_[:], op=ALU.mult), reads=[on_, rn_], writes=[cn_])
                        kb.dma(cT[:, t0:t0 + 64].rearrange("(c p) t -> p c t", p=128), c_[:], reads=[cn_], writes=["dram_cT"])
                kb.barrier()

        def stage_qknorm(l):
            with ExitStack() as st:
                agt = st.enter_context(sb("q_ag", [128, 4], F32))
                pmf = st.enter_context(sb("q_pmf", [128, 128], F32))
                pmb = st.enter_context(sb("q_pmb", [128, 128], BF16))
                xin = Rot(st, sb, "q_x", 3, [128, 512], F32)
                sqb = Rot(st, sb, "q_sq", 2, [128, 512], BF16)
                rsd = Rot(st, sb, "q_rs", 2, [128, 512], F32)
                xnb = Rot(st, sb, "q_xn", 3, [128, 512], BF16)
                cs = Rot(st, sb, "q_cs", 2, [128, 2, 512], F32)
                t1 = Rot(st, sb, "q_t1", 2, [128, 512], F32)
                t2 = Rot(st, sb, "q_t2", 2, [128, 512], F32)
                xr = Rot(st, sb, "q_xr", 2, [128, 512], BF16)
                pp = Rot(st, ps, "q_ps", 2, [128, 512], F32)
                pr = Rot(st, ps, "q_pr", 2, [128, 512], F32)
                vin = Rot(st, sb, "q_vin", 2, [128, 1280], F32)
                vo = Rot(st, sb, "q_vo", 2, [128, 1280], BF16)
                kb.dma(agt[:], I["att_g"][l], writes=["q_ag"])
                kb.dma(pmf[:], I["pm"][:, :], writes=["q_pmf"])
                kb.op("dve", lambda e: e.tensor_copy(out=pmb[:], in_=pmf[:]), reads=["q_pmf"], writes=["q_pmb"])
                for ti in range(NT):
                    t0 = ti * 128
                    a, an = vin.next()
                    o, on = vo.next()
                    kb.dma(a[:, 0:1024], PK[t0:t0 + 128, O_NA + 2048:O_NA + 3072], reads=["dram_PK"], writes=[an])
                    kb.dma(a[:, 1024:1280], PK[t0:t0 + 128, O_SV:O_SV + 256], reads=["dram_PK"], writes=[an])
                    kb.op("pool", lambda e: e.tensor_copy(out=o[:], in_=a[:]), reads=[an], writes=[on])
                    kb.dma(vtok[t0:t0 + 128, :], o[:], reads=[on], writes=["dram_vtok"])
                chunks = []
                for c in range(8):
                    chunks.append((O_NA + c * 128, nqk, c * 128, 0, True, False))
                for c in range(8):
                    chunks.append((O_NA + 1024 + c * 128, nqk, 1024 + c * 128, 1, False, False))
                for c in range(8):
                    chunks.append((O_SQ + c * 128, sqk, c * 128, 2, True, True))
                for c in range(2):
                    chunks.append((O_SK + c * 128, sqk, 1024 + c * 128, 3, False, True))
                for (b0, bn) in cfg.blocks():
                    is_ctx = b0 < TC
                    if not is_ctx:
                        c_, cn_ = cs.next()
                        kb.dma(c_[:, :, 0:bn], I["rope"][:, :, b0 - TC:b0 - TC + bn].rearrange("a p t -> p a t"), writes=[cn_])
                    for (srow, dst, drow, gcol, is_q, rope) in chunks:
                        x, xn_ = xin.next()
                        q2, q2n = sqb.next()
                        r_, rn_ = rsd.next()
                        xb, xbn = xnb.next()
                        p, pn = pp.next()
                        kb.dma(x[:, 0:bn], PT[srow:srow + 128, b0:b0 + bn], reads=["dram_PT"], writes=[xn_])
                        kb.op("act", lambda e: e.activation(out=q2[:, 0:bn], in_=x[:, 0:bn], func=AF.Square), reads=[xn_], writes=[q2n])
                        kb.op("pe", lambda e: e.matmul(p[:, 0:bn], lhsT=blk64[:], rhs=q2[:, 0:bn], start=True, stop=True), reads=["blk64", q2n], writes=[pn])
                        if is_q:
                            kb.op("act", lambda e: e.activation(out=r_[:, 0:bn], in_=p[:, 0:bn], func=AF.Ln, scale=1.0, bias=kcon[:, 3:4]),
                                  reads=[pn, "kcon"], writes=[rn_])
                        else:
                            kb.op("act", lambda e: e.activation(out=r_[:, 0:bn], in_=p[:, 0:bn], func=AF.Ln, scale=1.0 / 64, bias=C_EPS),
                                  reads=[pn, "kcon"], writes=[rn_])
                        kb.op("act", lambda e: e.activation(out=r_[:, 0:bn], in_=r_[:, 0:bn], func=AF.Exp, scale=-0.5), reads=[rn_], writes=[rn_])
                        kb.op("dve", lambda e: e.scalar_tensor_tensor(out=xb[:, 0:bn], in0=x[:, 0:bn], scalar=agt[:, gcol:gcol + 1], in1=r_[:, 0:bn],
                                                                       op0=ALU.mult, op1=ALU.mult), reads=[xn_, "q_ag", rn_], writes=[xbn])
                        if rope and not is_ctx:
                            rp, rpn = pr.next()
                            a1, a1n = t1.next()
                            a2, a2n = t2.next()
                            o, on = xr.next()
                            kb.op("pe", lambda e: e.matmul(rp[:, 0:bn], lhsT=pmb[:], rhs=xb[:, 0:bn], start=True, stop=True), reads=["q_pmb", xbn], writes=[rpn])
                            kb.op("dve", lambda e: e.tensor_tensor(out=a1[:, 0:bn], in0=rp[:, 0:bn], in1=c_[:, 1, 0:bn], op=ALU.mult), reads=[rpn, cn_], writes=[a1n])
                            kb.op("pool", lambda e: e.tensor_tensor(out=a2[:, 0:bn], in0=xb[:, 0:bn], in1=c_[:, 0, 0:bn], op=ALU.mult), reads=[xbn, cn_], writes=[a2n])
                            kb.op("pool", lambda e: e.tensor_tensor(out=o[:, 0:bn], in0=a1[:, 0:bn], in1=a2[:, 0:bn], op=ALU.add), reads=[a1n, a2n], writes=[on])
                            kb.dma(dst[drow:drow + 128, b0:b0 + bn], o[:, 0:bn], reads=[on], writes=["dram_qk"])
                        else:
                            kb.dma(dst[drow:drow + 128, b0:b0 + bn], xb[:, 0:bn], reads=[xbn], writes=["dram_qk"])
                kb.barrier()

        def stage_attn(l, kind):
            ntl = TL // 128
            nct = TC // 128
            cls_list, cls_of = na_classes(TL)
            with ExitStack() as st:
                qt = Rot(st, sb, "a_q", 2, [128, 8, 128], BF16)
                kc_ = st.enter_context(sb("a_kc", [128, 8, nct * 128], BF16))
                vc_ = st.enter_context(sb("a_vc", [128, nct, 1024], BF16))
                kl = Rot(st, sb, "a_kl", 2, [128, 8, 5 * 128], BF16)
                vl = Rot(st, sb, "a_vl", 2, [128, 5, 1024], BF16)
                pT = Rot(st, sb, "a_pT", 3, [128, 7, 128], BF16)
                rd = Rot(st, sb, "a_rd", 2, [64, 128], F32)
                ob = Rot(st, sb, "a_ob", 2, [64, 16, 128], BF16)
                p_sc = Rot(st, ps, "a_psc", 2, [128, 8, 128], F32)
                p_od = Rot(st, ps, "a_pod", 2, [64, 2, 128], F32)
                if kind == "na":
                    bf_ = st.enter_context(sb("a_bf", [128, 5 * 128], F32))
                    bias = st.enter_context(sb("a_bias", [128, 16, 5, 128], BF16))
                    qsrc, ksrc, vcol, dst = nqk[0:1024], nqk[1024:2048], 0, bT
                    nkc = 8
                else:
                    esk = st.enter_context(sb("a_esk", [64, 16], F32))
                    kb.dma(esk[:], I["esink"][l], writes=["a_esk"])
                    kb.op("act", lambda e: e.activation(out=esk[:], in_=esk[:], func=AF.Exp), reads=["a_esk"], writes=["a_esk"])
                    qsrc, ksrc, vcol, dst = sqk[0:1024], sqk[1024:1280], 1024, dT
                    nkc = 4

                def load_k(tile_, t0, n):
                    if kind == "na":
                        return [(tile_[:, :, 0:n], ksrc[:, t0:t0 + n].rearrange("(c p) t -> p c t", p=128))]
                    src = ksrc[:, t0:t0 + n].rearrange("(g d) t -> d g t", d=64)
                    return [(tile_[0:64, 0:4, 0:n], src), (tile_[64:128, 0:4, 0:n], src)]

                for (a_, b_) in load_k(kc_, 0, TC):
                    kb.dma(a_, b_, reads=["dram_qk"], writes=["a_kc"])
                nv = 1024 if kind == "na" else 256
                kb.dma(vc_[:, :, 0:nv], vtok[0:TC, vcol:vcol + nv].rearrange("(n p) c -> p n c", p=128), reads=["dram_vtok"], writes=["a_vc"])
                cur_cls = None
                for qi in range(nct + ntl):
                    t0 = qi * 128
                    q_, qn_ = qt.next()
                    o_, on_ = ob.next()
                    kb.dma(q_[:], qsrc[:, t0:t0 + 128].rearrange("(c p) t -> p c t", p=128), reads=["dram_qk"], writes=[qn_])
                    keys = [("c", j, None) for j in range(nct)]
                    if qi >= nct:
                        ti = qi - nct
                        k_, kn_ = kl.next()
                        v_, vn_ = vl.next()
                        if kind == "na":
                            ci = cls_of[ti]
                            off_a, off_b, klo_rel, nsl = cls_list[ci]
                            kt_lo = ti + klo_rel
                            if ci != cur_cls:
                                cur_cls = ci
                                for h in range(16):
                                    kb.dma(bf_[:], I["na_bias"][l, ci, :, h * 640:(h + 1) * 640], writes=["a_bf"])
                                    kb.op("pool", lambda e: e.tensor_copy(out=bias[:, h, :, :], in_=bf_[:].rearrange("p (s i) -> p s i", s=5)),
                                          reads=["a_bf"], writes=["a_bias"])
                            for sl in range(nsl):
                                keys.append(("l", sl, ("na", sl)))
                        else:
                            kt_lo = max(ti - 1, 0)
                            kt_hi = min(ti + 1, ntl - 1)
                            nsl = kt_hi - kt_lo + 1
                            for sl in range(nsl):
                                kt = kt_lo + sl
                                keys.append(("l", sl, None if kt == ti else ("m", 1 if kt < ti else 0)))
                        tk = TC + kt_lo * 128
                        for (a_, b_) in load_k(k_, tk, nsl * 128):
                            kb.dma(a_, b_, reads=["dram_qk"], writes=[kn_])
                        kb.dma(v_[:, 0:nsl, 0:nv], vtok[tk:tk + nsl * 128, vcol:vcol + nv].rearrange("(n p) c -> p n c", p=128),
                               reads=["dram_vtok"], writes=[vn_])
                    nk = len(keys)
                    for h in range(16):
                        c, b = h // 2, (h % 2) * 64
                        kc_i = c if kind == "na" else h // 4
                        vh = h if kind == "na" else h // 4
                        sc, scn = p_sc.next()
                        od, odn = p_od.next()
                        p_, pn_ = pT.next()
                        r_, rn_ = rd.next()
                        for j, (src, idx, bsp) in enumerate(keys):
                            kt_ = kc_ if src == "c" else k_
                            ktn = "a_kc" if src == "c" else kn_
                            kb.op("pe", lambda e: e.matmul(sc[:, j, :], lhsT=kt_[b:b + 64, kc_i, idx * 128:(idx + 1) * 128], rhs=q_[b:b + 64, c, :],
                                                           start=True, stop=(bsp is None)), reads=[ktn, qn_], writes=[scn])
                            if bsp is not None:
                                if bsp[0] == "na":
                                    kb.op("pe", lambda e: e.matmul(sc[:, j, :], lhsT=ident_b[:], rhs=bias[:, h, bsp[1], :], start=False, stop=True),
                                          reads=["ident_b", "a_bias"], writes=[scn])
                                else:
                                    kb.op("pe", lambda e: e.matmul(sc[:, j, :], lhsT=ident_b[:], rhs=negm_b[bsp[1]][:], start=False, stop=True),
                                          reads=["ident_b", "negm_b"], writes=[scn])
                        kb.op("act", lambda e: e.activation(out=p_[:, 0:nk, :], in_=sc[:, 0:nk, :], func=AF.Exp), reads=[scn], writes=[pn_])
                        for j, (src, idx, bsp) in enumerate(keys):
                            vt_ = vc_ if src == "c" else v_
                            vtn = "a_vc" if src == "c" else vn_
                            kb.op("pe", lambda e: e.matmul(od[:, 0, :], lhsT=vt_[:, idx, vh * 64:(vh + 1) * 64], rhs=p_[:, j, :], start=(j == 0), stop=(j == nk - 1)),
                                  reads=[vtn, pn_], writes=[odn])
                        for j in range(nk):
                            kb.op("pe", lambda e: e.matmul(od[:, 1, :], lhsT=ones_b[:, 0:64], rhs=p_[:, j, :], start=(j == 0), stop=(j == nk - 1)),
                                  reads=["ones_b", pn_], writes=[odn])
                        if kind == "swa":
                            kb.op("dve", lambda e: e.tensor_scalar(out=r_[:], in0=od[:, 1, :], scalar1=esk[:, h:h + 1], scalar2=None, op0=ALU.add),
                                  reads=[odn, "a_esk"], writes=[rn_])
                            kb.op("dve", lambda e: e.reciprocal(out=r_[:], in_=r_[:]), reads=[rn_], writes=[rn_])
                        else:
                            kb.op("dve", lambda e: e.reciprocal(out=r_[:], in_=od[:, 1, :]), reads=[odn], writes=[rn_])
                        kb.op("dve", lambda e: e.tensor_tensor(out=o_[:, h, :], in0=od[:, 0, :], in1=r_[:], op=ALU.mult), reads=[odn, rn_], writes=[on_])
                    kb.dma(dst[:, t0:t0 + 128].rearrange("(h d) t -> d h t", d=64), o_[:], reads=[on_], writes=["dram_o" + kind])
                kb.barrier()

        def stage_merge():
            with ExitStack() as st:
                wbr = Rot(st, sb, "m_w", 2, [128, 32, 128], BF16)
                ob = [Rot(st, sb, "m_o%d" % i, 2, [128, 8, 512], BF16) for i in range(4)]
                gt = Rot(st, sb, "m_g", 4, [128, 512], F32)
                acc = Rot(st, sb, "m_acc", 2, [128, 512], F32)
                tmp = Rot(st, sb, "m_tmp", 2, [128, 512], F32)
                mo = Rot(st, sb, "m_mo", 2, [128, 512], BF16)
                pp = Rot(st, ps, "m_ps", 4, [128, 512], F32)
                srcs = [aT, bT, cT, dT]
                for fc in range(16):
                    w, wn = wbr.next()
                    kb.dma(w[:], wb_br[:, fc * 128:(fc + 1) * 128].rearrange("(a p) f -> p a f", p=128), reads=["dram_wb_br"], writes=[wn])
                    for (b0, bn) in cfg.blocks():
                        a, an = acc.next()
                        for i in range(4):
                            o, on = ob[i].next()
                            g, gn = gt.next()
                            p, pn = pp.next()
                            kb.dma(o[:, :, 0:bn], srcs[i][:, b0:b0 + bn].rearrange("(c p) t -> p c t", p=128), reads=["dram_br%d" % i], writes=[on])
                            kb.dma(g[:, 0:bn], PT[O_GATE + i * 2048 + fc * 128:O_GATE + i * 2048 + (fc + 1) * 128, b0:b0 + bn], reads=["dram_PT"], writes=[gn])
                            for kc in range(8):
                                kb.op("pe", lambda e: e.matmul(p[:, 0:bn], lhsT=w[:, i * 8 + kc, :], rhs=o[:, kc, 0:bn], start=(kc == 0), stop=(kc == 7)),
                                      reads=[wn, on], writes=[pn])
                            kb.op("act", lambda e: e.activation(out=g[:, 0:bn], in_=g[:, 0:bn], func=AF.Sigmoid), reads=[gn], writes=[gn])
                            if i == 0:
                                kb.op("dve", lambda e: e.tensor_tensor(out=a[:, 0:bn], in0=p[:, 0:bn], in1=g[:, 0:bn], op=ALU.mult), reads=[pn, gn], writes=[an])
                            else:
                                t_, tn = tmp.next()
                                kb.op("dve", lambda e: e.tensor_tensor(out=t_[:, 0:bn], in0=p[:, 0:bn], in1=g[:, 0:bn], op=ALU.mult), reads=[pn, gn], writes=[tn])
                                kb.op("pool", lambda e: e.tensor_tensor(out=a[:, 0:bn], in0=a[:, 0:bn], in1=t_[:, 0:bn], op=ALU.add), reads=[an, tn], writes=[an])
                        m_, mn = mo.next()
                        kb.op("act", lambda e: e.copy(out=m_[:, 0:bn], in_=a[:, 0:bn]), reads=[an], writes=[mn])
                        kb.dma(mTd[fc * 128:(fc + 1) * 128, b0:b0 + bn], m_[:, 0:bn], reads=[mn], writes=["dram_mTd"])
                kb.barrier()

        def resid_tile(st_tag, x, xname, pacc, pn, gtb, o, on):
            for oc in range(4):
                sl = slice(oc * 512, (oc + 1) * 512)
                kb.op("dve", lambda e: e.tensor_tensor(out=o[:, sl], in0=pacc[:, oc, :], in1=gtb[:, sl], op=ALU.mult), reads=[pn, st_tag + "_gtb"], writes=[on])
                kb.op("pool", lambda e: e.tensor_tensor(out=o[:, sl], in0=o[:, sl], in1=x[:, sl], op=ALU.add), reads=[on, xname], writes=[on])

        def stage_outproj(src, dst):
            with ExitStack() as st:
                wo = st.enter_context(sb("o_w", [128, NKC, D], BF16))
                gtb = st.enter_context(sb("o_gtb", [128, D], F32))
                mb = Rot(st, sb, "o_mb", 2, [128, NKC, 512], BF16)
                xin = Rot(st, sb, "o_x", 2, [128, D], F32)
                xo = Rot(st, sb, "o_xo", 2, [128, D], F32)
                pp = Rot(st, ps, "o_ps", 2, [128, 4, 512], F32)
                kb.dma(wo[:], wb_out[:, :].rearrange("(kc p) f -> p kc f", p=128), reads=["dram_wb_out"], writes=["o_w"])
                for (b0, bn) in cfg.blocks():
                    j = 1 if b0 < TC else 0
                    if b0 == 0 or b0 == TC:
                        kb.dma(gtb[:], modd[j:j + 1, 32 * 128:48 * 128].to_broadcast([128, D]), reads=["modd"], writes=["o_gtb"])
                    m_, mn = mb.next()
                    kb.dma(m_[:, :, 0:bn], mTd[:, b0:b0 + bn].rearrange("(kc p) t -> p kc t", p=128), reads=["dram_mTd"], writes=[mn])
                    for ti in range(bn // 128):
                        t0 = b0 + ti * 128
                        x, xname = xin.next()
                        o, on = xo.next()
                        p, pn = pp.next()
                        kb.dma(x[:], src[t0:t0 + 128, :], reads=["dram_x"], writes=[xname])
                        for oc in range(4):
                            for kc in range(NKC):
                                kb.op("pe", lambda e: e.matmul(p[:, oc, :], lhsT=m_[:, kc, ti * 128:(ti + 1) * 128], rhs=wo[:, kc, oc * 512:(oc + 1) * 512],
                                                               start=(kc == 0), stop=(kc == NKC - 1)), reads=[mn, "o_w"], writes=[pn])
                        resid_tile("o", x, xname, p, pn, gtb, o, on)
                        kb.dma(dst[t0:t0 + 128, :], o[:], reads=[on], writes=["dram_x1"])
                kb.barrier()

        def stage_peer_prep(l):
            conv_bf16(I["peer_wq"][l], wb_q, D, D, "wb_q")
            conv_bf16(I["peer_v"][l], Vd, 16384, D, "Vd")
            with ExitStack() as st:
                uin = Rot(st, sb, "u_in", 2, [128, D], F32)
                ub = Rot(st, sb, "u_b", 2, [128, D], BF16)
                ut = Rot(st, sb, "u_t", 2, [128, NKC, 512], BF16)
                tp = Rot(st, ps, "u_tp", 2, [128, NKC, 128], BF16)
                for ig in range(32):
                    u_, un = ut.next()
                    for et in range(4):
                        e0 = (ig * 4 + et) * 128
                        a, an = uin.next()
                        b, bn_ = ub.next()
                        p, pn = tp.next()
                        kb.dma(a[:], I["peer_u"][l, e0:e0 + 128, :], writes=[an])
                        kb.op("pool" if et % 2 else "dve", lambda e: e.tensor_copy(out=b[:], in_=a[:]), reads=[an], writes=[bn_])
                        for kc in range(NKC):
                            kb.op("pe", lambda e: e.transpose(out=p[:, kc, :], in_=b[:, kc * 128:(kc + 1) * 128], identity=ident_b[:]),
                                  reads=[bn_, "ident_b"], writes=[pn])
                        kb.op("act", lambda e: e.copy(out=u_[:, :, et * 128:(et + 1) * 128], in_=p[:]), reads=[pn], writes=[un])
                    kb.dma(UTd[ig], u_[:].rearrange("p k e -> p (k e)"), reads=[un], writes=["dram_UTd"])
                kb.barrier()

        def stage_peer_q():
            with ExitStack() as st:
                wq = st.enter_context(sb("pq_w", [128, NKC, D], BF16))
                hb = Rot(st, sb, "pq_h", 2, [128, NKC, 512], BF16)
                og = Rot(st, sb, "pq_o", 3, [128, 512], F32)
                pp = Rot(st, ps, "pq_ps", 4, [128, 512], F32)
                kb.dma(wq[:], wb_q[:, :].rearrange("(kc p) f -> p kc f", p=128), reads=["dram_wb_q"], writes=["pq_w"])
                k = 0
                for (b0, bn) in cfg.blocks():
                    h_, hn = hb.next()
                    kb.dma(h_[:, :, 0:bn], hT[:, b0:b0 + bn].rearrange("(kc p) t -> p kc t", p=128), reads=["dram_hT"], writes=[hn])
                    for fc in range(16):
                        p, pn = pp.next()
                        o, on = og.next()
                        for kc in range(NKC):
                            kb.op("pe", lambda e: e.matmul(p[:, 0:bn], lhsT=wq[:, kc, fc * 128:(fc + 1) * 128], rhs=h_[:, kc, 0:bn], start=(kc == 0), stop=(kc == NKC - 1)),
                                  reads=["pq_w", hn], writes=[pn])
                        if k % 2:
                            kb.op("act", lambda e: e.copy(out=o[:, 0:bn], in_=p[:, 0:bn]), reads=[pn], writes=[on])
                        else:
                            kb.op("dve", lambda e: e.tensor_copy(out=o[:, 0:bn], in_=p[:, 0:bn]), reads=[pn], writes=[on])
                        k += 1
                        kb.dma(qTd[fc * 128:(fc + 1) * 128, b0:b0 + bn], o[:, 0:bn], reads=[on], writes=["dram_qTd"])
                kb.barrier()

        def stage_peer_topk(l):
            BIGN = -1.0e30
            with ExitStack() as st:
                kk = st.enter_context(sb("pt_kk", [128, 2, 128], F32))
                qt = Rot(st, sb, "pt_q", 2, [128, 16, 128], F32)
                ss = Rot(st, sb, "pt_ss", 2, [128, 16, 128], F32)
                v12 = Rot(st, sb, "pt_v", 2, [128, 2, 16], F32)
                tmp = Rot(st, sb, "pt_tmp", 2, [128, 128], F32)
                cand = Rot(st, sb, "pt_cand", 2, [128, 16, 16], F32)
                tmp2 = Rot(st, sb, "pt_tmp2", 2, [128, 256], F32)
                tv = Rot(st, sb, "pt_tv", 2, [128, 16], F32)
                ez = Rot(st, sb, "pt_ez", 2, [128, 16], F32)
                thr = Rot(st, sb, "pt_thr", 2, [128, 16], F32)
                zz = Rot(st, sb, "pt_zz", 2, [128, 8], F32)
                pp = Rot(st, ps, "pt_ps", 2, [128, 8, 128], F32)
                kb.dma(kk[:], I["peer_kk"][l], writes=["pt_kk"])
                for ti in range(NT):
                    t0 = ti * 128
                    q_, qn_ = qt.next()
                    s_, sn_ = ss.next()
                    th, thn = thr.next()
                    z_, zn_ = zz.next()
                    kb.dma(q_[:], qTd[:, t0:t0 + 128].rearrange("(c p) t -> p c t", p=128), reads=["dram_qTd"], writes=[qn_])
                    for half in range(2):
                        p, pn = pp.next()
                        for cc in range(8):
                            c = half * 8 + cc
                            kb.op("pe", lambda e: e.matmul(p[:, cc, :], lhsT=q_[:, c, :], rhs=kk[:, c % 2, :], start=True, stop=True), reads=[qn_, "pt_kk"], writes=[pn])
                        if half == 0:
                            kb.op("act", lambda e: e.copy(out=s_[:, 0:8, :], in_=p[:]), reads=[pn], writes=[sn_])
                        else:
                            kb.op("dve", lambda e: e.tensor_copy(out=s_[:, 8:16, :], in_=p[:]), reads=[pn], writes=[sn_])
                    kb.dma(sc_d[t0:t0 + 128, :], s_[:].rearrange("p c k -> p (c k)"), reads=[sn_], writes=["dram_sc_d"])
                    for h in range(8):
                        v_, vn_ = v12.next()
                        c_, cn_ = cand.next()
                        t2, t2n = tmp2.next()
                        tv_, tvn = tv.next()
                        e_, en_ = ez.next()
                        for u in range(2):
                            t_, tn_ = tmp.next()
                            kb.op("dve", lambda e: e.max(out=v_[:, u, 0:8], in_=s_[:, 2 * h + u, :]), reads=[sn_], writes=[vn_])
                            kb.op("dve", lambda e: e.match_replace(out=t_[:], in_to_replace=v_[:, u, 0:8], in_values=s_[:, 2 * h + u, :], imm_value=BIGN),
                                  reads=[sn_, vn_], writes=[tn_])
                            kb.op("dve", lambda e: e.max(out=v_[:, u, 8:16], in_=t_[:]), reads=[tn_], writes=[vn_])
                        kb.op("dve", lambda e: e.tensor_tensor(out=c_[:], in0=v_[:, 0, :].unsqueeze(2).to_broadcast([128, 16, 16]),
                                                               in1=v_[:, 1, :].unsqueeze(1).to_broadcast([128, 16, 16]), op=ALU.add), reads=[vn_], writes=[cn_])
                        cf = c_[:].rearrange("p a b -> p (a b)")
                        kb.op("dve", lambda e: e.max(out=tv_[:, 0:8], in_=cf), reads=[cn_], writes=[tvn])
                        kb.op("dve", lambda e: e.match_replace(out=t2[:], in_to_replace=tv_[:, 0:8], in_values=cf, imm_value=BIGN), reads=[cn_, tvn], writes=[t2n])
                        kb.op("dve", lambda e: e.max(out=tv_[:, 8:16], in_=t2[:]), reads=[t2n], writes=[tvn])
                        kb.op("dve", lambda e: e.tensor_scalar(out=th[:, h:h + 1], in0=tv_[:, 15:16], scalar1=-1.0, scalar2=None, op0=ALU.mult), reads=[tvn], writes=[thn])
                        kb.op("act", lambda e: e.activation(out=e_[:], in_=tv_[:], func=AF.Exp, bias=th[:, h:h + 1], scale=1.0), reads=[tvn, thn], writes=[en_])
                        kb.op("dve", lambda e: e.tensor_reduce(out=z_[:, h:h + 1], in_=e_[:], axis=AX.X, op=ALU.add), reads=[en_], writes=[zn_])
                    kb.op("act", lambda e: e.activation(out=z_[:], in_=z_[:], func=AF.Ln), reads=[zn_], writes=[zn_])
                    kb.op("dve", lambda e: e.tensor_scalar(out=th[:, 8:16], in0=z_[:], scalar1=-1.0, scalar2=None, op0=ALU.mult), reads=[zn_], writes=[thn])
                    kb.dma(thr_d[t0:t0 + 128, :], th[:], reads=[thn], writes=["dram_thr_d"])
                kb.barrier()

        def stage_peer_main(src, dst, t_lo, off=0):
            DELTA = -1.0e-4
            NTG = cfg.ntg
            tiles = list(range(t_lo // 128, NT))
            n_ = len(tiles)
            sizes = [NTG] * (n_ // NTG)
            r_ = n_ % NTG
            if r_ == 3:
                sizes.append(3)
            elif r_ == 2:
                sizes[-1:] = [3, 3]
            elif r_ == 1:
                sizes[-2:] = [3, 3, 3]
            assert sum(sizes) == n_ and min(sizes) >= 3 and NTG == 4
            groups, p_ = [], 0
            for z in sizes:
                groups.append(tiles[p_:p_ + z])
                p_ += z
            with ExitStack() as st:
                acc = [st.enter_context(sb("pm_acc%d" % k, [128, D], F32)) for k in range(NTG)]
                h2 = [st.enter_context(sb("pm_h2%d" % k, [128, NKC, 128], BF16)) for k in range(NTG)]
                s2 = [st.enter_context(sb("pm_s2%d" % k, [128, 8, 128], F32)) for k in range(NTG)]
                s1t = [st.enter_context(sb("pm_s1t%d" % k, [128, 8, 128], F32)) for k in range(NTG)]
                th = [st.enter_context(sb("pm_th%d" % k, [128, 16], F32)) for k in range(NTG)]
                ut = Rot(st, sb, "pm_ut", 2, [128, NKC, 512], BF16)
                vt = Rot(st, sb, "pm_vt", 2, [128, 4, D], BF16)
                ge = Rot(st, sb, "pm_ge", 4, [128, 512], F32)
                E_ = Rot(st, sb, "pm_E", 4, [128, 4, 128], F32)
                G_ = Rot(st, sb, "pm_G", 3, [128, 4, 128], F32)
                ga = Rot(st, sb, "pm_ga", 3, [128, 4, 128], F32)
                ap = Rot(st, sb, "pm_ap", 2, [128, 512], BF16)
                at = Rot(st, sb, "pm_at", 2, [128, 4, 128], BF16)
                p_a = Rot(st, ps, "pm_pa", 2, [128, 512], F32)
                p_t = st.enter_context(ps("pm_pt", [128, 4, 128], BF16))
                p_o = st.enter_context(ps("pm_po", [128, 4, 512], F32))
                for grp in groups:
                    for k, ti in enumerate(grp):
                        t0 = ti * 128
                        scv = sc_d[t0:t0 + 128, :].rearrange("p (h u k) -> p h u k", u=2, k=128)
                        kb.dma(h2[k][:], hT[:, t0:t0 + 128].rearrange("(kc p) t -> p kc t", p=128), reads=["dram_hT"], writes=["pm_h2%d" % k])
                        kb.dma(s2[k][:], scv[:, :, 1, :], reads=["dram_sc_d"], writes=["pm_s2%d" % k])
                        kb.dma(s1t[k][:], scv[:, :, 0, :], reads=["dram_sc_d"], writes=["pm_s1t%d" % k])
                        kb.dma(th[k][:], thr_d[t0:t0 + 128, :], reads=["dram_thr_d"], writes=["pm_th%d" % k])
                        kb.op("dve", lambda e: e.tensor_tensor(out=th[k][:, 0:8], in0=th[k][:, 0:8], in1=th[k][:, 8:16], op=ALU.add),
                              reads=["pm_th%d" % k], writes=["pm_th%d" % k])
                        kb.op("dve", lambda e: e.tensor_tensor(out=s1t[k][:], in0=s1t[k][:], in1=th[k][:, 0:8].unsqueeze(2).to_broadcast([128, 8, 128]), op=ALU.add),
                              reads=["pm_s1t%d" % k, "pm_th%d" % k], writes=["pm_s1t%d" % k])
                        kb.op("dve", lambda e: e.tensor_scalar(out=th[k][:, 8:16], in0=th[k][:, 8:16], scalar1=DELTA, scalar2=None, op0=ALU.add),
                              reads=["pm_th%d" % k], writes=["pm_th%d" % k])
                        kb.op("act", lambda e: e.activation(out=th[k][:, 8:16], in_=th[k][:, 8:16], func=AF.Exp), reads=["pm_th%d" % k], writes=["pm_th%d" % k])
                    igc = {}
                    units = [(ig, k) for ig in range(32) for k in range(len(grp))]

                    def P1(ig, k):
                        c = {}
                        if k == 0:
                            u_, un = ut.next()
                            v_, vn = vt.next()
                            kb.dma(u_[:].rearrange("p k e -> p (k e)"), UTd[ig], reads=["dram_UTd"], writes=[un])
                            kb.dma(v_[:], Vd[ig * 512:(ig + 1) * 512, :].rearrange("(et p) d -> p et d", p=128), reads=["dram_Vd"], writes=[vn])
                            igc[ig] = (u_, un, v_, vn)
                        u_, un, v_, vn = igc[ig]
                        pa, pan = p_a.next()
                        g_, gn = ge.next()
                        for kc in range(NKC):
                            kb.op("pe", lambda e: e.matmul(pa[:], lhsT=h2[k][:, kc, :], rhs=u_[:, kc, :], start=(kc == 0), stop=(kc == NKC - 1)),
                                  reads=["pm_h2%d" % k, un], writes=[pan])
                        kb.op("act", lambda e: e.activation(out=g_[:], in_=pa[:], func=AF.Gelu), reads=[pan], writes=[gn])
                        c["g"] = (g_, gn)
                        return c

                    def P2(ig, k, c):
                        ga_, gan = ga.next()
                        c["ga"] = (ga_, gan)
                        for h in range(8):
                            e_, en = E_.next()
                            for i4 in range(4):
                                col = ig * 4 + i4
                                kb.op("act", lambda e: e.activation(out=e_[:, i4, :], in_=s2[k][:, h, :], func=AF.Exp, bias=s1t[k][:, h, col:col + 1], scale=1.0),
                                      reads=["pm_s2%d" % k, "pm_s1t%d" % k], writes=[en])
                            if h == 0:
                                kb.op("dve", lambda e: e.scalar_tensor_tensor(out=ga_[:], in0=e_[:], scalar=th[k][:, 8 + h:9 + h], in1=e_[:], op0=ALU.is_ge, op1=ALU.mult),
                                      reads=[en, "pm_th%d" % k], writes=[gan])
                            else:
                                m_, mn = G_.next()
                                kb.op("dve", lambda e: e.scalar_tensor_tensor(out=m_[:], in0=e_[:], scalar=th[k][:, 8 + h:9 + h], in1=e_[:], op0=ALU.is_ge, op1=ALU.mult),
                                      reads=[en, "pm_th%d" % k], writes=[mn])
                                kb.op("pool", lambda e: e.tensor_tensor(out=ga_[:], in0=ga_[:], in1=m_[:], op=ALU.add), reads=[gan, mn], writes=[gan])

                    def P3a1(ig, k, c):
                        g_, gn = c["g"]
                        ga_, gan = c["ga"]
                        ap_, apn = ap.next()
                        kb.op("dve", lambda e: e.tensor_tensor(out=ap_[:], in0=g_[:], in1=ga_[:].rearrange("p a b -> p (a b)"), op=ALU.mult), reads=[gn, gan], writes=[apn])
                        for et in range(4):
                            kb.op("pe", lambda e: e.transpose(out=p_t[:, et, :], in_=ap_[:, et * 128:(et + 1) * 128], identity=ident_b[:]),
                                  reads=[apn, "ident_b"], writes=["pm_pt"])

                    def P3a2(ig, k, c):
                        u_, un, v_, vn = igc[ig]
                        at_, atn = at.next()
                        kb.op("act", lambda e: e.copy(out=at_[:], in_=p_t[:]), reads=["pm_pt"], writes=[atn])
                        for et in range(4):
                            for dc in range(4):
                                kb.op("pe", lambda e: e.matmul(p_o[:, dc, :], lhsT=at_[:, et, :], rhs=v_[:, et, dc * 512:(dc + 1) * 512],
                                                               start=(et == 0), stop=(et == 3)), reads=[atn, vn], writes=["pm_po"])

                    def P3b(ig, k, c):
                        an = "pm_acc%d" % k
                        if ig == 0:
                            for dc in range(4):
                                sl = slice(dc * 512, (dc + 1) * 512)
                                kb.op("act", lambda e: e.copy(out=acc[k][:, sl], in_=p_o[:, dc, :]), reads=["pm_po"], writes=[an])
                        else:
                            for hf in range(2):
                                sl = slice(hf * 1024, (hf + 1) * 1024)
                                kb.op("dve", lambda e: e.tensor_tensor(out=acc[k][:, sl], in0=p_o[:, 2 * hf:2 * hf + 2, :].rearrange("p a b -> p (a b)"), in1=acc[k][:, sl], op=ALU.add),
                                      reads=["pm_po", an], writes=[an])

                    N_ = len(units)
                    cs_ = {}
                    for i in range(min(2, N_)):
                        cs_[i] = P1(*units[i])
                    for i in range(N_ + 2):
                        if 0 <= i - 1 < N_:
                            P3a1(*units[i - 1], cs_[i - 1])
                        if i < N_:
                            P2(*units[i], cs_[i])
                        if i + 2 < N_:
                            cs_[i + 2] = P1(*units[i + 2])
                        if 0 <= i - 2 < N_:
                            P3b(*units[i - 2], cs_[i - 2])
                            del cs_[i - 2]
                        if 0 <= i - 1 < N_:
                            P3a2(*units[i - 1], cs_[i - 1])
                    ub0, ub0n = ut.items[0]
                    ub1, ub1n = ut.items[1]
                    gtb = ub0[:].rearrange("p k e -> p (k e)").bitcast(F32)[:, 0:D]
                    for k, ti in enumerate(grp):
                        t0 = ti * 128
                        j = 1 if t0 < TC else 0
                        xin = ub1[:].rearrange("p k e -> p (k e)").bitcast(F32)[:, 0:D]
                        kb.dma(gtb, modd[j:j + 1, 80 * 128:96 * 128].to_broadcast([128, D]), reads=["modd"], writes=[ub0n])
                        kb.dma(xin, src[t0:t0 + 128, :], reads=["dram_x1"], writes=[ub1n])
                        an = "pm_acc%d" % k
                        kb.op("dve", lambda e: e.tensor_tensor(out=acc[k][:], in0=acc[k][:], in1=gtb, op=ALU.mult), reads=[an, ub0n], writes=[an])
                        kb.op("pool", lambda e: e.tensor_tensor(out=acc[k][:], in0=acc[k][:], in1=xin, op=ALU.add), reads=[an, ub1n], writes=[an])
                        kb.dma(dst[t0 - off:t0 - off + 128, :], acc[k][:], reads=[an], writes=["dram_x2"])
                kb.barrier()

        kb.dma(xs[0][0:TC, :], I["ctx"][:, :], writes=["dram_x"])
        for t in range(0, TL, 1024):
            n = min(1024, TL - t)
            kb.dma(xs[0][TC + t:TC + t + n, :], I["x"][t:t + n, :], writes=["dram_x"])
        kb.barrier()
        xcur = xs[0]
        for l in range(L):
            last = (l == L - 1) and not cfg.dbg
            if run("prep"):
                conv_bf16(I["w_in"][l], wb_in, D, IN_W, "wb_in")
            if run("mod"):
                stage_mod(l)
            if run("norm1"):
                stage_norm(xcur, 0)
            if run("proj"):
                stage_proj()
            if run("ssd"):
                stage_ssd_conv(l)
                stage_ssd_tok()
                stage_ssd_scan(l, 0)
                stage_ssd_scan(l, 1)
            if run("gla"):
                stage_gla(l, 0)
                stage_gla(l, 1)
            if run("attn"):
                stage_qknorm(l)
                stage_attn(l, "na")
                stage_attn(l, "swa")
            if run("merge"):
                conv_bf16(I["w_branch"][l], wb_br, 4096, D, "wb_br")
                conv_bf16(I["w_out"][l], wb_out, D, D, "wb_out")
                stage_merge()
                stage_outproj(xcur, xs[1])
            if run("peer"):
                stage_peer_prep(l)
                stage_norm(xs[1], 1)
                stage_peer_q()
                stage_peer_topk(l)
                if last:
                    stage_peer_main(xs[1], y, TC, off=TC)
                else:
                    xnext = xs[2] if xcur is xs[0] else xs[0]
                    stage_peer_main(xs[1], xnext, 0)
                    xcur = xnext
            if cfg.dbg and l == cfg.dbg_layer:
                break

        for name in cfg.dbg:
            src = {"hT": hT, "PT": PTa, "PK": PK, "modd": modd, "aT": aT, "xcT": xcT, "yfT": yfT, "cT": cT, "ofT": ofT, "bT": bT, "dT": dT, "nqk": nqk, "sqk": sqk, "x1": xs[1], "x2": xs[2], "mTd": mTd, "qTd": qTd, "sc_d": sc_d, "thr_d": thr_d}[name]
            o = dout("dbg_" + name, list(src.shape), src.dtype)
            dbg_out[name] = o
            R = src.shape[0]
            step = max(1, (32 << 20) // (src.shape[1] * 4))
            for r in range(0, R, step):
                n = min(step, R - r)
                kb.dma(o[r:r + n, :], src[r:r + n, :], reads=["dram_" + name if name != "modd" else "modd"], writes=["out_" + name])
        if cfg.dbg or cfg.stages is not None:
            kb.dma(y[:, :], xs[0][TC:T, :], reads=["dram_x"], writes=["y"])
        kb.barrier()
        print("build: ninst", kb.ninst, "nwait", kb.nwait)
        print("cnt", kb.tot, "nsem", kb.nsem)
    return nc


def host_inputs(cfg, inp, b):
    L = cfg.L
    f = lambda a: np.ascontiguousarray(a, dtype=np.float32)
    m = {}
    m["x"] = f(inp["x"][b])
    m["ctx"] = f(inp["ctx"][b])
    cc = np.stack([inp["c"][b], inp["c_ctx"]], axis=-1)
    m["cc"] = f(cc.reshape(NKC, 128, 2).transpose(1, 0, 2).reshape(128, 32))
    m["w_ada"] = f(inp["w_ada"])
    m["bada"] = f(inp["b_ada"].reshape(L, 96, 128).transpose(0, 2, 1))
    gn = np.concatenate([inp["g_norm1"].reshape(L, 16, 128).transpose(0, 2, 1),
                         inp["g_norm2"].reshape(L, 16, 128).transpose(0, 2, 1)], axis=2)
    m["gn"] = f(gn)
    m["w_in"] = f(inp["w_in"])
    m["ssd_cw"] = f(inp["ssd_conv_w"].reshape(L, 5, 16, 128).transpose(0, 3, 2, 1))
    m["ssd_cb"] = f(inp["ssd_conv_b"].reshape(L, 16, 128).transpose(0, 2, 1))
    m["ssd_row"] = f(np.concatenate([inp["ssd_a_log"].reshape(L, 32), inp["ssd_dt_bias"].reshape(L, 32)], axis=1).reshape(L, 1, 64))
    dch = np.repeat(inp["ssd_d"], 64, axis=1).reshape(L, 8, 128).transpose(0, 2, 1)
    ng = inp["ssd_norm_g"].reshape(L, 8, 128).transpose(0, 2, 1)
    m["ssd_dg"] = f(np.concatenate([dch, ng], axis=2))
    t2_ = lambda a: np.tile(a, (1, 2))
    m["att_g"] = f(np.stack([t2_(inp["na_q_norm"]), t2_(inp["na_k_norm"]), t2_(inp["swa_q_norm"]), t2_(inp["swa_k_norm"])], axis=2))
    m["esink"] = f(np.broadcast_to(inp["swa_sink"][:, None, :], (L, 64, 16)))
    m["rope"] = rope_tables(cfg.TL)
    m["pm"] = rot_matrix()
    m["na_bias"] = f(np.stack([na_bias_tables(cfg.TL, inp["na_rpb"][l]) for l in range(L)], 0))
    m["w_branch"] = f(inp["w_branch"].reshape(L, 4096, D))
    m["w_out"] = f(inp["w_out"])
    m["peer_wq"] = f(inp["peer_wq"])
    m["peer_kk"] = f(np.stack([inp["peer_k1"].transpose(0, 2, 1), inp["peer_k2"].transpose(0, 2, 1)], axis=2))
    m["peer_u"] = f(inp["peer_u"])
    m["peer_v"] = f(inp["peer_v"])
    m["gla_wg"] = f(inp["gla_w_gate"])
    m["gla_bg"] = f(inp["gla_b_gate"].reshape(L, 2, 1, 512))
    m["gla_ng"] = f(inp["gla_norm_g"].reshape(L, 2, 128).transpose(0, 2, 1))
    return m


def kernel(**inputs):
    cfg = Cfg()
    nc = build(cfg)
    B = inputs["x"].shape[0]
    in_maps = [host_inputs(cfg, inputs, b) for b in range(B)]
    res = run_bass_kernel_spmd(nc, in_maps, core_ids=list(range(B)))
    return np.stack([res.results[b]["y"] for b in range(B)], axis=0).astype(np.float32)
```
